# Optimizing a Trainium2 kernel written in Bass

```python
import math
import jax, jax.numpy as jnp
from jax import lax
import numpy as np

D_MODEL = 4096
BATCH = 2
SEQ = 4096
DEPTH = 2

MIX_HALF = D_MODEL // 2
SB_HEAD_DIM = 128
SB_HEADS = MIX_HALF // SB_HEAD_DIM
SB_BLOCK = 128
GM_GROUP_DIM = 128
GM_GROUPS = MIX_HALF // GM_GROUP_DIM
GM_CHUNK = 128
SSM_WIDTH = D_MODEL
SSM_GROUP = 16
SSM_GROUPS = SSM_WIDTH // SSM_GROUP
SSM_STATE = 64
DT_MIN = 1e-3
DT_MAX = 1e-1
FFN_DENSE = 11008
N_EXPERTS = 8
TOP_K = 2
FFN_EXPERT = 4096
EPS = 1e-6

kernel_name = 'hybrid_sb_gmlp_s5_moe_adaln'


def rms_norm(x, g):
    xf = x.astype(jnp.float32)
    y = xf * lax.rsqrt(jnp.mean(xf * xf, axis=-1, keepdims=True) + EPS)
    return (y * g.astype(jnp.float32)).astype(x.dtype)


def ada_params(c, ada_w, ada_b):
    m = (jax.nn.silu(c) @ ada_w + ada_b)[:, None, :]
    shift, scale, gate = jnp.split(m, 3, axis=-1)
    return shift, scale, gate


def modulated_norm(x, g, shift, scale):
    return rms_norm(x, g) * (1.0 + scale) + shift


def swiglu(h, w_gate, w_up, w_down):
    return (jax.nn.silu(h @ w_gate) * (h @ w_up)) @ w_down


def stick_breaking_attention(q, k, v):
    bsz, seq, nh, dh = q.shape
    nb = seq // SB_BLOCK
    qb = q.reshape(bsz, nb, SB_BLOCK, nh, dh).transpose(1, 0, 3, 2, 4).astype(jnp.float32)
    kf = k.transpose(0, 2, 1, 3).astype(jnp.float32)
    vf = v.transpose(0, 2, 1, 3).astype(jnp.float32)
    key_pos = jnp.arange(seq)
    inv_sqrt_d = 1.0 / math.sqrt(dh)

    def one_block(args):
        q_blk, blk_idx = args
        q_pos = blk_idx * SB_BLOCK + jnp.arange(SB_BLOCK)
        z = jnp.einsum('bhqd,bhkd->bhqk', q_blk, kf) * inv_sqrt_d
        past = key_pos[None, :] < q_pos[:, None]
        log_beta = jax.nn.log_sigmoid(z)
        log_one_minus = jnp.where(past, jax.nn.log_sigmoid(-z), 0.0)
        after = lax.cumsum(log_one_minus, axis=3, reverse=True) - log_one_minus
        w = jnp.where(past, jnp.exp(log_beta + after), 0.0)
        return jnp.einsum('bhqk,bhkd->bhqd', w, vf)

    out = lax.map(one_block, (qb, jnp.arange(nb)))
    return out.transpose(1, 0, 3, 2, 4).reshape(bsz, seq, nh, dh).astype(q.dtype)


def chunked_spatial_gating(z1, z2, ln_g, w_s, b_s):
    bsz, seq, ng, cd = z1.shape
    nc = seq // GM_CHUNK
    u = jax.nn.gelu(z1)
    vf = jax.nn.gelu(z2).astype(jnp.float32)
    mu = jnp.mean(vf, axis=-1, keepdims=True)
    var = jnp.mean(jnp.square(vf - mu), axis=-1, keepdims=True)
    vn = (vf - mu) * lax.rsqrt(var + EPS) * ln_g.astype(jnp.float32)
    vn = vn.reshape(bsz, nc, GM_CHUNK, ng, cd)
    causal = jnp.tril(jnp.ones((GM_CHUNK, GM_CHUNK), dtype=bool))
    w = jnp.where(causal[None], w_s.astype(jnp.float32), 0.0)
    mixed = jnp.einsum('gts,bnsgc->bntgc', w, vn) + b_s.astype(jnp.float32).T[None, None, :, :, None]
    return u * mixed.reshape(bsz, seq, ng, cd).astype(u.dtype)


def _ssm_combine(left, right):
    a1r, a1i, b1r, b1i = left
    a2r, a2i, b2r, b2i = right
    return (a2r * a1r - a2i * a1i,
            a2r * a1i + a2i * a1r,
            a2r * b1r - a2i * b1i + b2r,
            a2r * b1i + a2i * b1r + b2i)


def s5_ssm(u, lam_re, lam_im, log_dt, b_re, b_im, c_re, c_im, d_skip):
    bsz, seq, width = u.shape
    uf = u.astype(jnp.float32).reshape(bsz, seq, SSM_GROUPS, SSM_GROUP)
    lr = jnp.minimum(lam_re.astype(jnp.float32), -1e-4)
    li = lam_im.astype(jnp.float32)
    dt = jnp.exp(log_dt.astype(jnp.float32))[:, None]
    mag = jnp.exp(lr * dt)
    a_re = mag * jnp.cos(li * dt)
    a_im = mag * jnp.sin(li * dt)
    den = lr * lr + li * li
    nr = a_re - 1.0
    f_re = (nr * lr + a_im * li) / den
    f_im = (a_im * lr - nr * li) / den
    br = b_re.astype(jnp.float32)
    bi = b_im.astype(jnp.float32)
    bb_re = f_re[:, :, None] * br - f_im[:, :, None] * bi
    bb_im = f_re[:, :, None] * bi + f_im[:, :, None] * br
    bu_re = jnp.einsum('gpc,bsgc->bsgp', bb_re, uf)
    bu_im = jnp.einsum('gpc,bsgc->bsgp', bb_im, uf)
    full = (bsz, seq, SSM_GROUPS, SSM_STATE)
    a_re_t = jnp.broadcast_to(a_re[None, None], full)
    a_im_t = jnp.broadcast_to(a_im[None, None], full)
    _, _, x_re, x_im = lax.associative_scan(_ssm_combine, (a_re_t, a_im_t, bu_re, bu_im), axis=1)
    y = (jnp.einsum('gcp,bsgp->bsgc', c_re.astype(jnp.float32), x_re)
         - jnp.einsum('gcp,bsgp->bsgc', c_im.astype(jnp.float32), x_im))
    y = y.reshape(bsz, seq, width) + d_skip.astype(jnp.float32) * u.astype(jnp.float32)
    return y.astype(u.dtype)


def moe_swiglu(h, w_router, w_gate, w_up, w_down):
    logits = jnp.einsum('bsd,de->bse', h, w_router).astype(jnp.float32)
    top_val, top_idx = lax.top_k(logits, TOP_K)
    probs = jax.nn.softmax(top_val, axis=-1)
    gates = jnp.sum(jax.nn.one_hot(top_idx, N_EXPERTS, dtype=jnp.float32) * probs[..., None], axis=-2)
    out = jnp.zeros_like(h)
    for e in range(N_EXPERTS):
        out = out + gates[..., e:e + 1].astype(h.dtype) * swiglu(h, w_gate[e], w_up[e], w_down[e])
    return out


def setup_inputs(seed: int = 0) -> dict:
    key = jax.random.key(seed)
    keys = iter(jax.random.split(key, 64))
    n_e = (DEPTH + 1) // 2
    n_o = DEPTH // 2
    D = D_MODEL

    def nrm(shape, scale):
        return jax.random.normal(next(keys), shape, jnp.float32) * scale

    def gain(shape):
        return 1.0 + nrm(shape, 0.02)

    def ada_w(n):
        return nrm((n, D, 3 * D), 0.2 * D ** -0.5)

    def ada_b(n):
        return jnp.concatenate([nrm((n, 2 * D), 0.02), 1.0 + nrm((n, D), 0.02)], axis=-1)

    lam_im0 = jnp.pi * jnp.arange(SSM_STATE, dtype=jnp.float32)
    inp = {
        'x': nrm((BATCH, SEQ, D), 1.0),
        'c': nrm((BATCH, D), 1.0),
        'mix0_norm_g': gain((n_e, D)),
        'mix0_ada_w': ada_w(n_e),
        'mix0_ada_b': ada_b(n_e),
        'mix0_w_in': nrm((n_e, D, 5 * MIX_HALF), D ** -0.5),
        'gm_ln_g': gain((n_e, GM_GROUPS, GM_GROUP_DIM)),
        'gm_w_s': nrm((n_e, GM_GROUPS, GM_CHUNK, GM_CHUNK), GM_CHUNK ** -0.5),
        'gm_b_s': gain((n_e, GM_GROUPS, GM_CHUNK)),
        'mix0_w_out': nrm((n_e, 2 * MIX_HALF, D), (2 * MIX_HALF) ** -0.5),
        'ffn0_norm_g': gain((n_e, D)),
        'ffn0_ada_w': ada_w(n_e),
        'ffn0_ada_b': ada_b(n_e),
        'ffn0_w_gate': nrm((n_e, D, FFN_DENSE), D ** -0.5),
        'ffn0_w_up': nrm((n_e, D, FFN_DENSE), D ** -0.5),
        'ffn0_w_down': nrm((n_e, FFN_DENSE, D), FFN_DENSE ** -0.5),
        'mix1_norm_g': gain((n_o, D)),
        'mix1_ada_w': ada_w(n_o),
        'mix1_ada_b': ada_b(n_o),
        'ssm_w_in': nrm((n_o, D, SSM_WIDTH), D ** -0.5),
        'ssm_lam_re': -0.5 + nrm((n_o, SSM_GROUPS, SSM_STATE), 0.01),
        'ssm_lam_im': lam_im0 + nrm((n_o, SSM_GROUPS, SSM_STATE), 0.01),
        'ssm_log_dt': jax.random.uniform(next(keys), (n_o, SSM_GROUPS), jnp.float32,
                                         minval=math.log(DT_MIN), maxval=math.log(DT_MAX)),
        'ssm_b_re': nrm((n_o, SSM_GROUPS, SSM_STATE, SSM_GROUP), (2 * SSM_GROUP) ** -0.5),
        'ssm_b_im': nrm((n_o, SSM_GROUPS, SSM_STATE, SSM_GROUP), (2 * SSM_GROUP) ** -0.5),
        'ssm_c_re': nrm((n_o, SSM_GROUPS, SSM_GROUP, SSM_STATE), 0.5),
        'ssm_c_im': nrm((n_o, SSM_GROUPS, SSM_GROUP, SSM_STATE), 0.5),
        'ssm_d': nrm((n_o, SSM_WIDTH), 0.5),
        'glu_w_a': nrm((n_o, SSM_WIDTH, D), SSM_WIDTH ** -0.5),
        'glu_w_b': nrm((n_o, SSM_WIDTH, D), SSM_WIDTH ** -0.5),
        'moe_norm_g': gain((n_o, D)),
        'moe_ada_w': ada_w(n_o),
        'moe_ada_b': ada_b(n_o),
        'moe_w_router': nrm((n_o, D, N_EXPERTS), D ** -0.5),
        'moe_w_gate': nrm((n_o, N_EXPERTS, D, FFN_EXPERT), D ** -0.5),
        'moe_w_up': nrm((n_o, N_EXPERTS, D, FFN_EXPERT), D ** -0.5),
        'moe_w_down': nrm((n_o, N_EXPERTS, FFN_EXPERT, D), FFN_EXPERT ** -0.5),
        'final_norm_g': gain((D,)),
    }
    return inp


def reference(x, c, mix0_norm_g, mix0_ada_w, mix0_ada_b, mix0_w_in, gm_ln_g, gm_w_s, gm_b_s, mix0_w_out,
              ffn0_norm_g, ffn0_ada_w, ffn0_ada_b, ffn0_w_gate, ffn0_w_up, ffn0_w_down,
              mix1_norm_g, mix1_ada_w, mix1_ada_b, ssm_w_in, ssm_lam_re, ssm_lam_im, ssm_log_dt,
              ssm_b_re, ssm_b_im, ssm_c_re, ssm_c_im, ssm_d, glu_w_a, glu_w_b,
              moe_norm_g, moe_ada_w, moe_ada_b, moe_w_router, moe_w_gate, moe_w_up, moe_w_down,
              final_norm_g):
    bsz, seq, _ = x.shape
    for layer in range(DEPTH):
        i = layer // 2
        if layer % 2 == 0:
            shift, scale, gate = ada_params(c, mix0_ada_w[i], mix0_ada_b[i])
            h = modulated_norm(x, mix0_norm_g[i], shift, scale)
            q, k, v, z1, z2 = jnp.split(h @ mix0_w_in[i], 5, axis=-1)
            sb_shape = (bsz, seq, SB_HEADS, SB_HEAD_DIM)
            gm_shape = (bsz, seq, GM_GROUPS, GM_GROUP_DIM)
            a_out = stick_breaking_attention(q.reshape(sb_shape), k.reshape(sb_shape), v.reshape(sb_shape))
            b_out = chunked_spatial_gating(z1.reshape(gm_shape), z2.reshape(gm_shape),
                                           gm_ln_g[i], gm_w_s[i], gm_b_s[i])
            mixed = jnp.concatenate([a_out.reshape(bsz, seq, MIX_HALF), b_out.reshape(bsz, seq, MIX_HALF)], axis=-1)
            x = x + gate * (mixed @ mix0_w_out[i])
            shift, scale, gate = ada_params(c, ffn0_ada_w[i], ffn0_ada_b[i])
            h = modulated_norm(x, ffn0_norm_g[i], shift, scale)
            x = x + gate * swiglu(h, ffn0_w_gate[i], ffn0_w_up[i], ffn0_w_down[i])
        else:
            shift, scale, gate = ada_params(c, mix1_ada_w[i], mix1_ada_b[i])
            h = modulated_norm(x, mix1_norm_g[i], shift, scale)
            y = s5_ssm(h @ ssm_w_in[i], ssm_lam_re[i], ssm_lam_im[i], ssm_log_dt[i],
                       ssm_b_re[i], ssm_b_im[i], ssm_c_re[i], ssm_c_im[i], ssm_d[i])
            y = jax.nn.gelu(y)
            x = x + gate * ((y @ glu_w_a[i]) * jax.nn.sigmoid(y @ glu_w_b[i]))
            shift, scale, gate = ada_params(c, moe_ada_w[i], moe_ada_b[i])
            h = modulated_norm(x, moe_norm_g[i], shift, scale)
            x = x + gate * moe_swiglu(h, moe_w_router[i], moe_w_gate[i], moe_w_up[i], moe_w_down[i])
    return rms_norm(x, final_norm_g)
```

```python
import math
from contextlib import ExitStack

import numpy as np
import concourse.bass as bass
import concourse.mybir as mybir
from concourse.bass_utils import run_bass_kernel_spmd

F32 = mybir.dt.float32
BF16 = mybir.dt.bfloat16
ALU = mybir.AluOpType
AF = mybir.ActivationFunctionType
AX = mybir.AxisListType

D = 4096
KC = D // 128
TB = 512
EPS = 1e-6
FFN_DENSE = 11008
N_EXP = 8
FFN_EXP = 4096


class _Op:
    __slots__ = ("eng", "fn", "deps", "sig", "sigval", "is_dma", "dsem", "dval", "dprev")

    def __init__(self, eng, fn, deps, is_dma):
        self.eng = eng
        self.fn = fn
        self.deps = deps
        self.sig = False
        self.sigval = 0
        self.is_dma = is_dma
        self.dsem = -1
        self.dval = 0
        self.dprev = 0


class Prog:
    ENG = ("pe", "act", "dve", "pool", "sp")

    def __init__(self, nc, stack, n_dsem=32):
        self.nc = nc
        self.sems = {e: stack.enter_context(nc.semaphore("s_" + e)) for e in self.ENG}
        self.sigcount = {e: 0 for e in self.ENG}
        self.dsems = [stack.enter_context(nc.semaphore("dq%d" % i)) for i in range(n_dsem)]
        self.dtotal = [0] * n_dsem
        self.dnext = 0
        self.seen = {e: {} for e in self.ENG}
        self._reset()

    def _reset(self):
        self.ops = {e: [] for e in self.ENG}
        self.order = []
        self.last_w = {}
        self.readers = {}

    def _rec(self, eng, fn, reads, writes, is_dma):
        deps = []
        for k in reads:
            w = self.last_w.get(k)
            if w is not None:
                deps.append(w)
        for k in writes:
            w = self.last_w.get(k)
            if w is not None:
                deps.append(w)
            deps.extend(self.readers.get(k, ()))
        o = _Op(eng, fn, deps, is_dma)
        for k in reads:
            self.readers.setdefault(k, []).append(o)
        for k in writes:
            self.last_w[k] = o
            self.readers[k] = []
        self.ops[eng].append(o)
        self.order.append(o)
        return o

    def op(self, eng, fn, reads=(), writes=()):
        return self._rec(eng, fn, reads, writes, False)

    def dma(self, queue, out, in_, reads=(), writes=(), **kw):
        return self._rec(queue, lambda e: e.dma_start(out=out, in_=in_, **kw), reads, writes, True)

    def emit(self):
        nc = self.nc
        for o in self.order:
            for d in o.deps:
                if d.is_dma:
                    continue
                if d.eng == "pe" and o.eng == "pe" and not o.is_dma:
                    continue
                d.sig = True
        for e in self.ENG:
            for o in reversed(self.ops[e]):
                if not o.is_dma:
                    o.sig = True
                    break
        for e in self.ENG:
            for o in self.ops[e]:
                if not o.is_dma and o.sig:
                    self.sigcount[e] += 1
                    o.sigval = self.sigcount[e]
        n = len(self.dsems)
        for o in self.order:
            if o.is_dma:
                k = self.dnext
                self.dnext = (k + 1) % n
                o.dsem = k
                o.dprev = self.dtotal[k]
                self.dtotal[k] += 16
                o.dval = self.dtotal[k]

        def run(engobj, e):
            seen = self.seen[e]

            def wait(key, sem, val):
                if val <= 0 or seen.get(key, 0) >= val:
                    return
                seen[key] = val
                engobj.wait_ge(sem, val)

            for o in self.ops[e]:
                for d in o.deps:
                    if d.is_dma:
                        wait(("d", d.dsem), self.dsems[d.dsem], d.dval)
                    elif d.eng == "pe" and e == "pe" and not o.is_dma:
                        continue
                    else:
                        wait(("c", d.eng), self.sems[d.eng], d.sigval)
                if o.is_dma:
                    wait(("d", o.dsem), self.dsems[o.dsem], o.dprev)
                ins = o.fn(engobj)
                if o.is_dma:
                    ins.then_inc(self.dsems[o.dsem], 16)
                elif o.sig:
                    ins.then_inc(self.sems[e], 1)
            for e2 in self.ENG:
                wait(("c", e2), self.sems[e2], self.sigcount[e2])
            for k in range(n):
                wait(("d", k), self.dsems[k], self.dtotal[k])

        with nc.Block() as block:
            @block.tensor
            def _(t):
                run(t, "pe")

            @block.scalar
            def _(t):
                run(t, "act")

            @block.vector
            def _(t):
                run(t, "dve")

            @block.gpsimd
            def _(t):
                run(t, "pool")

            @block.sync
            def _(t):
                run(t, "sp")
        self._reset()


class Builder:
    def __init__(self, S, stages, dbg=()):
        self.S = S
        self.NTB = S // TB
        self.stages = stages
        self.dbg = set(dbg)
        self.nc = bass.Bass("TRN2", target_bir_lowering=False)
        self.gstack = ExitStack()
        self.P = Prog(self.nc, self.gstack)
        self.inputs = {}
        self.ucount = 0

    def ext_in(self, name, shape, dtype=F32):
        t = self.nc.dram_tensor(name, list(shape), dtype, kind="ExternalInput").ap()
        self.inputs[name] = t
        return t

    def ext_out(self, name, shape, dtype=F32):
        return self.nc.dram_tensor(name, list(shape), dtype, kind="ExternalOutput").ap()

    def scratch(self, name, shape, dtype):
        return self.nc.dram_tensor(name, list(shape), dtype, kind="Internal").ap()

    def gtile(self, name, shape, dtype):
        return self.gstack.enter_context(self.nc.sbuf_tensor(name, list(shape), dtype))

    def mkT(self, st):
        self.ucount += 1
        sid = self.ucount
        nc = self.nc
        return lambda n, s, d: st.enter_context(nc.sbuf_tensor("%s_s%d" % (n, sid), list(s), d))

    def PS(self, st, name, shape, dtype):
        self.ucount += 1
        return st.enter_context(self.nc.psum_tensor("%s_p%d" % (name, self.ucount), list(shape), dtype))

    def uid(self, p):
        self.ucount += 1
        return "%s%d" % (p, self.ucount)

    def setup_consts(self):
        P = self.P
        self.ones_bf = self.gtile("ones_bf", [128, 128], BF16)
        self.ones_f = self.gtile("ones_f", [128, 128], F32)
        self.ident_f = self.gtile("ident_f", [128, 128], F32)
        self.ident_bf = self.gtile("ident_bf", [128, 128], BF16)
        P.op("pool", lambda e: e.memset(self.ones_bf[:], 1.0), writes=["ones_bf"])
        P.op("pool", lambda e: e.memset(self.ones_f[:], 1.0), writes=["ones_f"])
        P.op("pool", lambda e: e.affine_select(
            out=self.ident_f[:], in_=self.ones_f[:], pattern=[[-1, 128]], compare_op=ALU.is_equal,
            fill=0.0, base=0, channel_multiplier=1), reads=["ones_f"], writes=["ident_f"])
        P.op("dve", lambda e: e.tensor_copy(out=self.ident_bf[:], in_=self.ident_f[:]),
             reads=["ident_f"], writes=["ident_bf"])

    def stage_ada(self, c_ap, mats):
        nc, P = self.nc, self.P
        M = len(mats)
        self.ada = self.gtile("ada", [128, M, 96], F32)
        self.gsc = self.gtile("gsc", [128, M, KC], F32)
        with ExitStack() as st:
            T = self.mkT(st)
            cT = T("cT", [128, KC], F32)
            scT = T("scT", [128, KC], BF16)
            row = T("row", [1, 3 * D], F32)
            bias = T("bias", [128, 96], F32)
            gT = T("gT", [128, KC], F32)
            one11 = T("one11", [1, 1], F32)
            slab = [T("aslab%d" % i, [128, KC, 512], BF16) for i in range(2)]
            ps = [self.PS(st, "aps%d" % i, [128, 512], F32) for i in range(2)]
            psT = self.PS(st, "apsT", [128, 512], F32)
            P.dma("sp", cT[:], c_ap.rearrange("o (c p) -> p (o c)", p=128), writes=["cT"],
                  allow_slow_non_contiguous=True)
            P.op("pool", lambda e: e.memset(one11[:], 1.0), writes=["one11"])
            P.op("act", lambda e: e.activation(out=scT[:], in_=cT[:], func=AF.Silu),
                 reads=["cT"], writes=["scT"])
            it = 0
            for m, (w, b, g) in enumerate(mats):
                for q in range(3):
                    P.dma("sp", bias[:, q * 32:(q + 1) * 32],
                          b[:, q * D:(q + 1) * D].rearrange("o (c p) -> p (o c)", p=128),
                          writes=["bias%d" % q], allow_slow_non_contiguous=True)
                P.dma("sp", gT[:], g.rearrange("o (c p) -> p (o c)", p=128), writes=["gT"],
                      allow_slow_non_contiguous=True)
                for j in range(3 * D // 512):
                    sl = slab[it % 2]
                    pj = ps[it % 2]
                    sk = "aslab%d" % (it % 2)
                    pk = "aps%d" % (it % 2)
                    it += 1
                    P.dma("pool", sl[:], w[:, j * 512:(j + 1) * 512].rearrange("(c p) n -> p c n", p=128),
                          writes=[sk])
                    for kc in range(KC):
                        P.op("pe", lambda e, sl=sl, pj=pj, kc=kc: e.matmul(
                            pj[0:1, :], scT[:, kc:kc + 1], sl[:, kc, :], start=(kc == 0), stop=(kc == KC - 1)),
                            reads=[sk, "scT"], writes=[pk])
                    P.op("act", lambda e, pj=pj, j=j: e.copy(out=row[0:1, j * 512:(j + 1) * 512], in_=pj[0:1, :]),
                         reads=[pk], writes=["row"])
                for i in range(96):
                    P.op("pe", lambda e, i=i: e.matmul(psT[:, i:i + 1], row[0:1, i * 128:(i + 1) * 128],
                                                       one11[0:1, 0:1], start=True, stop=True),
                         reads=["row", "one11"], writes=["psT"])
                P.op("dve", lambda e, m=m: e.tensor_tensor(out=self.ada[:, m, :], in0=psT[:, 0:96], in1=bias[:],
                                                           op=ALU.add),
                     reads=["psT", "bias0", "bias1", "bias2"], writes=["ada"])
                P.op("dve", lambda e, m=m: e.scalar_tensor_tensor(
                    out=self.gsc[:, m, :], in0=self.ada[:, m, 32:64], scalar=1.0, in1=gT[:],
                    op0=ALU.add, op1=ALU.mult), reads=["ada", "gT"], writes=["gsc"])
            P.emit()

    def shift(self, m, c):
        return self.ada[:, m, c:c + 1]

    def gate(self, m, c):
        return self.ada[:, m, 64 + c:65 + c]

    def stage_norm(self, src, dst, m, out_dtype=BF16, final_g=None, router=None):
        nc, P = self.nc, self.P
        with ExitStack() as st:
            T = self.mkT(st)
            xt = T("n_xt", [128, KC, TB], F32)
            sq = T("n_sq", [128, KC, TB], BF16)
            hsb = T("n_h", [128, KC, TB], out_dtype)
            rstd = T("n_rstd", [128, TB], F32)
            tmp = [T("n_tmp%d" % i, [128, TB], F32) for i in range(2)]
            ps = self.PS(st, "n_ps", [128, TB], F32)
            if final_g is not None:
                gfin = T("n_gfin", [128, KC], F32)
                P.dma("sp", gfin[:], final_g.rearrange("o (c p) -> p (o c)", p=128), writes=["gfin"],
                      allow_slow_non_contiguous=True)
            if router is not None:
                wr, gdst = router
                wr_sb = T("n_wr", [128, KC, N_EXP], F32)
                h32 = [T("n_h32%d" % i, [128, TB], F32) for i in range(2)]
                lg = T("n_lg", [N_EXP, TB], F32)
                lgt = T("n_lgt", [128, 4, N_EXP], F32)
                top8 = T("n_top8", [128, 4, 8], F32)
                gm = T("n_gm", [128, 4, N_EXP], F32)
                ge = T("n_ge", [128, 4, N_EXP], F32)
                den = T("n_den", [128, 4], F32)
                nm1 = T("n_nm1", [128, 4], F32)
                gts = T("n_gts", [N_EXP, TB], F32)
                psl = self.PS(st, "n_psl", [128, TB], F32)
                pst = self.PS(st, "n_pst", [128, TB], F32)
                P.dma("sp", wr_sb[:], wr.rearrange("(c p) e -> p c e", p=128), writes=["wr"],
                      allow_slow_non_contiguous=True)
            for tb in range(self.NTB):
                ts = slice(tb * TB, (tb + 1) * TB)
                P.dma("sp", xt[:], src[:, ts].rearrange("(c p) t -> p c t", p=128), writes=["xt"])
                for q in range(4):
                    P.op("act", lambda e, q=q: e.activation(out=sq[:, q * 8:(q + 1) * 8, :],
                                                            in_=xt[:, q * 8:(q + 1) * 8, :], func=AF.Square),
                         reads=["xt"], writes=["sq%d" % q])
                for c in range(KC):
                    P.op("pe", lambda e, c=c: e.matmul(ps[:], self.ones_bf[:], sq[:, c, :],
                                                       start=(c == 0), stop=(c == KC - 1)),
                         reads=["sq%d" % (c // 8)], writes=["ps"])
                P.op("dve", lambda e: e.tensor_scalar(out=rstd[:], in0=ps[:], scalar1=1.0 / D, scalar2=EPS,
                                                      op0=ALU.mult, op1=ALU.add), reads=["ps"], writes=["rstd"])
                P.op("act", lambda e: e.activation(out=rstd[:], in_=rstd[:], func=AF.Sqrt),
                     reads=["rstd"], writes=["rstd"])
                P.op("dve", lambda e: e.reciprocal(out=rstd[:], in_=rstd[:]), reads=["rstd"], writes=["rstd"])
                for c in range(KC):
                    tk = "tmp%d" % (c % 2)
                    tt = tmp[c % 2]
                    P.op("dve", lambda e, c=c, tt=tt: e.tensor_tensor(out=tt[:], in0=xt[:, c, :], in1=rstd[:],
                                                                      op=ALU.mult),
                         reads=["xt", "rstd"], writes=[tk])
                    if final_g is not None:
                        P.op("act", lambda e, c=c, tt=tt: e.activation(out=hsb[:, c, :], in_=tt[:], func=AF.Copy,
                                                                       scale=gfin[:, c:c + 1]),
                             reads=[tk, "gfin"], writes=["hsb"])
                    elif router is None:
                        P.op("act", lambda e, c=c, tt=tt: e.activation(
                            out=hsb[:, c, :], in_=tt[:], func=AF.Identity, scale=self.gsc[:, m, c:c + 1],
                            bias=self.shift(m, c)), reads=[tk], writes=["hsb"])
                    else:
                        hk = "h32%d" % (c % 2)
                        hh = h32[c % 2]
                        P.op("act", lambda e, c=c, tt=tt, hh=hh: e.activation(
                            out=hh[:], in_=tt[:], func=AF.Identity, scale=self.gsc[:, m, c:c + 1],
                            bias=self.shift(m, c)), reads=[tk], writes=[hk])
                        P.op("dve", lambda e, c=c, hh=hh: e.tensor_copy(out=hsb[:, c, :], in_=hh[:]),
                             reads=[hk], writes=["hsb"])
                        P.op("pe", lambda e, c=c, hh=hh: e.matmul(psl[0:N_EXP, :], wr_sb[:, c, :], hh[:],
                                                                  start=(c == 0), stop=(c == KC - 1)),
                             reads=[hk, "wr"], writes=["psl"])
                P.dma("sp", dst[:, ts].rearrange("(c p) t -> p c t", p=128), hsb[:], reads=["hsb"])
                if router is not None:
                    P.op("act", lambda e: e.copy(out=lg[:], in_=psl[0:N_EXP, :]), reads=["psl"], writes=["lg"])
                    for q in range(4):
                        P.op("pe", lambda e, q=q: e.transpose(pst[:, q * N_EXP:(q + 1) * N_EXP],
                                                              lg[:, q * 128:(q + 1) * 128],
                                                              self.ident_f[0:N_EXP, 0:N_EXP]),
                             reads=["lg"], writes=["pst"])
                    P.op("dve", lambda e: e.tensor_copy(out=lgt[:], in_=pst[:, 0:4 * N_EXP].rearrange(
                        "p (q e) -> p q e", q=4)), reads=["pst"], writes=["lgt"])
                    for q in range(4):
                        P.op("dve", lambda e, q=q: e.max(out=top8[:, q, :], in_=lgt[:, q, :]),
                             reads=["lgt"], writes=["top8_%d" % q])
                    for q in range(4):
                        P.op("dve", lambda e, q=q: e.tensor_scalar(
                            out=gm[:, q, :], in0=lgt[:, q, :], scalar1=top8[:, q, 1:2], scalar2=None,
                            op0=ALU.is_ge), reads=["lgt", "top8_%d" % q], writes=["gm%d" % q])
                        P.op("dve", lambda e, q=q: e.tensor_scalar(
                            out=nm1[:, q:q + 1], in0=top8[:, q, 0:1], scalar1=-1.0, scalar2=None,
                            op0=ALU.mult), reads=["top8_%d" % q], writes=["nm1_%d" % q])
                        P.op("act", lambda e, q=q: e.activation(out=ge[:, q, :], in_=lgt[:, q, :], func=AF.Exp,
                                                                bias=nm1[:, q:q + 1]),
                             reads=["lgt", "nm1_%d" % q], writes=["ge%d" % q])
                        P.op("dve", lambda e, q=q: e.tensor_tensor(out=ge[:, q, :], in0=ge[:, q, :],
                                                                   in1=gm[:, q, :], op=ALU.mult),
                             reads=["ge%d" % q, "gm%d" % q], writes=["ge%d" % q])
                        P.op("dve", lambda e, q=q: e.reduce_sum(out=den[:, q:q + 1], in_=ge[:, q, :], axis=AX.X),
                             reads=["ge%d" % q], writes=["den%d" % q])
                        P.op("dve", lambda e, q=q: e.reciprocal(out=den[:, q:q + 1], in_=den[:, q:q + 1]),
                             reads=["den%d" % q], writes=["den%d" % q])
                        P.op("dve", lambda e, q=q: e.tensor_scalar(
                            out=ge[:, q, :], in0=ge[:, q, :], scalar1=den[:, q:q + 1], scalar2=None,
                            op0=ALU.mult), reads=["ge%d" % q, "den%d" % q], writes=["ge%d" % q])
                        P.op("pe", lambda e, q=q: e.transpose(psl[0:N_EXP, q * 128:(q + 1) * 128],
                                                              ge[:, q, :], self.ident_f[:]),
                             reads=["ge%d" % q], writes=["psl"])
                    P.op("act", lambda e: e.copy(out=gts[:], in_=psl[0:N_EXP, :]), reads=["psl"], writes=["gts"])
                    P.dma("sp", gdst[:, ts], gts[:], reads=["gts"])
            P.emit()

    def gelu_ops(self, pfx, src_ap, src_keys, out_ap, out_key, t1, t1k, t2, t2k):
        P = self.P
        P.op("act", lambda e: e.activation(out=t1, in_=src_ap, func=AF.Square), reads=src_keys, writes=[t1k])
        P.op("dve", lambda e: e.tensor_scalar(out=t1, in0=t1, scalar1=0.044715, scalar2=1.0,
                                              op0=ALU.mult, op1=ALU.add), reads=[t1k], writes=[t1k])
        P.op("dve", lambda e: e.tensor_tensor(out=t1, in0=t1, in1=src_ap, op=ALU.mult),
             reads=[t1k] + list(src_keys), writes=[t1k])
        P.op("act", lambda e: e.activation(out=t2, in_=t1, func=AF.Sigmoid, scale=1.5957691216057308),
             reads=[t1k], writes=[t2k])
        P.op("dve", lambda e: e.tensor_tensor(out=out_ap, in0=t2, in1=src_ap, op=ALU.mult),
             reads=[t2k] + list(src_keys), writes=[out_key])

    def stage_linear_fm(self, inT, K, jobs):
        nc, P = self.nc, self.P
        kcn = K // 128
        with ExitStack() as st:
            T = self.mkT(st)
            inb = T("l_in", [128, kcn, TB], BF16)
            nslab = 4
            slab = [T("l_slab%d" % i, [128, kcn, 256], BF16) for i in range(nslab)]
            ost = [T("l_ost%d" % i, [128, 2, TB], BF16) for i in range(2)]
            xo = [T("l_xo%d" % i, [128, TB], F32) for i in range(2)]
            xn = [T("l_xn%d" % i, [128, TB], F32) for i in range(2)]
            t1 = [T("l_t1%d" % i, [128, TB], F32) for i in range(2)]
            t2 = [T("l_t2%d" % i, [128, TB], F32) for i in range(2)]
            ps = [self.PS(st, "l_ps%d" % i, [128, TB], F32) for i in range(4)]
            si = 0
            ci = 0
            use_cache = self.NTB > 1
            if use_cache:
                sid = self.uid("lc")
                for ji, job in enumerate(jobs):
                    job["cache"] = [self.scratch("%s_%d_%d" % (sid, ji, wi), [job["N"] // 256, 128, kcn * 256], BF16)
                                    for wi in range(len(job["w"]))]
            for tb in range(self.NTB):
                ts = slice(tb * TB, (tb + 1) * TB)
                P.dma("sp", inb[:], inT[:, ts].rearrange("(c p) t -> p c t", p=128), writes=["inb"])
                for ji, job in enumerate(jobs):
                    N = job["N"]
                    epi = job["epi"]
                    for ng in range(N // 256):
                        sls = []
                        for wi, w in enumerate(job["w"]):
                            sl = slab[si % nslab]
                            sk = "slab%d" % (si % nslab)
                            si += 1
                            ck = "c%d_%d_%d" % (ji, wi, ng)
                            if tb == 0 or not use_cache:
                                P.dma("pool", sl[:], w[:, ng * 256:(ng + 1) * 256].rearrange("(c p) n -> p c n", p=128),
                                      writes=[sk])
                                if use_cache:
                                    P.dma("sp", job["cache"][wi][ng], sl[:].rearrange("p c n -> p (c n)"),
                                          reads=[sk], writes=[ck])
                            else:
                                P.dma("pool", sl[:].rearrange("p c n -> p (c n)"), job["cache"][wi][ng],
                                      reads=[ck], writes=[sk])
                            sls.append((sl, sk))
                        for j in range(2):
                            nch = ng * 2 + j
                            pss = []
                            for (sl, sk) in sls:
                                pt = ps[ci % 4]
                                pk = "ps%d" % (ci % 4)
                                ci += 1
                                for kc in range(kcn):
                                    P.op("pe", lambda e, pt=pt, sl=sl, kc=kc, j=j: e.matmul(
                                        pt[:], sl[:, kc, j * 128:(j + 1) * 128], inb[:, kc, :],
                                        start=(kc == 0), stop=(kc == kcn - 1)), reads=[sk, "inb"], writes=[pk])
                                pss.append((pt, pk))
                            b = ci % 2
                            if epi == "bf16":
                                o = ost[ng % 2]
                                ok = "ost%d_%d" % (ng % 2, j)
                                P.op("act", lambda e, o=o, j=j, pt=pss[0][0]: e.copy(out=o[:, j, :], in_=pt[:]),
                                     reads=[pss[0][1]], writes=[ok])
                            elif epi == "gelu":
                                o = ost[ng % 2]
                                ok = "ost%d_%d" % (ng % 2, j)
                                self.gelu_ops("l", pss[0][0][:], [pss[0][1]], o[:, j, :], ok,
                                              t1[b][:], "t1%d" % b, t2[b][:], "t2%d" % b)
                            else:
                                m = job["m"]
                                rows = slice(nch * 128, (nch + 1) * 128)
                                P.dma("sp", xo[b][:], job["xsrc"][rows, ts], writes=["xo%d" % b])
                                if epi == "resid":
                                    val, vk = pss[0][0][:], pss[0][1]
                                else:
                                    P.op("act", lambda e, b=b, pt=pss[1][0]: e.activation(
                                        out=t1[b][:], in_=pt[:], func=AF.Sigmoid), reads=[pss[1][1]],
                                        writes=["t1%d" % b])
                                    P.op("dve", lambda e, b=b, pt=pss[0][0]: e.tensor_tensor(
                                        out=t2[b][:], in0=pt[:], in1=t1[b][:], op=ALU.mult),
                                        reads=[pss[0][1], "t1%d" % b], writes=["t2%d" % b])
                                    val, vk = t2[b][:], "t2%d" % b
                                P.op("dve", lambda e, b=b, val=val, m=m, nch=nch: e.scalar_tensor_tensor(
                                    out=xn[b][:], in0=val, scalar=self.gate(m, nch), in1=xo[b][:],
                                    op0=ALU.mult, op1=ALU.add), reads=[vk, "xo%d" % b], writes=["xn%d" % b])
                                P.dma("sp", job["xdst"][rows, ts], xn[b][:], reads=["xn%d" % b])
                        if epi in ("bf16", "gelu"):
                            o = ost[ng % 2]
                            P.dma("sp", job["out"][ng * 256:(ng + 1) * 256, ts].rearrange("(j p) t -> p j t", p=128),
                                  o[:], reads=["ost%d_0" % (ng % 2), "ost%d_1" % (ng % 2)])
            P.emit()

    def stage_linear_tm(self, inT, w, N, out, out_dtype, gelu=False):
        nc, P = self.nc, self.P
        with ExitStack() as st:
            T = self.mkT(st)
            inb = T("m_in", [128, KC, TB], BF16)
            slab = [T("m_slab%d" % i, [128, KC, 512], BF16) for i in range(2)]
            ost = [T("m_ost%d" % i, [128, 512], out_dtype) for i in range(2)]
            t1 = [T("m_t1%d" % i, [128, 512], F32) for i in range(2)]
            t2 = [T("m_t2%d" % i, [128, 512], F32) for i in range(2)]
            ps = [self.PS(st, "m_ps%d" % i, [128, 512], F32) for i in range(2)]
            si = 0
            ci = 0
            use_cache = self.NTB > 1
            if use_cache:
                cache = self.scratch(self.uid("mc"), [N // 512, 128, KC * 512], BF16)
            for tb in range(self.NTB):
                ts = slice(tb * TB, (tb + 1) * TB)
                P.dma("sp", inb[:], inT[:, ts].rearrange("(c p) t -> p c t", p=128), writes=["inb"])
                for ns in range(N // 512):
                    sl = slab[si % 2]
                    sk = "slab%d" % (si % 2)
                    si += 1
                    if tb == 0 or not use_cache:
                        P.dma("pool", sl[:], w[:, ns * 512:(ns + 1) * 512].rearrange("(c p) n -> p c n", p=128),
                              writes=[sk])
                        if use_cache:
                            P.dma("sp", cache[ns], sl[:].rearrange("p c n -> p (c n)"), reads=[sk],
                                  writes=["c%d" % ns])
                    else:
                        P.dma("pool", sl[:].rearrange("p c n -> p (c n)"), cache[ns], reads=["c%d" % ns],
                              writes=[sk])
                    for tq in range(TB // 128):
                        b = ci % 2
                        ci += 1
                        pt = ps[b]
                        pk = "ps%d" % b
                        for kc in range(KC):
                            P.op("pe", lambda e, pt=pt, sl=sl, kc=kc, tq=tq: e.matmul(
                                pt[:], inb[:, kc, tq * 128:(tq + 1) * 128], sl[:, kc, :],
                                start=(kc == 0), stop=(kc == KC - 1)), reads=[sk, "inb"], writes=[pk])
                        if gelu:
                            self.gelu_ops("m", pt[:], [pk], ost[b][:], "ost%d" % b,
                                          t1[b][:], "t1%d" % b, t2[b][:], "t2%d" % b)
                        else:
                            P.op("act", lambda e, b=b, pt=pt: e.copy(out=ost[b][:], in_=pt[:]),
                                 reads=[pk], writes=["ost%d" % b])
                        r0 = tb * TB + tq * 128
                        P.dma("sp", out[r0:r0 + 128, ns * 512:(ns + 1) * 512], ost[b][:], reads=["ost%d" % b])
            P.emit()

    def stage_ffn(self, hT, experts, F, m, xsrc, xdst, gatesT=None):
        nc, P = self.nc, self.P
        FG = 256
        with ExitStack() as st:
            T = self.mkT(st)
            hb = T("f_h", [128, KC, TB], BF16)
            acc = T("f_acc", [128, KC, TB], F32)
            wgs = [T("f_wg%d" % i, [128, KC, FG], BF16) for i in range(2)]
            wus = [T("f_wu%d" % i, [128, KC, FG], BF16) for i in range(2)]
            wds = [T("f_wd%d" % i, [128, FG // 128, D], BF16) for i in range(2)]
            act_t = [T("f_act%d" % i, [128, FG // 128, TB], BF16) for i in range(2)]
            sg = [T("f_sg%d" % i, [128, TB], F32) for i in range(2)]
            xo = sg
            if gatesT is not None:
                grow = T("f_grow", [1, TB], F32)
                gbc = [T("f_gbc%d" % i, [128, TB], F32) for i in range(1)] * 2
            psg = [self.PS(st, "f_pg%d" % i, [128, TB], F32) for i in range(2)]
            psu = [self.PS(st, "f_pu%d" % i, [128, TB], F32) for i in range(2)]
            psd = [self.PS(st, "f_pd%d" % i, [128, TB], F32) for i in range(3)]
            gi = 0
            ci = 0
            di = 0
            use_cache = self.NTB > 1
            if use_cache:
                sid = self.uid("fc")
                NG = F // FG
                cgs = [self.scratch("%s_g%d" % (sid, i), [NG, 128, KC * FG], BF16) for i in range(len(experts))]
                cus = [self.scratch("%s_u%d" % (sid, i), [NG, 128, KC * FG], BF16) for i in range(len(experts))]
                cds = [self.scratch("%s_d%d" % (sid, i), [NG, 128, (FG // 128) * D], BF16)
                       for i in range(len(experts))]
            for tb in range(self.NTB):
                ts = slice(tb * TB, (tb + 1) * TB)
                P.dma("sp", hb[:], hT[:, ts].rearrange("(c p) t -> p c t", p=128), writes=["hb"])
                first = True
                pend = []
                for ex, (wg, wu, wd) in enumerate(experts):
                    if gatesT is not None:
                        pt = psd[di % 3]
                        pk = "pd%d" % (di % 3)
                        di += 1
                        P.dma("sp", grow[:], gatesT[ex:ex + 1, ts], writes=["grow"])
                        P.op("pe", lambda e, pt=pt: e.matmul(pt[:], self.ones_f[0:1, :], grow[0:1, :],
                                                             start=True, stop=True),
                             reads=["grow"], writes=[pk])
                        P.op("act", lambda e, pt=pt, ex=ex: e.copy(out=gbc[0][:], in_=pt[:]),
                             reads=[pk], writes=["gbc0"])
                    for fg in range(F // FG):
                        b = gi % 2
                        gi += 1
                        fs = slice(fg * FG, (fg + 1) * FG)
                        if tb == 0 or not use_cache:
                            P.dma("pool", wgs[b][:], wg[:, fs].rearrange("(c p) n -> p c n", p=128),
                                  writes=["wg%d" % b])
                            P.dma("pool", wus[b][:], wu[:, fs].rearrange("(c p) n -> p c n", p=128),
                                  writes=["wu%d" % b])
                            P.dma("pool", wds[b][:], wd[fs, :].rearrange("(j p) n -> p j n", p=128), writes=["wd%d" % b])
                            if use_cache:
                                P.dma("sp", cgs[ex][fg], wgs[b][:].rearrange("p c n -> p (c n)"),
                                      reads=["wg%d" % b], writes=["cg%d_%d" % (ex, fg)])
                                P.dma("sp", cus[ex][fg], wus[b][:].rearrange("p c n -> p (c n)"),
                                      reads=["wu%d" % b], writes=["cu%d_%d" % (ex, fg)])
                                P.dma("sp", cds[ex][fg], wds[b][:].rearrange("p j n -> p (j n)"),
                                      reads=["wd%d" % b], writes=["cd%d_%d" % (ex, fg)])
                        else:
                            P.dma("pool", wgs[b][:].rearrange("p c n -> p (c n)"), cgs[ex][fg],
                                  reads=["cg%d_%d" % (ex, fg)], writes=["wg%d" % b])
                            P.dma("pool", wus[b][:].rearrange("p c n -> p (c n)"), cus[ex][fg],
                                  reads=["cu%d_%d" % (ex, fg)], writes=["wu%d" % b])
                            P.dma("pool", wds[b][:].rearrange("p j n -> p (j n)"), cds[ex][fg],
                                  reads=["cd%d_%d" % (ex, fg)], writes=["wd%d" % b])
                        for j in range(FG // 128):
                            cb = ci % 2
                            ci += 1
                            for kc in range(KC):
                                P.op("pe", lambda e, cb=cb, b=b, kc=kc, j=j: e.matmul(
                                    psg[cb][:], wgs[b][:, kc, j * 128:(j + 1) * 128], hb[:, kc, :],
                                    start=(kc == 0), stop=(kc == KC - 1)), reads=["wg%d" % b, "hb"],
                                    writes=["pg%d" % cb])
                            for kc in range(KC):
                                P.op("pe", lambda e, cb=cb, b=b, kc=kc, j=j: e.matmul(
                                    psu[cb][:], wus[b][:, kc, j * 128:(j + 1) * 128], hb[:, kc, :],
                                    start=(kc == 0), stop=(kc == KC - 1)), reads=["wu%d" % b, "hb"],
                                    writes=["pu%d" % cb])
                            P.op("act", lambda e, cb=cb: e.activation(out=sg[cb][:], in_=psg[cb][:], func=AF.Silu),
                                 reads=["pg%d" % cb], writes=["sg%d" % cb])
                            if gatesT is not None:
                                P.op("dve", lambda e, cb=cb, ex=ex: e.tensor_tensor(
                                    out=sg[cb][:], in0=sg[cb][:], in1=gbc[ex % 2][:], op=ALU.mult),
                                    reads=["sg%d" % cb, "gbc0"], writes=["sg%d" % cb])
                            P.op("dve", lambda e, cb=cb, b=b, j=j: e.tensor_tensor(
                                out=act_t[b][:, j, :], in0=psu[cb][:], in1=sg[cb][:], op=ALU.mult),
                                reads=["pu%d" % cb, "sg%d" % cb], writes=["act%d_%d" % (b, j)])
                        pend.append(b)
                        last_group = (ex == len(experts) - 1) and (fg == F // FG - 1)
                        if len(pend) < 2 and not last_group:
                            continue
                        items = [(pb, j) for pb in pend for j in range(FG // 128)]
                        pend = []
                        for nch in range(KC):
                            pt = psd[di % 3]
                            pk = "pd%d" % (di % 3)
                            di += 1
                            for ii, (pb, j) in enumerate(items):
                                P.op("pe", lambda e, pt=pt, pb=pb, j=j, nch=nch, ii=ii, n_=len(items): e.matmul(
                                    pt[:], wds[pb][:, j, nch * 128:(nch + 1) * 128], act_t[pb][:, j, :],
                                    start=(ii == 0), stop=(ii == n_ - 1)),
                                    reads=["wd%d" % pb, "act%d_%d" % (pb, j)], writes=[pk])
                            eng = "dve" if nch % 2 == 0 else "act"
                            if first:
                                if eng == "dve":
                                    P.op("dve", lambda e, pt=pt, nch=nch: e.tensor_copy(out=acc[:, nch, :], in_=pt[:]),
                                         reads=[pk], writes=["acc%d" % nch])
                                else:
                                    P.op("act", lambda e, pt=pt, nch=nch: e.copy(out=acc[:, nch, :], in_=pt[:]),
                                         reads=[pk], writes=["acc%d" % nch])
                            else:
                                P.op("dve", lambda e, pt=pt, nch=nch: e.tensor_tensor(
                                    out=acc[:, nch, :], in0=pt[:], in1=acc[:, nch, :], op=ALU.add),
                                    reads=[pk, "acc%d" % nch], writes=["acc%d" % nch])
                        first = False
                for nch in range(KC):
                    b = nch % 2
                    rows = slice(nch * 128, (nch + 1) * 128)
                    P.dma("sp", xo[b][:], xsrc[rows, ts], writes=["sg%d" % b])
                    P.op("dve", lambda e, b=b, nch=nch: e.scalar_tensor_tensor(
                        out=xo[b][:], in0=acc[:, nch, :], scalar=self.gate(m, nch), in1=xo[b][:],
                        op0=ALU.mult, op1=ALU.add), reads=["acc%d" % nch, "sg%d" % b], writes=["sg%d" % b])
                    P.dma("sp", xdst[rows, ts], xo[b][:], reads=["sg%d" % b])
            P.emit()

    def stage_attn(self, qT, kT, v, outT, n_heads=16):
        nc, P = self.nc, self.P
        S = self.S
        NB = S // 128
        sc = 1.0 / math.sqrt(128.0)
        CH = 1024
        with ExitStack() as st:
            T = self.mkT(st)
            qh = [T("a_q%d" % i, [128, S], BF16) for i in range(2)]
            kh = [T("a_k%d" % i, [128, S], BF16) for i in range(2)]
            vh = [T("a_v%d" % i, [128, NB, 128], BF16) for i in range(2)]
            oh = [T("a_o%d" % i, [128, S], BF16) for i in range(1)] * 2
            SP = [T("a_sp%d" % i, [128, S], F32) for i in range(2)]
            U = [T("a_u%d" % i, [128, S], F32) for i in range(2)]
            CS = [T("a_cs%d" % i, [128, S], F32) for i in range(2)]
            W = [T("a_w%d" % i, [128, S], BF16) for i in range(2)]
            WT = [T("a_wt%d" % i, [128, S], BF16) for i in range(2)]
            ones = T("a_ones", [128, S], F32)
            ntot = [T("a_nt%d" % i, [128, 1], F32) for i in range(2)]
            m01 = T("a_m01", [128, 128], F32)
            m01b = T("a_m01b", [128, 128], BF16)
            psS = [self.PS(st, "a_ps%d" % i, [128, CH], F32) for i in range(2)]
            psT = [self.PS(st, "a_pt%d" % i, [128, 1024], BF16) for i in range(2)]
            psO = [self.PS(st, "a_po%d" % i, [128, 512], F32) for i in range(2)]
            P.op("pool", lambda e: e.memset(ones[:], 1.0), writes=["ones"])
            P.op("pool", lambda e: e.affine_select(
                out=m01[:], in_=ones[:, 0:128], pattern=[[-1, 128]], compare_op=ALU.is_ge, fill=0.0,
                base=-1, channel_multiplier=1), reads=["ones"], writes=["m01"])
            P.op("pool", lambda e: e.tensor_copy(out=m01b[:], in_=m01[:]), reads=["m01"], writes=["m01b"])
            it = 0
            sci = 0
            tci = 0
            for h in range(n_heads):
                hb = h % 2
                rows = slice(h * 128, (h + 1) * 128)
                P.dma("sp", qh[hb][:], qT[rows, :], writes=["q%d" % hb])
                P.dma("sp", kh[hb][:], kT[rows, :], writes=["k%d" % hb])
                P.dma("sp", vh[hb][:], v[:, rows].rearrange("(kb p) d -> p kb d", p=128), writes=["v%d" % hb])
                for qb in range(NB):
                    b = it % 2
                    it += 1
                    nk = (qb + 1) * 128
                    dg = slice(qb * 128, nk)
                    for c0 in range(0, nk, CH):
                        w_ = min(CH, nk - c0)
                        sb_ = sci % 2
                        sci += 1
                        pS = psS[sb_]
                        sk = "pS%d" % sb_
                        for o_ in range(0, w_, 512):
                            ww = min(512, w_ - o_)
                            P.op("pe", lambda e, pS=pS, o_=o_, ww=ww, c0=c0, hb=hb, qb=qb: e.matmul(
                                pS[:, o_:o_ + ww], qh[hb][:, qb * 128:(qb + 1) * 128],
                                kh[hb][:, c0 + o_:c0 + o_ + ww], start=True, stop=True),
                                reads=["q%d" % hb, "k%d" % hb], writes=[sk])
                        cs_ = slice(c0, c0 + w_)
                        P.op("act", lambda e, pS=pS, w_=w_, cs_=cs_, b=b: e.activation(
                            out=SP[b][:, cs_], in_=pS[:, 0:w_], func=AF.Exp, scale=sc),
                            reads=[sk], writes=["SP%d" % b])
                        P.op("act", lambda e, cs_=cs_, b=b: e.activation(
                            out=SP[b][:, cs_], in_=SP[b][:, cs_], func=AF.Ln, bias=1.0),
                            reads=["SP%d" % b], writes=["SP%d" % b])
                        P.op("dve", lambda e, pS=pS, w_=w_, cs_=cs_, b=b: e.scalar_tensor_tensor(
                            out=U[b][:, cs_], in0=pS[:, 0:w_], scalar=sc, in1=SP[b][:, cs_],
                            op0=ALU.mult, op1=ALU.subtract), reads=[sk, "SP%d" % b], writes=["U%d" % b])
                    P.op("dve", lambda e, b=b, dg=dg: e.tensor_tensor(
                        out=SP[b][:, dg], in0=SP[b][:, dg], in1=m01[:], op=ALU.mult),
                        reads=["SP%d" % b, "m01"], writes=["SP%d" % b])
                    P.op("dve", lambda e, b=b, nk=nk: e.tensor_tensor_scan(
                        out=CS[b][:, 0:nk], data0=ones[:, 0:nk], data1=SP[b][:, 0:nk], initial=0.0,
                        op0=ALU.mult, op1=ALU.add), reads=["SP%d" % b, "ones"], writes=["CS%d" % b])
                    P.op("dve", lambda e, b=b, nk=nk: e.tensor_tensor(
                        out=U[b][:, 0:nk], in0=U[b][:, 0:nk], in1=CS[b][:, 0:nk], op=ALU.add),
                        reads=["U%d" % b, "CS%d" % b], writes=["U%d" % b])
                    P.op("dve", lambda e, b=b, nk=nk: e.tensor_scalar(
                        out=ntot[b][:], in0=CS[b][:, nk - 1:nk], scalar1=-1.0, scalar2=None, op0=ALU.mult),
                        reads=["CS%d" % b], writes=["nt%d" % b])
                    P.op("act", lambda e, b=b, nk=nk: e.activation(
                        out=W[b][:, 0:nk], in_=U[b][:, 0:nk], func=AF.Exp, bias=ntot[b][:, 0:1]),
                        reads=["U%d" % b, "nt%d" % b], writes=["W%d" % b])
                    P.op("dve", lambda e, b=b, dg=dg: e.tensor_tensor(
                        out=W[b][:, dg], in0=W[b][:, dg], in1=m01b[:], op=ALU.mult),
                        reads=["W%d" % b, "m01b"], writes=["W%d" % b])
                    for k0 in range(0, qb + 1, 8):
                        k1 = min(qb + 1, k0 + 8)
                        tb_ = tci % 2
                        tci += 1
                        for kb in range(k0, k1):
                            P.op("pe", lambda e, tb_=tb_, kb=kb, k0=k0, b=b: e.transpose(
                                psT[tb_][:, (kb - k0) * 128:(kb - k0 + 1) * 128], W[b][:, kb * 128:(kb + 1) * 128],
                                self.ident_bf[:]), reads=["W%d" % b], writes=["pT%d" % tb_])
                        eng = "act" if (tci % 2 == 0) else "dve"
                        if eng == "act":
                            P.op("act", lambda e, tb_=tb_, k0=k0, k1=k1, b=b: e.copy(
                                out=WT[b][:, k0 * 128:k1 * 128], in_=psT[tb_][:, 0:(k1 - k0) * 128]),
                                reads=["pT%d" % tb_], writes=["WT%d_%d" % (b, k0)])
                        else:
                            P.op("dve", lambda e, tb_=tb_, k0=k0, k1=k1, b=b: e.tensor_copy(
                                out=WT[b][:, k0 * 128:k1 * 128], in_=psT[tb_][:, 0:(k1 - k0) * 128]),
                                reads=["pT%d" % tb_], writes=["WT%d_%d" % (b, k0)])
                    for kb in range(qb + 1):
                        P.op("pe", lambda e, b=b, kb=kb, hb=hb, qb=qb: e.matmul(
                            psO[b][:, 0:128], vh[hb][:, kb, :], WT[b][:, kb * 128:(kb + 1) * 128],
                            start=(kb == 0), stop=(kb == qb)),
                            reads=["v%d" % hb, "WT%d_%d" % (b, (kb // 8) * 8)], writes=["pO%d" % b])
                    P.op("act", lambda e, b=b, hb=hb, qb=qb: e.copy(
                        out=oh[hb][:, qb * 128:(qb + 1) * 128], in_=psO[b][:, 0:128]),
                        reads=["pO%d" % b], writes=["o0"])
                P.dma("sp", outT[rows, :], oh[hb][:], reads=["o0"])
            P.emit()

    def stage_gmlp(self, uT, z2g, ln_g, w_s, b_s, outT, n_groups=16):
        nc, P = self.nc, self.P
        S = self.S
        G = n_groups
        with ExitStack() as st:
            T = self.mkT(st)
            lng = T("g_lng", [128, G * 128], F32)
            brow = T("g_brow", [1, G * 128], F32)
            wst = [T("g_ws%d" % i, [128, 128], F32) for i in range(2)]
            wmT = T("g_wmT", [128, G, 128], BF16)
            z2c = [T("g_z2%d" % i, [128, G, 128], F32) for i in range(2)]
            uc = [T("g_u%d" % i, [128, G, 128], BF16) for i in range(2)]
            stats = T("g_stats", [128, G, 6], F32)
            mv = T("g_mv", [128, G, 2], F32)
            rstd = T("g_rstd", [128, G], F32)
            vt = T("g_vt", [128, G, 128], F32)
            vn = [T("g_vn%d" % i, [128, G, 128], BF16) for i in range(2)]
            og = [T("g_og%d" % i, [128, G, 128], BF16) for i in range(2)]
            pw = self.PS(st, "g_pw", [128, 512], F32)
            pm = [self.PS(st, "g_pm%d" % i, [128, 512], F32) for i in range(4)]
            P.dma("sp", lng[:], ln_g.rearrange("g c -> (g c)").partition_broadcast(128), writes=["lng"])
            P.dma("sp", brow[:], b_s.rearrange("g t -> (g t)").partition_broadcast(1), writes=["brow"])
            for g in range(G):
                w = wst[g % 2]
                wk = "ws%d" % (g % 2)
                P.dma("sp", w[:], w_s[g, :, :], writes=[wk])
                P.op("pool", lambda e, w=w: e.affine_select(
                    out=w[:], in_=w[:], pattern=[[-1, 128]], compare_op=ALU.is_ge, fill=0.0, base=0,
                    channel_multiplier=1), reads=[wk], writes=[wk])
                P.op("pe", lambda e, w=w: e.transpose(pw[:, 0:128], w[:], self.ident_f[:]),
                     reads=[wk], writes=["pw"])
                P.op("act", lambda e, g=g: e.copy(out=wmT[:, g, :], in_=pw[:, 0:128]), reads=["pw"],
                     writes=["wmT"])
            pi = 0
            for n in range(S // 128):
                b = n % 2
                ts = slice(n * 128, (n + 1) * 128)
                P.dma("sp", z2c[b][:], z2g[ts, :].rearrange("t (g c) -> t g c", g=G), writes=["z2%d" % b])
                P.dma("sp", uc[b][:], uT[:, ts].rearrange("(g c) t -> c g t", g=G), writes=["u%d" % b])
                for g in range(G):
                    P.op("dve", lambda e, b=b, g=g: e.bn_stats(out=stats[:, g, :], in_=z2c[b][:, g, :]),
                         reads=["z2%d" % b], writes=["st%d" % g])
                    P.op("dve", lambda e, g=g: e.bn_aggr(out=mv[:, g, :], in_=stats[:, g, :]),
                         reads=["st%d" % g], writes=["mv%d" % g])
                mvk = ["mv%d" % g for g in range(G)]
                P.op("dve", lambda e: e.tensor_scalar(out=rstd[:], in0=mv[:, :, 1], scalar1=EPS, scalar2=None,
                                                      op0=ALU.add), reads=mvk, writes=["rstd"])
                P.op("act", lambda e: e.activation(out=rstd[:], in_=rstd[:], func=AF.Sqrt),
                     reads=["rstd"], writes=["rstd"])
                P.op("dve", lambda e: e.reciprocal(out=rstd[:], in_=rstd[:]), reads=["rstd"], writes=["rstd"])
                for g in range(G):
                    P.op("dve", lambda e, b=b, g=g: e.tensor_scalar(
                        out=vt[:, g, :], in0=z2c[b][:, g, :], scalar1=mv[:, g, 0:1], scalar2=rstd[:, g:g + 1],
                        op0=ALU.subtract, op1=ALU.mult), reads=["z2%d" % b, "mv%d" % g, "rstd"], writes=["vt"])
                P.op("dve", lambda e, b=b: e.tensor_tensor(
                    out=vn[b][:].rearrange("p g c -> p (g c)"), in0=vt[:].rearrange("p g c -> p (g c)"),
                    in1=lng[:], op=ALU.mult), reads=["vt", "lng"], writes=["vn%d" % b])
                for g4 in range(G // 4):
                    pb = pi % 4
                    pi += 1
                    for gg in range(4):
                        g = g4 * 4 + gg
                        P.op("pe", lambda e, pb=pb, gg=gg, g=g, b=b: e.matmul(
                            pm[pb][:, gg * 128:(gg + 1) * 128], vn[b][:, g, :], wmT[:, g, :],
                            start=True, stop=False), reads=["vn%d" % b, "wmT"], writes=["pm%d" % pb])
                        P.op("pe", lambda e, pb=pb, gg=gg, g=g: e.matmul(
                            pm[pb][:, gg * 128:(gg + 1) * 128], self.ones_f[0:1, :], brow[0:1, g * 128:(g + 1) * 128],
                            start=False, stop=True), reads=["brow"], writes=["pm%d" % pb])
                    P.op("dve", lambda e, pb=pb, g4=g4, b=b: e.tensor_tensor(
                        out=og[b][:, g4 * 4:(g4 + 1) * 4, :].rearrange("p g c -> p (g c)"), in0=pm[pb][:],
                        in1=uc[b][:, g4 * 4:(g4 + 1) * 4, :].rearrange("p g c -> p (g c)"), op=ALU.mult),
                        reads=["pm%d" % pb, "u%d" % b], writes=["og%d_%d" % (b, g4)])
                P.dma("sp", outT[:, ts].rearrange("(g c) t -> c g t", g=G), og[b][:],
                      reads=["og%d_%d" % (b, g4) for g4 in range(G // 4)])
            P.emit()

    def stage_ssm_prep(self, lam_re, lam_im, log_dt, b_re, b_im, c_re, c_im, MB, MP, NL, G=256):
        nc, P = self.nc, self.P
        GB = 32
        PI2 = math.pi / 2
        with ExitStack() as st:
            T = self.mkT(st)
            NS = 96
            PW = T("sp_pw", [128, NS, G], F32)
            cnt = [0]

            def new():
                i = cnt[0]
                cnt[0] += 1
                assert i < NS
                return (PW[:, i, :], "pw%d" % i)

            def TT(o, a, b, op, eng="dve"):
                P.op(eng, lambda e: e.tensor_tensor(out=o[0], in0=a[0], in1=b[0], op=op),
                     reads=[a[1], b[1]], writes=[o[1]])

            def TS(o, a, s1, op0, s2=None, op1=None):
                if op1 is None:
                    P.op("dve", lambda e: e.tensor_scalar(out=o[0], in0=a[0], scalar1=s1, scalar2=None, op0=op0),
                         reads=[a[1]], writes=[o[1]])
                else:
                    P.op("dve", lambda e: e.tensor_scalar(out=o[0], in0=a[0], scalar1=s1, scalar2=s2, op0=op0,
                                                          op1=op1), reads=[a[1]], writes=[o[1]])

            def STT(o, a, s, b, op0, op1):
                P.op("dve", lambda e: e.scalar_tensor_tensor(out=o[0], in0=a[0], scalar=s, in1=b[0], op0=op0,
                                                             op1=op1), reads=[a[1], b[1]], writes=[o[1]])

            def ACTF(o, a, func, scale=1.0, bias=0.0):
                P.op("act", lambda e: e.activation(out=o[0], in_=a[0], func=func, scale=scale, bias=bias),
                     reads=[a[1]], writes=[o[1]])

            t1, t2 = new(), new()

            def cmul(dr, di, ar, ai, br, bi):
                TT(t1, ar, br, ALU.mult)
                TT(t2, ai, bi, ALU.mult)
                TT(dr, t1, t2, ALU.subtract)
                TT(t1, ar, bi, ALU.mult)
                TT(t2, ai, br, ALU.mult)
                TT(di, t1, t2, ALU.add)

            def csq(dr, di, ar, ai):
                TT(t1, ar, ar, ALU.mult)
                TT(t2, ai, ai, ALU.mult)
                STT(di, ar, 2.0, ai, ALU.mult, ALU.mult)
                TT(dr, t1, t2, ALU.subtract)

            nat = T("sp_nat", [128, 128], F32)
            pst = self.PS(st, "sp_pst", [128, 512], F32)
            psA = [self.PS(st, "sp_psA%d" % i, [128, 512], F32) for i in range(2)]
            lrT, liT, ldt = new(), new(), new()
            for arr, dst in ((lam_re, lrT), (lam_im, liT)):
                for gt in range(G // 128):
                    P.dma("sp", nat[:, 0:64], arr[gt * 128:(gt + 1) * 128, :], writes=["nat"])
                    P.dma("sp", nat[:, 64:128], arr[gt * 128:(gt + 1) * 128, :], writes=["nat"])
                    P.op("pe", lambda e: e.transpose(pst[:, 0:128], nat[:], self.ident_f[:]), reads=["nat"],
                         writes=["pst"])
                    P.op("act", lambda e, dst=dst, gt=gt: e.copy(out=dst[0][:, gt * 128:(gt + 1) * 128],
                                                                 in_=pst[:, 0:128]), reads=["pst"], writes=[dst[1]])
            P.dma("sp", ldt[0], log_dt.rearrange("o g -> (o g)").partition_broadcast(128), writes=[ldt[1]])
            dt, lr, x1, th = new(), new(), new(), new()
            ACTF(dt, ldt, AF.Exp)
            TS(lr, lrT, -1e-4, ALU.min)
            TT(x1, lr, dt, ALU.mult)
            TT(th, liT, dt, ALU.mult)
            mag, sn, cs = new(), new(), new()
            ACTF(mag, x1, AF.Exp, scale=1.0 / 32)
            ACTF(sn, th, AF.Sin, scale=1.0 / 32)
            ACTF(cs, th, AF.Sin, scale=1.0 / 32, bias=PI2)
            cur = (new(), new())
            TT(cur[0], mag, cs, ALU.mult)
            TT(cur[1], mag, sn, ALU.mult)
            for _ in range(5):
                nx = (new(), new())
                csq(nx[0], nx[1], cur[0], cur[1])
                cur = nx
            pw = {1: cur}
            for i in range(2, 9):
                pw[i] = (new(), new())
            csq(*pw[2], *pw[1])
            cmul(*pw[3], *pw[2], *pw[1])
            csq(*pw[4], *pw[2])
            cmul(*pw[5], *pw[4], *pw[1])
            csq(*pw[6], *pw[3])
            cmul(*pw[7], *pw[6], *pw[1])
            csq(*pw[8], *pw[4])
            big = [pw[8]]
            for k in range(1, NL):
                nx = (new(), new())
                csq(nx[0], nx[1], big[-1][0], big[-1][1])
                big.append(nx)
            ipw = {}
            n2 = new()
            for s in range(1, 8):
                ipw[s] = (new(), new())
                TT(t1, pw[s][0], pw[s][0], ALU.mult)
                TT(t2, pw[s][1], pw[s][1], ALU.mult)
                TT(n2, t1, t2, ALU.add)
                P.op("dve", lambda e: e.reciprocal(out=n2[0], in_=n2[0]), reads=[n2[1]], writes=[n2[1]])
                TT(ipw[s][0], pw[s][0], n2, ALU.mult)
                STT(ipw[s][1], pw[s][1], -1.0, n2, ALU.mult, ALU.mult)
            nr, den, fre, fim = new(), new(), new(), new()
            are, aim = pw[1]
            TS(nr, are, -1.0, ALU.add)
            TT(t1, lr, lr, ALU.mult)
            TT(t2, liT, liT, ALU.mult)
            TT(den, t1, t2, ALU.add)
            P.op("dve", lambda e: e.reciprocal(out=den[0], in_=den[0]), reads=[den[1]], writes=[den[1]])
            TT(t1, nr, lr, ALU.mult)
            TT(t2, aim, liT, ALU.mult)
            TT(t1, t1, t2, ALU.add)
            TT(fre, t1, den, ALU.mult)
            TT(t1, aim, lr, ALU.mult)
            TT(t2, nr, liT, ALU.mult)
            TT(t1, t1, t2, ALU.subtract)
            TT(fim, t1, den, ALU.mult)
            zs = [pw[7]] + big
            sims = []
            for z in zs:
                sm = new()
                P.op("dve", lambda e, sm=sm, z=z: e.tensor_copy(out=sm[0][0:64, :], in_=z[1][0][0:64, :]),
                     reads=[z[1][1]], writes=[sm[1]])
                P.op("dve", lambda e, sm=sm, z=z: e.tensor_scalar(out=sm[0][64:128, :], in0=z[1][0][64:128, :],
                                                                  scalar1=-1.0, scalar2=None, op0=ALU.mult),
                     reads=[z[1][1]], writes=[sm[1]])
                sims.append(sm)
            jsw = T("sp_jsw", [128, 128], F32)
            P.op("pool", lambda e: e.memset(jsw[:], 0.0), writes=["jsw"])
            P.op("pool", lambda e: e.tensor_copy(out=jsw[0:64, 64:128], in_=self.ident_f[0:64, 0:64]),
                 reads=["jsw"], writes=["jsw"])
            P.op("pool", lambda e: e.tensor_copy(out=jsw[64:128, 0:64], in_=self.ident_f[64:128, 64:128]),
                 reads=["jsw"], writes=["jsw"])
            mT0 = T("sp_mT0", [128, 8, 16], F32)
            P.op("pool", lambda e: e.memset(mT0[:], 1.0), writes=["mT0"])
            P.op("pool", lambda e: e.affine_select(
                out=mT0[:], in_=mT0[:], pattern=[[16, 8], [0, 16]], compare_op=ALU.is_ge, fill=0.0, base=15,
                channel_multiplier=-1), reads=["mT0"], writes=["mT0"])

            Bre = T("sp_Bre", [128, GB, 16], F32)
            Bim = T("sp_Bim", [128, GB, 16], F32)
            Wre = T("sp_Wre", [128, GB, 16], F32)
            Wim = T("sp_Wim", [128, GB, 16], F32)
            CTre = T("sp_CTre", [128, GB, 16], F32)
            CTim = T("sp_CTim", [128, GB, 16], F32)
            u1 = T("sp_u1", [128, GB, 16], F32)
            u2 = T("sp_u2", [128, GB, 16], F32)
            Xs = T("sp_Xs", [128, GB, 8, 16], F32)
            Ys = T("sp_Ys", [128, GB, 9, 16], F32)
            natc = T("sp_natc", [128, 128], F32)
            Mt = [T("sp_Mt%d" % i, [128, 128], F32) for i in range(2)]
            stB = [T("sp_stB%d" % i, [128, 3, 128], BF16) for i in range(2)]
            stP = [T("sp_stP%d" % i, [128, NL, 128], F32) for i in range(2)]
            c_re2 = c_re.rearrange("g c p -> (g c) p")
            c_im2 = c_im.rearrange("g c p -> (g c) p")

            def bc(slot, g0):
                return slot[0][:, g0:g0 + GB].unsqueeze(2).broadcast_to([128, GB, 16])

            def hop(eng, fn, reads, writes):
                P.op(eng, fn, reads=reads, writes=writes)

            for gb in range(G // GB):
                g0 = gb * GB
                for half in range(2):
                    hs = slice(half * 64, (half + 1) * 64)
                    P.dma("sp", Bre[hs], b_re[g0:g0 + GB].rearrange("g p c -> p g c"), writes=["Bre"])
                    P.dma("sp", Bim[hs], b_im[g0:g0 + GB].rearrange("g p c -> p g c"), writes=["Bim"])
                fr, fi = bc(fre, g0), bc(fim, g0)
                hop("dve", lambda e, fr=fr: e.tensor_tensor(out=u1[:], in0=Bre[:], in1=fr, op=ALU.mult),
                    ["Bre", fre[1]], ["u1"])
                hop("dve", lambda e, fi=fi: e.tensor_tensor(out=u2[:], in0=Bim[:], in1=fi, op=ALU.mult),
                    ["Bim", fim[1]], ["u2"])
                hop("dve", lambda e: e.tensor_tensor(out=Wre[:], in0=u1[:], in1=u2[:], op=ALU.subtract),
                    ["u1", "u2"], ["Wre"])
                hop("dve", lambda e, fr=fr: e.tensor_tensor(out=u1[:], in0=Bim[:], in1=fr, op=ALU.mult),
                    ["Bim", fre[1]], ["u1"])
                hop("dve", lambda e, fi=fi: e.tensor_tensor(out=u2[:], in0=Bre[:], in1=fi, op=ALU.mult),
                    ["Bre", fim[1]], ["u2"])
                hop("dve", lambda e: e.tensor_tensor(out=Wim[:], in0=u1[:], in1=u2[:], op=ALU.add),
                    ["u1", "u2"], ["Wim"])
                top, bot = slice(0, 64), slice(64, 128)
                hop("dve", lambda e: e.tensor_copy(out=Xs[top, :, 0, :], in_=Wre[top]), ["Wre"], ["Xs"])
                hop("dve", lambda e: e.tensor_copy(out=Xs[bot, :, 0, :], in_=Wim[bot]), ["Wim"], ["Xs"])
                for s in range(1, 8):
                    ir, ii = bc(ipw[s][0], g0), bc(ipw[s][1], g0)
                    rk = [ipw[s][0][1], ipw[s][1][1]]
                    hop("dve", lambda e, ir=ir: e.tensor_tensor(out=u1[top], in0=Wre[top], in1=ir[top], op=ALU.mult),
                        ["Wre"] + rk, ["u1"])
                    hop("dve", lambda e, ii=ii: e.tensor_tensor(out=u2[top], in0=Wim[top], in1=ii[top], op=ALU.mult),
                        ["Wim"] + rk, ["u2"])
                    hop("dve", lambda e, s=s: e.tensor_tensor(out=Xs[top, :, s, :], in0=u1[top], in1=u2[top],
                                                              op=ALU.subtract), ["u1", "u2"], ["Xs"])
                    hop("dve", lambda e, ir=ir: e.tensor_tensor(out=u1[bot], in0=Wim[bot], in1=ir[bot], op=ALU.mult),
                        ["Wim"] + rk, ["u1"])
                    hop("dve", lambda e, ii=ii: e.tensor_tensor(out=u2[bot], in0=Wre[bot], in1=ii[bot], op=ALU.mult),
                        ["Wre"] + rk, ["u2"])
                    hop("dve", lambda e, s=s: e.tensor_tensor(out=Xs[bot, :, s, :], in0=u1[bot], in1=u2[bot],
                                                              op=ALU.add), ["u1", "u2"], ["Xs"])
                for arr2, dstT, dk in ((c_re2, CTre, "CTre"), (c_im2, CTim, "CTim")):
                    for i in range(GB * 16 // 128):
                        r0 = g0 * 16 + i * 128
                        P.dma("sp", natc[:, 0:64], arr2[r0:r0 + 128, :], writes=["natc"])
                        P.dma("sp", natc[:, 64:128], arr2[r0:r0 + 128, :], writes=["natc"])
                        P.op("pe", lambda e: e.transpose(pst[:, 128:256], natc[:], self.ident_f[:]),
                             reads=["natc"], writes=["pst2"])
                        P.op("act", lambda e, dstT=dstT, i=i: e.copy(
                            out=dstT[:, i * 8:(i + 1) * 8, :], in_=pst[:, 128:256].rearrange("p (g c) -> p g c", c=16)),
                            reads=["pst2"], writes=[dk])
                hop("dve", lambda e: e.tensor_copy(out=Ys[top, :, 0, :], in_=CTre[top]), ["CTre"], ["Ys"])
                hop("dve", lambda e: e.tensor_scalar(out=Ys[bot, :, 0, :], in0=CTim[bot], scalar1=-1.0, scalar2=None,
                                                     op0=ALU.mult), ["CTim"], ["Ys"])
                for t in range(1, 9):
                    pr, pi_ = bc(pw[t][0], g0), bc(pw[t][1], g0)
                    rk = [pw[t][0][1], pw[t][1][1]]
                    hop("dve", lambda e, pr=pr: e.tensor_tensor(out=u1[top], in0=CTre[top], in1=pr[top], op=ALU.mult),
                        ["CTre"] + rk, ["u1"])
                    hop("dve", lambda e, pi_=pi_: e.tensor_tensor(out=u2[top], in0=CTim[top], in1=pi_[top],
                                                                  op=ALU.mult), ["CTim"] + rk, ["u2"])
                    hop("dve", lambda e, t=t: e.tensor_tensor(out=Ys[top, :, t, :], in0=u1[top], in1=u2[top],
                                                              op=ALU.subtract), ["u1", "u2"], ["Ys"])
                    hop("dve", lambda e, pr=pr: e.tensor_tensor(out=u1[bot], in0=CTim[bot], in1=pr[bot], op=ALU.mult),
                        ["CTim"] + rk, ["u1"])
                    hop("dve", lambda e, pi_=pi_: e.tensor_tensor(out=u2[bot], in0=CTre[bot], in1=pi_[bot],
                                                                  op=ALU.mult), ["CTre"] + rk, ["u2"])
                    hop("dve", lambda e, t=t: e.scalar_tensor_tensor(
                        out=Ys[bot, :, t, :], in0=u1[bot], scalar=-1.0, in1=u2[bot], op0=ALU.mult, op1=ALU.subtract),
                        ["u1", "u2"], ["Ys"])
                for gl in range(GB):
                    g = g0 + gl
                    b = g % 2
                    Xg = Xs[:, gl, :, :].rearrange("p s c -> p (s c)")
                    Y0 = Ys[:, gl, 0:8, :].rearrange("p t c -> p (t c)")
                    Y1 = Ys[:, gl, 1:9, :].rearrange("p t c -> p (t c)")
                    pa = psA[b]
                    P.op("pe", lambda e, pa=pa, Xg=Xg, Y0=Y0: e.matmul(pa[:, 0:128], Xg, Y0, start=True, stop=True),
                         reads=["Xs", "Ys"], writes=["psA%d" % b])
                    P.op("dve", lambda e, pa=pa, b=b: e.tensor_tensor(
                        out=stB[b][:, 0, :], in0=pa[:, 0:128], in1=mT0[:].rearrange("p t c -> p (t c)"),
                        op=ALU.mult), reads=["psA%d" % b, "mT0"], writes=["stB%d" % b])
                    P.op("dve", lambda e, b=b, g=g: e.tensor_scalar(
                        out=Mt[b][:], in0=self.ident_f[:], scalar1=zs[0][0][0][:, g:g + 1], scalar2=None,
                        op0=ALU.mult), reads=[zs[0][0][1]], writes=["Mt%d" % b])
                    P.op("dve", lambda e, b=b, g=g: e.scalar_tensor_tensor(
                        out=Mt[b][:], in0=jsw[:], scalar=sims[0][0][:, g:g + 1], in1=Mt[b][:],
                        op0=ALU.mult, op1=ALU.add), reads=["jsw", sims[0][1], "Mt%d" % b], writes=["Mt%d" % b])
                    P.op("pe", lambda e, pa=pa, Xg=Xg, b=b: e.matmul(pa[:, 128:256], Xg, Mt[b][:], start=True,
                                                                     stop=True),
                         reads=["Xs", "Mt%d" % b], writes=["psA%d" % b])
                    P.op("act", lambda e, pa=pa, b=b: e.copy(out=stB[b][:, 1, :], in_=pa[:, 128:256]),
                         reads=["psA%d" % b], writes=["stB%d" % b])
                    P.op("act", lambda e, b=b, Y1=Y1: e.copy(out=stB[b][:, 2, :], in_=Y1),
                         reads=["Ys"], writes=["stB%d" % b])
                    P.dma("sp", MB[g], stB[b][:], reads=["stB%d" % b])
                    for k in range(NL):
                        z = zs[1 + k]
                        sm = sims[1 + k]
                        P.op("dve", lambda e, b=b, g=g, k=k, z=z: e.tensor_scalar(
                            out=stP[b][:, k, :], in0=self.ident_f[:], scalar1=z[0][0][:, g:g + 1], scalar2=None,
                            op0=ALU.mult), reads=[z[0][1]], writes=["stP%d" % b])
                        P.op("dve", lambda e, b=b, g=g, k=k, sm=sm: e.scalar_tensor_tensor(
                            out=stP[b][:, k, :], in0=jsw[:], scalar=sm[0][:, g:g + 1], in1=stP[b][:, k, :],
                            op0=ALU.mult, op1=ALU.add), reads=["jsw", sm[1], "stP%d" % b], writes=["stP%d" % b])
                    P.dma("sp", MP[g], stP[b][:], reads=["stP%d" % b])
            P.emit()

    def stage_ssm_run(self, utok, MB, MP, d_ap, yT, NL, G=256):
        nc, P = self.nc, self.P
        S = self.S
        NJ = S // 8
        JP = min(128, NJ)
        NJT = NJ // JP
        GBK = 32
        NW = 4
        NB2 = 2 * NW
        with ExitStack() as st:
            T = self.mkT(st)
            Ust = [T("r_Ust%d" % i, [128, 8, 512], BF16) for i in range(2)]
            U2 = T("r_U2", [128, NJT, GBK, 8, 16], BF16)
            Yblk = T("r_Y", [128, NJT, 8, 512], BF16)
            dbc = T("r_d", [128, 512], F32)
            mb = [T("r_mb%d" % i, [128, 3, 128], BF16) for i in range(NB2)]
            mp = [T("r_mp%d" % i, [128, NL, 128], F32) for i in range(NB2)]
            Ug = [T("r_Ug%d" % i, [128, NJ], BF16) for i in range(NB2)]
            Sg = [T("r_Sg%d" % i, [128, NJ], F32) for i in range(NB2)]
            Sp = [T("r_Sp%d" % i, [128, NJ], BF16) for i in range(NB2)]
            Yg = [T("r_Yg%d" % i, [128, NJ], BF16) for i in range(NB2)]
            tmp = T("r_tmp", [128, 4, 512], F32)
            g1 = T("r_g1", [128, 4, 512], F32)
            g2 = T("r_g2", [128, 4, 512], F32)
            yact = [T("r_ya%d" % i, [128, 4, 512], BF16) for i in range(2)]
            yTb = [T("r_yT%d" % i, [128, 4, JP * 8], BF16) for i in range(1)] * 2
            psU = self.PS(st, "r_psU", [128, 1024], BF16)
            ring = [self.PS(st, "r_ring%d" % i, [128, 512], F32) for i in range(NW)]
            psYT = self.PS(st, "r_psYT", [128, 1024], BF16)
            psT2 = self.PS(st, "r_psT2", [128, 1024], BF16)
            for i in range(NB2):
                P.op("pool", lambda e, i=i: e.memset(Sp[i][:], 0.0), writes=["Sp%d" % i])
            wv = 0
            ti = 0
            for blk in range(G // GBK):
                ch0 = blk * 512
                for jt in range(NJT):
                    ub = jt % 2
                    P.dma("pool", Ust[ub][0:JP], utok[jt * JP * 8:(jt + 1) * JP * 8, ch0:ch0 + 512].rearrange(
                        "(jp s) c -> jp s c", s=8), writes=["Ust%d" % ub])
                    P.op("dve", lambda e, ub=ub, jt=jt: e.tensor_copy(
                        out=U2[0:JP, jt], in_=Ust[ub][0:JP].rearrange("p s (g c) -> p g s c", c=16)),
                        reads=["Ust%d" % ub], writes=["U2"])
                P.dma("sp", dbc[0:JP], d_ap[0, ch0:ch0 + 512].partition_broadcast(JP), writes=["dbc"])
                cut = getattr(self, "cut", 9)
                for w0 in range(0, GBK, NW):
                    if cut < 2:
                        break
                    sl = [(wv % 2) * NW + i for i in range(NW)]
                    wv += 1
                    gls = [w0 + i for i in range(NW)]
                    for i in range(NW):
                        g = blk * GBK + gls[i]
                        P.dma("sp", mb[sl[i]][:], MB[g], writes=["mb%d" % sl[i]])
                        P.dma("sp", mp[sl[i]][:], MP[g], writes=["mp%d" % sl[i]])
                    if cut < 2.5:
                        continue
                    for i in range(NW):
                        s_, gl = sl[i], gls[i]
                        hf = 0
                        for jt in range(NJT):
                            P.op("pe", lambda e, jt=jt, gl=gl, hf=hf: e.transpose(
                                psU[:, hf * 512 + jt * JP: hf * 512 + (jt + 1) * JP],
                                U2[0:JP, jt, gl].rearrange("p s c -> p (s c)"), self.ident_bf[0:JP, 0:JP]),
                                reads=["U2"], writes=["psU%d" % hf])
                        if cut < 2.7:
                            continue
                        P.op("act", lambda e, s_=s_, hf=hf: e.copy(out=Ug[s_][:], in_=psU[:, hf * 512:hf * 512 + NJ]),
                             reads=["psU%d" % hf], writes=["Ug%d" % s_])
                    if cut < 3:
                        continue
                    for i in range(NW):
                        s_ = sl[i]
                        P.op("pe", lambda e, i=i, s_=s_: e.matmul(ring[i][:, 0:NJ], mb[s_][:, 1, :], Ug[s_][:],
                                                                  start=True, stop=True),
                             reads=["mb%d" % s_, "Ug%d" % s_], writes=["ring%d" % i])
                    for i in range(NW):
                        s_ = sl[i]
                        P.op("dve", lambda e, i=i, s_=s_: e.tensor_copy(out=Sg[s_][:], in_=ring[i][:, 0:NJ]),
                             reads=["ring%d" % i], writes=["Sg%d" % s_])
                    for k in range(NL):
                        sh = 1 << k
                        for i in range(NW):
                            s_ = sl[i]
                            P.op("pe", lambda e, i=i, s_=s_, k=k, sh=sh: e.matmul(
                                ring[i][:, 0:NJ - sh], mp[s_][:, k, :], Sg[s_][:, 0:NJ - sh], start=True, stop=True),
                                reads=["mp%d" % s_, "Sg%d" % s_], writes=["ring%d" % i])
                        for i in range(NW):
                            s_ = sl[i]
                            P.op("dve", lambda e, i=i, s_=s_, sh=sh: e.tensor_tensor(
                                out=Sg[s_][:, sh:NJ], in0=ring[i][:, 0:NJ - sh], in1=Sg[s_][:, sh:NJ], op=ALU.add),
                                reads=["ring%d" % i, "Sg%d" % s_], writes=["Sg%d" % s_])
                    if cut < 4:
                        continue
                    for i in range(NW):
                        s_ = sl[i]
                        P.op("act", lambda e, s_=s_: e.copy(out=Sp[s_][:, 1:NJ], in_=Sg[s_][:, 0:NJ - 1]),
                             reads=["Sg%d" % s_], writes=["Sp%d" % s_])
                    for i in range(NW):
                        s_ = sl[i]
                        P.op("pe", lambda e, i=i, s_=s_: e.matmul(ring[i][:, 0:NJ], mb[s_][:, 0, :], Ug[s_][:],
                                                                  start=True, stop=False),
                             reads=["mb%d" % s_, "Ug%d" % s_], writes=["ring%d" % i])
                        P.op("pe", lambda e, i=i, s_=s_: e.matmul(ring[i][:, 0:NJ], mb[s_][:, 2, :], Sp[s_][:],
                                                                  start=False, stop=True),
                             reads=["mb%d" % s_, "Sp%d" % s_], writes=["ring%d" % i])
                        P.op("act", lambda e, i=i, s_=s_: e.copy(out=Yg[s_][:], in_=ring[i][:, 0:NJ]),
                             reads=["ring%d" % i], writes=["Yg%d" % s_])
                    for i in range(NW):
                        s_, gl = sl[i], gls[i]
                        hf = 0
                        for jt in range(NJT):
                            P.op("pe", lambda e, jt=jt, s_=s_, hf=hf: e.transpose(
                                psYT[0:JP, hf * 512 + jt * 128: hf * 512 + (jt + 1) * 128],
                                Yg[s_][:, jt * JP:(jt + 1) * JP], self.ident_bf[:]),
                                reads=["Yg%d" % s_], writes=["psYT%d" % hf])
                        P.op("dve", lambda e, gl=gl, hf=hf: e.tensor_copy(
                            out=Yblk[0:JP, :, :, gl * 16:(gl + 1) * 16],
                            in_=psYT[0:JP, hf * 512:hf * 512 + NJT * 128].rearrange("p (j t c) -> p j t c", j=NJT, t=8)),
                            reads=["psYT%d" % hf], writes=["Yblk"])
                for jt in range(NJT):
                    if cut < 5:
                        break
                    for th in range(2):
                        tsl = slice(th * 4, (th + 1) * 4)
                        yb = ti % 2
                        ti += 1
                        P.op("dve", lambda e, jt=jt, tsl=tsl: e.tensor_tensor(
                            out=tmp[0:JP].rearrange("p t (g c) -> p g t c", c=16), in0=U2[0:JP, jt, :, tsl, :],
                            in1=dbc[0:JP].rearrange("p (g c) -> p g c", c=16).unsqueeze(2).broadcast_to(
                                [JP, GBK, 4, 16]), op=ALU.mult),
                            reads=["U2", "dbc"], writes=["tmp"])
                        P.op("dve", lambda e, jt=jt, tsl=tsl: e.tensor_tensor(
                            out=tmp[0:JP], in0=tmp[0:JP], in1=Yblk[0:JP, jt, tsl, :], op=ALU.add),
                            reads=["tmp", "Yblk"], writes=["tmp"])
                        self.gelu_ops("r", tmp[0:JP], ["tmp"], yact[yb][0:JP], "ya%d" % yb,
                                      g1[0:JP], "g1", g2[0:JP], "g2")
                        for cb in range(4):
                            hf = 0
                            for tt in range(4):
                                P.op("pe", lambda e, yb=yb, tt=tt, cb=cb, hf=hf: e.transpose(
                                    psT2[:, hf * 512 + tt * JP: hf * 512 + (tt + 1) * JP],
                                    yact[yb][0:JP, tt, cb * 128:(cb + 1) * 128], self.ident_bf[0:JP, 0:JP]),
                                    reads=["ya%d" % yb], writes=["psT2%d" % hf])
                            eng = "act" if cb % 2 == 0 else "dve"
                            src = psT2[:, hf * 512:hf * 512 + 4 * JP].rearrange("p (t j) -> p t j", t=4)
                            yb_ = jt % 2
                            dst = yTb[yb_][:, cb, :].rearrange("p (j t) -> p j t", t=8)[:, :, tsl].rearrange(
                                "p j t -> p t j")
                            if eng == "act":
                                P.op("act", lambda e, src=src, dst=dst: e.copy(out=dst, in_=src),
                                     reads=["psT2%d" % hf], writes=["yTb0"])
                            else:
                                P.op("dve", lambda e, src=src, dst=dst: e.tensor_copy(out=dst, in_=src),
                                     reads=["psT2%d" % hf], writes=["yTb0"])
                    P.dma("sp", yT[ch0:ch0 + 512, jt * JP * 8:(jt + 1) * JP * 8].rearrange("(cb p) t -> p cb t", p=128),
                          yTb[jt % 2][:], reads=["yTb0"])
            P.emit()


W_SPECS = [
    ("mix0_norm_g", [1, D]), ("mix0_ada_w", [D, 3 * D]), ("mix0_ada_b", [1, 3 * D]),
    ("mix0_w_in", [D, 10240]), ("gm_ln_g", [16, 128]), ("gm_w_s", [16, 128, 128]), ("gm_b_s", [16, 128]),
    ("mix0_w_out", [D, D]),
    ("ffn0_norm_g", [1, D]), ("ffn0_ada_w", [D, 3 * D]), ("ffn0_ada_b", [1, 3 * D]),
    ("ffn0_w_gate", [D, FFN_DENSE]), ("ffn0_w_up", [D, FFN_DENSE]), ("ffn0_w_down", [FFN_DENSE, D]),
    ("mix1_norm_g", [1, D]), ("mix1_ada_w", [D, 3 * D]), ("mix1_ada_b", [1, 3 * D]),
    ("ssm_w_in", [D, D]), ("ssm_lam_re", [256, 64]), ("ssm_lam_im", [256, 64]), ("ssm_log_dt", [1, 256]),
    ("ssm_b_re", [256, 64, 16]), ("ssm_b_im", [256, 64, 16]), ("ssm_c_re", [256, 16, 64]),
    ("ssm_c_im", [256, 16, 64]), ("ssm_d", [1, D]), ("glu_w_a", [D, D]), ("glu_w_b", [D, D]),
    ("moe_norm_g", [1, D]), ("moe_ada_w", [D, 3 * D]), ("moe_ada_b", [1, 3 * D]),
    ("moe_w_router", [D, N_EXP]), ("moe_w_gate", [N_EXP, D, FFN_EXP]), ("moe_w_up", [N_EXP, D, FFN_EXP]),
    ("moe_w_down", [N_EXP, FFN_EXP, D]), ("final_norm_g", [1, D]),
]


def build_model(S):
    B = Builder(S, None)
    NL = int(round(math.log2(S // 8)))
    xT = B.ext_in("xT", [D, S])
    c = B.ext_in("c", [1, D])
    w = {n: B.ext_in(n, shp) for n, shp in W_SPECS}
    outT = B.ext_out("outT", [D, S])
    hT = B.scratch("hT", [D, S], BF16)
    xres = B.scratch("xres", [D, S], F32)
    qT = B.scratch("qT", [2048, S], BF16)
    kT = B.scratch("kT", [2048, S], BF16)
    uT = B.scratch("uT", [2048, S], BF16)
    v = B.scratch("v", [S, 2048], BF16)
    z2g = B.scratch("z2g", [S, 2048], F32)
    mixT = B.scratch("mixT", [D, S], BF16)
    utok = B.scratch("utok", [S, D], BF16)
    yT = B.scratch("yT", [D, S], BF16)
    MB = B.scratch("MB", [256, 128, 3, 128], BF16)
    MP = B.scratch("MP", [256, 128, NL, 128], F32)
    gatesT = B.scratch("gatesT", [N_EXP, S], F32)
    B.setup_consts()
    B.stage_ada(c, [(w[p + "_ada_w"], w[p + "_ada_b"], w[p + "_norm_g"]) for p in ("mix0", "ffn0", "mix1", "moe")])
    B.stage_ssm_prep(w["ssm_lam_re"], w["ssm_lam_im"], w["ssm_log_dt"], w["ssm_b_re"], w["ssm_b_im"],
                     w["ssm_c_re"], w["ssm_c_im"], MB, MP, NL)
    win = w["mix0_w_in"]
    B.stage_norm(xT, hT, 0)
    B.stage_linear_fm(hT, D, [dict(w=[win[:, 0:2048]], N=2048, epi="bf16", out=qT),
                              dict(w=[win[:, 2048:4096]], N=2048, epi="bf16", out=kT),
                              dict(w=[win[:, 6144:8192]], N=2048, epi="gelu", out=uT)])
    B.stage_linear_tm(hT, win[:, 4096:6144], 2048, v, BF16)
    B.stage_linear_tm(hT, win[:, 8192:10240], 2048, z2g, F32, gelu=True)
    B.stage_attn(qT, kT, v, mixT[0:2048, :])
    B.stage_gmlp(uT, z2g, w["gm_ln_g"], w["gm_w_s"], w["gm_b_s"], mixT[2048:4096, :])
    B.stage_linear_fm(mixT, D, [dict(w=[w["mix0_w_out"]], N=D, epi="resid", m=0, xsrc=xT, xdst=xres)])
    B.stage_norm(xres, hT, 1)
    B.stage_ffn(hT, [(w["ffn0_w_gate"], w["ffn0_w_up"], w["ffn0_w_down"])], FFN_DENSE, 1, xres, xres)
    B.stage_norm(xres, hT, 2)
    B.stage_linear_tm(hT, w["ssm_w_in"], D, utok, BF16)
    B.stage_ssm_run(utok, MB, MP, w["ssm_d"], yT, NL)
    B.stage_linear_fm(yT, D, [dict(w=[w["glu_w_a"], w["glu_w_b"]], N=D, epi="glu", m=2, xsrc=xres, xdst=xres)])
    B.stage_norm(xres, hT, 3, router=(w["moe_w_router"], gatesT))
    B.stage_ffn(hT, [(w["moe_w_gate"][e], w["moe_w_up"][e], w["moe_w_down"][e]) for e in range(N_EXP)],
                FFN_EXP, 3, xres, xres, gatesT=gatesT)
    B.stage_norm(xres, outT, 0, out_dtype=F32, final_g=w["final_norm_g"])
    return B


def weight_map(inputs):
    m = {}
    for n, shp in W_SPECS:
        m[n] = np.ascontiguousarray(np.asarray(inputs[n], dtype=np.float32).reshape(shp))
    return m


SEQ = 4096
N_CORES = 2


def kernel(**inputs):
    x = np.asarray(inputs["x"], dtype=np.float32)
    c = np.asarray(inputs["c"], dtype=np.float32)
    bsz = x.shape[0]
    B = build_model(SEQ)
    wm = weight_map(inputs)
    in_maps = []
    for b in range(bsz):
        m = dict(wm)
        m["xT"] = np.ascontiguousarray(x[b].T)
        m["c"] = np.ascontiguousarray(c[b:b + 1])
        in_maps.append(m)
    res = run_bass_kernel_spmd(B.nc, in_maps, core_ids=list(range(bsz)))
    out = np.stack([np.ascontiguousarray(res.results[b]["outT"].T) for b in range(bsz)], axis=0)
    return out.astype(np.float32)
```

```python
import math
from contextlib import ExitStack

import numpy as np
import concourse.bass as bass
import concourse.mybir as mybir
from concourse.bass_utils import run_bass_kernel_spmd

F32 = mybir.dt.float32
BF16 = mybir.dt.bfloat16
ALU = mybir.AluOpType
AF = mybir.ActivationFunctionType
AX = mybir.AxisListType

D = 4096
KC = D // 128
TB = 512
EPS = 1e-6
FFN_DENSE = 11008
N_EXP = 8
FFN_EXP = 4096


class _Op:
    __slots__ = ("eng", "fn", "deps", "sig", "sigval", "is_dma", "dsem", "dval", "dprev")

    def __init__(self, eng, fn, deps, is_dma):
        self.eng = eng
        self.fn = fn
        self.deps = deps
        self.sig = False
        self.sigval = 0
        self.is_dma = is_dma
        self.dsem = -1
        self.dval = 0
        self.dprev = 0


class Prog:
    ENG = ("pe", "act", "dve", "pool", "sp")

    def __init__(self, nc, stack, n_dsem=32):
        self.nc = nc
        self.sems = {e: stack.enter_context(nc.semaphore("s_" + e)) for e in self.ENG}
        self.sigcount = {e: 0 for e in self.ENG}
        self.dsems = [stack.enter_context(nc.semaphore("dq%d" % i)) for i in range(n_dsem)]
        self.dtotal = [0] * n_dsem
        self.dnext = 0
        self.seen = {e: {} for e in self.ENG}
        self._reset()

    def _reset(self):
        self.ops = {e: [] for e in self.ENG}
        self.order = []
        self.last_w = {}
        self.readers = {}

    def _rec(self, eng, fn, reads, writes, is_dma):
        deps = []
        for k in reads:
            w = self.last_w.get(k)
            if w is not None:
                deps.append(w)
        for k in writes:
            w = self.last_w.get(k)
            if w is not None:
                deps.append(w)
            deps.extend(self.readers.get(k, ()))
        o = _Op(eng, fn, deps, is_dma)
        for k in reads:
            self.readers.setdefault(k, []).append(o)
        for k in writes:
            self.last_w[k] = o
            self.readers[k] = []
        self.ops[eng].append(o)
        self.order.append(o)
        return o

    def op(self, eng, fn, reads=(), writes=()):
        return self._rec(eng, fn, reads, writes, False)

    def dma(self, queue, out, in_, reads=(), writes=(), **kw):
        return self._rec(queue, lambda e: e.dma_start(out=out, in_=in_, **kw), reads, writes, True)

    def emit(self):
        nc = self.nc
        for o in self.order:
            for d in o.deps:
                if d.is_dma:
                    continue
                if d.eng == "pe" and o.eng == "pe" and not o.is_dma:
                    continue
                d.sig = True
        for e in self.ENG:
            for o in reversed(self.ops[e]):
                if not o.is_dma:
                    o.sig = True
                    break
        for e in self.ENG:
            for o in self.ops[e]:
                if not o.is_dma and o.sig:
                    self.sigcount[e] += 1
                    o.sigval = self.sigcount[e]
        n = len(self.dsems)
        for o in self.order:
            if o.is_dma:
                k = self.dnext
                self.dnext = (k + 1) % n
                o.dsem = k
                o.dprev = self.dtotal[k]
                self.dtotal[k] += 16
                o.dval = self.dtotal[k]

        def run(engobj, e):
            seen = self.seen[e]

            def wait(key, sem, val):
                if val <= 0 or seen.get(key, 0) >= val:
                    return
                seen[key] = val
                engobj.wait_ge(sem, val)

            for o in self.ops[e]:
                for d in o.deps:
                    if d.is_dma:
                        wait(("d", d.dsem), self.dsems[d.dsem], d.dval)
                    elif d.eng == "pe" and e == "pe" and not o.is_dma:
                        continue
                    else:
                        wait(("c", d.eng), self.sems[d.eng], d.sigval)
                if o.is_dma:
                    wait(("d", o.dsem), self.dsems[o.dsem], o.dprev)
                ins = o.fn(engobj)
                if o.is_dma:
                    ins.then_inc(self.dsems[o.dsem], 16)
                elif o.sig:
                    ins.then_inc(self.sems[e], 1)
            for e2 in self.ENG:
                wait(("c", e2), self.sems[e2], self.sigcount[e2])
            for k in range(n):
                wait(("d", k), self.dsems[k], self.dtotal[k])

        with nc.Block() as block:
            @block.tensor
            def _(t):
                run(t, "pe")

            @block.scalar
            def _(t):
                run(t, "act")

            @block.vector
            def _(t):
                run(t, "dve")

            @block.gpsimd
            def _(t):
                run(t, "pool")

            @block.sync
            def _(t):
                run(t, "sp")
        self._reset()


class Builder:
    def __init__(self, S, stages, dbg=()):
        self.S = S
        self.NTB = S // TB
        self.stages = stages
        self.dbg = set(dbg)
        self.nc = bass.Bass("TRN2", target_bir_lowering=False)
        self.gstack = ExitStack()
        self.P = Prog(self.nc, self.gstack)
        self.inputs = {}
        self.ucount = 0

    def ext_in(self, name, shape, dtype=F32):
        t = self.nc.dram_tensor(name, list(shape), dtype, kind="ExternalInput").ap()
        self.inputs[name] = t
        return t

    def ext_out(self, name, shape, dtype=F32):
        return self.nc.dram_tensor(name, list(shape), dtype, kind="ExternalOutput").ap()

    def scratch(self, name, shape, dtype):
        return self.nc.dram_tensor(name, list(shape), dtype, kind="Internal").ap()

    def gtile(self, name, shape, dtype):
        return self.gstack.enter_context(self.nc.sbuf_tensor(name, list(shape), dtype))

    def mkT(self, st):
        self.ucount += 1
        sid = self.ucount
        nc = self.nc
        return lambda n, s, d: st.enter_context(nc.sbuf_tensor("%s_s%d" % (n, sid), list(s), d))

    def PS(self, st, name, shape, dtype):
        self.ucount += 1
        return st.enter_context(self.nc.psum_tensor("%s_p%d" % (name, self.ucount), list(shape), dtype))

    def uid(self, p):
        self.ucount += 1
        return "%s%d" % (p, self.ucount)

    def setup_consts(self):
        P = self.P
        self.ones_bf = self.gtile("ones_bf", [128, 128], BF16)
        self.ones_f = self.gtile("ones_f", [128, 128], F32)
        self.ident_f = self.gtile("ident_f", [128, 128], F32)
        self.ident_bf = self.gtile("ident_bf", [128, 128], BF16)
        P.op("pool", lambda e: e.memset(self.ones_bf[:], 1.0), writes=["ones_bf"])
        P.op("pool", lambda e: e.memset(self.ones_f[:], 1.0), writes=["ones_f"])
        P.op("pool", lambda e: e.affine_select(
            out=self.ident_f[:], in_=self.ones_f[:], pattern=[[-1, 128]], compare_op=ALU.is_equal,
            fill=0.0, base=0, channel_multiplier=1), reads=["ones_f"], writes=["ident_f"])
        P.op("dve", lambda e: e.tensor_copy(out=self.ident_bf[:], in_=self.ident_f[:]),
             reads=["ident_f"], writes=["ident_bf"])

    def stage_ada(self, c_ap, mats):
        nc, P = self.nc, self.P
        M = len(mats)
        self.ada = self.gtile("ada", [128, M, 96], F32)
        self.gsc = self.gtile("gsc", [128, M, KC], F32)
        with ExitStack() as st:
            T = self.mkT(st)
            cT = T("cT", [128, KC], F32)
            scT = T("scT", [128, KC], BF16)
            row = T("row", [1, 3 * D], F32)
            bias = T("bias", [128, 96], F32)
            gT = T("gT", [128, KC], F32)
            one11 = T("one11", [1, 1], F32)
            slab = [T("aslab%d" % i, [128, KC, 512], BF16) for i in range(2)]
            ps = [self.PS(st, "aps%d" % i, [128, 512], F32) for i in range(2)]
            psT = self.PS(st, "apsT", [128, 512], F32)
            P.dma("sp", cT[:], c_ap.rearrange("o (c p) -> p (o c)", p=128), writes=["cT"],
                  allow_slow_non_contiguous=True)
            P.op("pool", lambda e: e.memset(one11[:], 1.0), writes=["one11"])
            P.op("act", lambda e: e.activation(out=scT[:], in_=cT[:], func=AF.Silu),
                 reads=["cT"], writes=["scT"])
            it = 0
            for m, (w, b, g) in enumerate(mats):
                for q in range(3):
                    P.dma("sp", bias[:, q * 32:(q + 1) * 32],
                          b[:, q * D:(q + 1) * D].rearrange("o (c p) -> p (o c)", p=128),
                          writes=["bias%d" % q], allow_slow_non_contiguous=True)
                P.dma("sp", gT[:], g.rearrange("o (c p) -> p (o c)", p=128), writes=["gT"],
                      allow_slow_non_contiguous=True)
                for j in range(3 * D // 512):
                    sl = slab[it % 2]
                    pj = ps[it % 2]
                    sk = "aslab%d" % (it % 2)
                    pk = "aps%d" % (it % 2)
                    it += 1
                    P.dma("pool", sl[:], w[:, j * 512:(j + 1) * 512].rearrange("(c p) n -> p c n", p=128),
                          writes=[sk])
                    for kc in range(KC):
                        P.op("pe", lambda e, sl=sl, pj=pj, kc=kc: e.matmul(
                            pj[0:1, :], scT[:, kc:kc + 1], sl[:, kc, :], start=(kc == 0), stop=(kc == KC - 1)),
                            reads=[sk, "scT"], writes=[pk])
                    P.op("act", lambda e, pj=pj, j=j: e.copy(out=row[0:1, j * 512:(j + 1) * 512], in_=pj[0:1, :]),
                         reads=[pk], writes=["row"])
                for i in range(96):
                    P.op("pe", lambda e, i=i: e.matmul(psT[:, i:i + 1], row[0:1, i * 128:(i + 1) * 128],
                                                       one11[0:1, 0:1], start=True, stop=True),
                         reads=["row", "one11"], writes=["psT"])
                P.op("dve", lambda e, m=m: e.tensor_tensor(out=self.ada[:, m, :], in0=psT[:, 0:96], in1=bias[:],
                                                           op=ALU.add),
                     reads=["psT", "bias0", "bias1", "bias2"], writes=["ada"])
                P.op("dve", lambda e, m=m: e.scalar_tensor_tensor(
                    out=self.gsc[:, m, :], in0=self.ada[:, m, 32:64], scalar=1.0, in1=gT[:],
                    op0=ALU.add, op1=ALU.mult), reads=["ada", "gT"], writes=["gsc"])
            P.emit()

    def shift(self, m, c):
        return self.ada[:, m, c:c + 1]

    def gate(self, m, c):
        return self.ada[:, m, 64 + c:65 + c]

    def stage_norm(self, src, dst, m, out_dtype=BF16, final_g=None, router=None):
        nc, P = self.nc, self.P
        with ExitStack() as st:
            T = self.mkT(st)
            xt = T("n_xt", [128, KC, TB], F32)
            sq = T("n_sq", [128, KC, TB], BF16)
            hsb = T("n_h", [128, KC, TB], out_dtype)
            rstd = T("n_rstd", [128, TB], F32)
            tmp = [T("n_tmp%d" % i, [128, TB], F32) for i in range(2)]
            ps = self.PS(st, "n_ps", [128, TB], F32)
            if final_g is not None:
                gfin = T("n_gfin", [128, KC], F32)
                P.dma("sp", gfin[:], final_g.rearrange("o (c p) -> p (o c)", p=128), writes=["gfin"],
                      allow_slow_non_contiguous=True)
            if router is not None:
                wr, gdst = router
                wr_sb = T("n_wr", [128, KC, N_EXP], F32)
                h32 = [T("n_h32%d" % i, [128, TB], F32) for i in range(2)]
                lg = T("n_lg", [N_EXP, TB], F32)
                lgt = T("n_lgt", [128, 4, N_EXP], F32)
                top8 = T("n_top8", [128, 4, 8], F32)
                gm = T("n_gm", [128, 4, N_EXP], F32)
                ge = T("n_ge", [128, 4, N_EXP], F32)
                den = T("n_den", [128, 4], F32)
                nm1 = T("n_nm1", [128, 4], F32)
                gts = T("n_gts", [N_EXP, TB], F32)
                psl = self.PS(st, "n_psl", [128, TB], F32)
                pst = self.PS(st, "n_pst", [128, TB], F32)
                P.dma("sp", wr_sb[:], wr.rearrange("(c p) e -> p c e", p=128), writes=["wr"],
                      allow_slow_non_contiguous=True)
            for tb in range(self.NTB):
                ts = slice(tb * TB, (tb + 1) * TB)
                P.dma("sp", xt[:], src[:, ts].rearrange("(c p) t -> p c t", p=128), writes=["xt"])
                for q in range(4):
                    P.op("act", lambda e, q=q: e.activation(out=sq[:, q * 8:(q + 1) * 8, :],
                                                            in_=xt[:, q * 8:(q + 1) * 8, :], func=AF.Square),
                         reads=["xt"], writes=["sq%d" % q])
                for c in range(KC):
                    P.op("pe", lambda e, c=c: e.matmul(ps[:], self.ones_bf[:], sq[:, c, :],
                                                       start=(c == 0), stop=(c == KC - 1)),
                         reads=["sq%d" % (c // 8)], writes=["ps"])
                P.op("dve", lambda e: e.tensor_scalar(out=rstd[:], in0=ps[:], scalar1=1.0 / D, scalar2=EPS,
                                                      op0=ALU.mult, op1=ALU.add), reads=["ps"], writes=["rstd"])
                P.op("act", lambda e: e.activation(out=rstd[:], in_=rstd[:], func=AF.Sqrt),
                     reads=["rstd"], writes=["rstd"])
                P.op("dve", lambda e: e.reciprocal(out=rstd[:], in_=rstd[:]), reads=["rstd"], writes=["rstd"])
                for c in range(KC):
                    tk = "tmp%d" % (c % 2)
                    tt = tmp[c % 2]
                    P.op("dve", lambda e, c=c, tt=tt: e.tensor_tensor(out=tt[:], in0=xt[:, c, :], in1=rstd[:],
                                                                      op=ALU.mult),
                         reads=["xt", "rstd"], writes=[tk])
                    if final_g is not None:
                        P.op("act", lambda e, c=c, tt=tt: e.activation(out=hsb[:, c, :], in_=tt[:], func=AF.Copy,
                                                                       scale=gfin[:, c:c + 1]),
                             reads=[tk, "gfin"], writes=["hsb"])
                    elif router is None:
                        P.op("act", lambda e, c=c, tt=tt: e.activation(
                            out=hsb[:, c, :], in_=tt[:], func=AF.Identity, scale=self.gsc[:, m, c:c + 1],
                            bias=self.shift(m, c)), reads=[tk], writes=["hsb"])
                    else:
                        hk = "h32%d" % (c % 2)
                        hh = h32[c % 2]
                        P.op("act", lambda e, c=c, tt=tt, hh=hh: e.activation(
                            out=hh[:], in_=tt[:], func=AF.Identity, scale=self.gsc[:, m, c:c + 1],
                            bias=self.shift(m, c)), reads=[tk], writes=[hk])
                        P.op("dve", lambda e, c=c, hh=hh: e.tensor_copy(out=hsb[:, c, :], in_=hh[:]),
                             reads=[hk], writes=["hsb"])
                        P.op("pe", lambda e, c=c, hh=hh: e.matmul(psl[0:N_EXP, :], wr_sb[:, c, :], hh[:],
                                                                  start=(c == 0), stop=(c == KC - 1)),
                             reads=[hk, "wr"], writes=["psl"])
                P.dma("sp", dst[:, ts].rearrange("(c p) t -> p c t", p=128), hsb[:], reads=["hsb"])
                if router is not None:
                    P.op("act", lambda e: e.copy(out=lg[:], in_=psl[0:N_EXP, :]), reads=["psl"], writes=["lg"])
                    for q in range(4):
                        P.op("pe", lambda e, q=q: e.transpose(pst[:, q * N_EXP:(q + 1) * N_EXP],
                                                              lg[:, q * 128:(q + 1) * 128],
                                                              self.ident_f[0:N_EXP, 0:N_EXP]),
                             reads=["lg"], writes=["pst"])
                    P.op("dve", lambda e: e.tensor_copy(out=lgt[:], in_=pst[:, 0:4 * N_EXP].rearrange(
                        "p (q e) -> p q e", q=4)), reads=["pst"], writes=["lgt"])
                    for q in range(4):
                        P.op("dve", lambda e, q=q: e.max(out=top8[:, q, :], in_=lgt[:, q, :]),
                             reads=["lgt"], writes=["top8_%d" % q])
                    for q in range(4):
                        P.op("dve", lambda e, q=q: e.tensor_scalar(
                            out=gm[:, q, :], in0=lgt[:, q, :], scalar1=top8[:, q, 1:2], scalar2=None,
                            op0=ALU.is_ge), reads=["lgt", "top8_%d" % q], writes=["gm%d" % q])
                        P.op("dve", lambda e, q=q: e.tensor_scalar(
                            out=nm1[:, q:q + 1], in0=top8[:, q, 0:1], scalar1=-1.0, scalar2=None,
                            op0=ALU.mult), reads=["top8_%d" % q], writes=["nm1_%d" % q])
                        P.op("act", lambda e, q=q: e.activation(out=ge[:, q, :], in_=lgt[:, q, :], func=AF.Exp,
                                                                bias=nm1[:, q:q + 1]),
                             reads=["lgt", "nm1_%d" % q], writes=["ge%d" % q])
                        P.op("dve", lambda e, q=q: e.tensor_tensor(out=ge[:, q, :], in0=ge[:, q, :],
                                                                   in1=gm[:, q, :], op=ALU.mult),
                             reads=["ge%d" % q, "gm%d" % q], writes=["ge%d" % q])
                        P.op("dve", lambda e, q=q: e.reduce_sum(out=den[:, q:q + 1], in_=ge[:, q, :], axis=AX.X),
                             reads=["ge%d" % q], writes=["den%d" % q])
                        P.op("dve", lambda e, q=q: e.reciprocal(out=den[:, q:q + 1], in_=den[:, q:q + 1]),
                             reads=["den%d" % q], writes=["den%d" % q])
                        P.op("dve", lambda e, q=q: e.tensor_scalar(
                            out=ge[:, q, :], in0=ge[:, q, :], scalar1=den[:, q:q + 1], scalar2=None,
                            op0=ALU.mult), reads=["ge%d" % q, "den%d" % q], writes=["ge%d" % q])
                        P.op("pe", lambda e, q=q: e.transpose(psl[0:N_EXP, q * 128:(q + 1) * 128],
                                                              ge[:, q, :], self.ident_f[:]),
                             reads=["ge%d" % q], writes=["psl"])
                    P.op("act", lambda e: e.copy(out=gts[:], in_=psl[0:N_EXP, :]), reads=["psl"], writes=["gts"])
                    P.dma("sp", gdst[:, ts], gts[:], reads=["gts"])
            P.emit()

    def gelu_ops(self, pfx, src_ap, src_keys, out_ap, out_key, t1, t1k, t2, t2k):
        P = self.P
        P.op("act", lambda e: e.activation(out=t1, in_=src_ap, func=AF.Square), reads=src_keys, writes=[t1k])
        P.op("dve", lambda e: e.tensor_scalar(out=t1, in0=t1, scalar1=0.044715, scalar2=1.0,
                                              op0=ALU.mult, op1=ALU.add), reads=[t1k], writes=[t1k])
        P.op("dve", lambda e: e.tensor_tensor(out=t1, in0=t1, in1=src_ap, op=ALU.mult),
             reads=[t1k] + list(src_keys), writes=[t1k])
        P.op("act", lambda e: e.activation(out=t2, in_=t1, func=AF.Sigmoid, scale=1.5957691216057308),
             reads=[t1k], writes=[t2k])
        P.op("dve", lambda e: e.tensor_tensor(out=out_ap, in0=t2, in1=src_ap, op=ALU.mult),
             reads=[t2k] + list(src_keys), writes=[out_key])

    def stage_linear_fm(self, inT, K, jobs):
        nc, P = self.nc, self.P
        kcn = K // 128
        with ExitStack() as st:
            T = self.mkT(st)
            inb = T("l_in", [128, kcn, TB], BF16)
            nslab = 4
            slab = [T("l_slab%d" % i, [128, kcn, 256], BF16) for i in range(nslab)]
            ost = [T("l_ost%d" % i, [128, 2, TB], BF16) for i in range(2)]
            xo = [T("l_xo%d" % i, [128, TB], F32) for i in range(2)]
            xn = [T("l_xn%d" % i, [128, TB], F32) for i in range(2)]
            t1 = [T("l_t1%d" % i, [128, TB], F32) for i in range(2)]
            t2 = [T("l_t2%d" % i, [128, TB], F32) for i in range(2)]
            ps = [self.PS(st, "l_ps%d" % i, [128, TB], F32) for i in range(4)]
            si = 0
            ci = 0
            use_cache = self.NTB > 1
            if use_cache:
                sid = self.uid("lc")
                for ji, job in enumerate(jobs):
                    job["cache"] = [self.scratch("%s_%d_%d" % (sid, ji, wi), [job["N"] // 256, 128, kcn * 256], BF16)
                                    for wi in range(len(job["w"]))]
            for tb in range(self.NTB):
                ts = slice(tb * TB, (tb + 1) * TB)
                P.dma("sp", inb[:], inT[:, ts].rearrange("(c p) t -> p c t", p=128), writes=["inb"])
                for ji, job in enumerate(jobs):
                    N = job["N"]
                    epi = job["epi"]
                    for ng in range(N // 256):
                        sls = []
                        for wi, w in enumerate(job["w"]):
                            sl = slab[si % nslab]
                            sk = "slab%d" % (si % nslab)
                            si += 1
                            ck = "c%d_%d_%d" % (ji, wi, ng)
                            if tb == 0 or not use_cache:
                                P.dma("pool", sl[:], w[:, ng * 256:(ng + 1) * 256].rearrange("(c p) n -> p c n", p=128),
                                      writes=[sk])
                                if use_cache:
                                    P.dma("sp", job["cache"][wi][ng], sl[:].rearrange("p c n -> p (c n)"),
                                          reads=[sk], writes=[ck])
                            else:
                                P.dma("pool", sl[:].rearrange("p c n -> p (c n)"), job["cache"][wi][ng],
                                      reads=[ck], writes=[sk])
                            sls.append((sl, sk))
                        for j in range(2):
                            nch = ng * 2 + j
                            pss = []
                            for (sl, sk) in sls:
                                pt = ps[ci % 4]
                                pk = "ps%d" % (ci % 4)
                                ci += 1
                                for kc in range(kcn):
                                    P.op("pe", lambda e, pt=pt, sl=sl, kc=kc, j=j: e.matmul(
                                        pt[:], sl[:, kc, j * 128:(j + 1) * 128], inb[:, kc, :],
                                        start=(kc == 0), stop=(kc == kcn - 1)), reads=[sk, "inb"], writes=[pk])
                                pss.append((pt, pk))
                            b = ci % 2
                            if epi == "bf16":
                                o = ost[ng % 2]
                                ok = "ost%d_%d" % (ng % 2, j)
                                P.op("act", lambda e, o=o, j=j, pt=pss[0][0]: e.copy(out=o[:, j, :], in_=pt[:]),
                                     reads=[pss[0][1]], writes=[ok])
                            elif epi == "gelu":
                                o = ost[ng % 2]
                                ok = "ost%d_%d" % (ng % 2, j)
                                self.gelu_ops("l", pss[0][0][:], [pss[0][1]], o[:, j, :], ok,
                                              t1[b][:], "t1%d" % b, t2[b][:], "t2%d" % b)
                            else:
                                m = job["m"]
                                rows = slice(nch * 128, (nch + 1) * 128)
                                P.dma("sp", xo[b][:], job["xsrc"][rows, ts], writes=["xo%d" % b])
                                if epi == "resid":
                                    val, vk = pss[0][0][:], pss[0][1]
                                else:
                                    P.op("act", lambda e, b=b, pt=pss[1][0]: e.activation(
                                        out=t1[b][:], in_=pt[:], func=AF.Sigmoid), reads=[pss[1][1]],
                                        writes=["t1%d" % b])
                                    P.op("dve", lambda e, b=b, pt=pss[0][0]: e.tensor_tensor(
                                        out=t2[b][:], in0=pt[:], in1=t1[b][:], op=ALU.mult),
                                        reads=[pss[0][1], "t1%d" % b], writes=["t2%d" % b])
                                    val, vk = t2[b][:], "t2%d" % b
                                P.op("dve", lambda e, b=b, val=val, m=m, nch=nch: e.scalar_tensor_tensor(
                                    out=xn[b][:], in0=val, scalar=self.gate(m, nch), in1=xo[b][:],
                                    op0=ALU.mult, op1=ALU.add), reads=[vk, "xo%d" % b], writes=["xn%d" % b])
                                P.dma("sp", job["xdst"][rows, ts], xn[b][:], reads=["xn%d" % b])
                        if epi in ("bf16", "gelu"):
                            o = ost[ng % 2]
                            P.dma("sp", job["out"][ng * 256:(ng + 1) * 256, ts].rearrange("(j p) t -> p j t", p=128),
                                  o[:], reads=["ost%d_0" % (ng % 2), "ost%d_1" % (ng % 2)])
            P.emit()

    def stage_linear_tm(self, inT, w, N, out, out_dtype, gelu=False):
        nc, P = self.nc, self.P
        with ExitStack() as st:
            T = self.mkT(st)
            inb = T("m_in", [128, KC, TB], BF16)
            slab = [T("m_slab%d" % i, [128, KC, 512], BF16) for i in range(2)]
            ost = [T("m_ost%d" % i, [128, 512], out_dtype) for i in range(2)]
            t1 = [T("m_t1%d" % i, [128, 512], F32) for i in range(2)]
            t2 = [T("m_t2%d" % i, [128, 512], F32) for i in range(2)]
            ps = [self.PS(st, "m_ps%d" % i, [128, 512], F32) for i in range(2)]
            si = 0
            ci = 0
            use_cache = self.NTB > 1
            if use_cache:
                cache = self.scratch(self.uid("mc"), [N // 512, 128, KC * 512], BF16)
            for tb in range(self.NTB):
                ts = slice(tb * TB, (tb + 1) * TB)
                P.dma("sp", inb[:], inT[:, ts].rearrange("(c p) t -> p c t", p=128), writes=["inb"])
                for ns in range(N // 512):
                    sl = slab[si % 2]
                    sk = "slab%d" % (si % 2)
                    si += 1
                    if tb == 0 or not use_cache:
                        P.dma("pool", sl[:], w[:, ns * 512:(ns + 1) * 512].rearrange("(c p) n -> p c n", p=128),
                              writes=[sk])
                        if use_cache:
                            P.dma("sp", cache[ns], sl[:].rearrange("p c n -> p (c n)"), reads=[sk],
                                  writes=["c%d" % ns])
                    else:
                        P.dma("pool", sl[:].rearrange("p c n -> p (c n)"), cache[ns], reads=["c%d" % ns],
                              writes=[sk])
                    for tq in range(TB // 128):
                        b = ci % 2
                        ci += 1
                        pt = ps[b]
                        pk = "ps%d" % b
                        for kc in range(KC):
                            P.op("pe", lambda e, pt=pt, sl=sl, kc=kc, tq=tq: e.matmul(
                                pt[:], inb[:, kc, tq * 128:(tq + 1) * 128], sl[:, kc, :],
                                start=(kc == 0), stop=(kc == KC - 1)), reads=[sk, "inb"], writes=[pk])
                        if gelu:
                            self.gelu_ops("m", pt[:], [pk], ost[b][:], "ost%d" % b,
                                          t1[b][:], "t1%d" % b, t2[b][:], "t2%d" % b)
                        else:
                            P.op("act", lambda e, b=b, pt=pt: e.copy(out=ost[b][:], in_=pt[:]),
                                 reads=[pk], writes=["ost%d" % b])
                        r0 = tb * TB + tq * 128
                        P.dma("sp", out[r0:r0 + 128, ns * 512:(ns + 1) * 512], ost[b][:], reads=["ost%d" % b])
            P.emit()

    def stage_ffn(self, hT, experts, F, m, xsrc, xdst, gatesT=None):
        nc, P = self.nc, self.P
        FG = 256
        with ExitStack() as st:
            T = self.mkT(st)
            hb = T("f_h", [128, KC, TB], BF16)
            acc = T("f_acc", [128, KC, TB], F32)
            wgs = [T("f_wg%d" % i, [128, KC, FG], BF16) for i in range(2)]
            wus = [T("f_wu%d" % i, [128, KC, FG], BF16) for i in range(2)]
            wds = [T("f_wd%d" % i, [128, FG // 128, D], BF16) for i in range(2)]
            act_t = [T("f_act%d" % i, [128, FG // 128, TB], BF16) for i in range(2)]
            sg = [T("f_sg%d" % i, [128, TB], F32) for i in range(2)]
            xo = sg
            if gatesT is not None:
                grow = T("f_grow", [1, TB], F32)
                gbc = [T("f_gbc%d" % i, [128, TB], F32) for i in range(1)] * 2
            psg = [self.PS(st, "f_pg%d" % i, [128, TB], F32) for i in range(2)]
            psu = [self.PS(st, "f_pu%d" % i, [128, TB], F32) for i in range(2)]
            psd = [self.PS(st, "f_pd%d" % i, [128, TB], F32) for i in range(3)]
            gi = 0
            ci = 0
            di = 0
            use_cache = self.NTB > 1
            if use_cache:
                sid = self.uid("fc")
                NG = F // FG
                cgs = [self.scratch("%s_g%d" % (sid, i), [NG, 128, KC * FG], BF16) for i in range(len(experts))]
                cus = [self.scratch("%s_u%d" % (sid, i), [NG, 128, KC * FG], BF16) for i in range(len(experts))]
                cds = [self.scratch("%s_d%d" % (sid, i), [NG, 128, (FG // 128) * D], BF16)
                       for i in range(len(experts))]
            for tb in range(self.NTB):
                ts = slice(tb * TB, (tb + 1) * TB)
                P.dma("sp", hb[:], hT[:, ts].rearrange("(c p) t -> p c t", p=128), writes=["hb"])
                first = True
                pend = []
                for ex, (wg, wu, wd) in enumerate(experts):
                    if gatesT is not None:
                        pt = psd[di % 3]
                        pk = "pd%d" % (di % 3)
                        di += 1
                        P.dma("sp", grow[:], gatesT[ex:ex + 1, ts], writes=["grow"])
                        P.op("pe", lambda e, pt=pt: e.matmul(pt[:], self.ones_f[0:1, :], grow[0:1, :],
                                                             start=True, stop=True),
                             reads=["grow"], writes=[pk])
                        P.op("act", lambda e, pt=pt, ex=ex: e.copy(out=gbc[0][:], in_=pt[:]),
                             reads=[pk], writes=["gbc0"])
                    for fg in range(F // FG):
                        b = gi % 2
                        gi += 1
                        fs = slice(fg * FG, (fg + 1) * FG)
                        if tb == 0 or not use_cache:
                            P.dma("pool", wgs[b][:], wg[:, fs].rearrange("(c p) n -> p c n", p=128),
                                  writes=["wg%d" % b])
                            P.dma("pool", wus[b][:], wu[:, fs].rearrange("(c p) n -> p c n", p=128),
                                  writes=["wu%d" % b])
                            P.dma("pool", wds[b][:], wd[fs, :].rearrange("(j p) n -> p j n", p=128), writes=["wd%d" % b])
                            if use_cache:
                                P.dma("sp", cgs[ex][fg], wgs[b][:].rearrange("p c n -> p (c n)"),
                                      reads=["wg%d" % b], writes=["cg%d_%d" % (ex, fg)])
                                P.dma("sp", cus[ex][fg], wus[b][:].rearrange("p c n -> p (c n)"),
                                      reads=["wu%d" % b], writes=["cu%d_%d" % (ex, fg)])
                                P.dma("sp", cds[ex][fg], wds[b][:].rearrange("p j n -> p (j n)"),
                                      reads=["wd%d" % b], writes=["cd%d_%d" % (ex, fg)])
                        else:
                            P.dma("pool", wgs[b][:].rearrange("p c n -> p (c n)"), cgs[ex][fg],
                                  reads=["cg%d_%d" % (ex, fg)], writes=["wg%d" % b])
                            P.dma("pool", wus[b][:].rearrange("p c n -> p (c n)"), cus[ex][fg],
                                  reads=["cu%d_%d" % (ex, fg)], writes=["wu%d" % b])
                            P.dma("pool", wds[b][:].rearrange("p j n -> p (j n)"), cds[ex][fg],
                                  reads=["cd%d_%d" % (ex, fg)], writes=["wd%d" % b])
                        for j in range(FG // 128):
                            cb = ci % 2
                            ci += 1
                            for kc in range(KC):
                                P.op("pe", lambda e, cb=cb, b=b, kc=kc, j=j: e.matmul(
                                    psg[cb][:], wgs[b][:, kc, j * 128:(j + 1) * 128], hb[:, kc, :],
                                    start=(kc == 0), stop=(kc == KC - 1)), reads=["wg%d" % b, "hb"],
                                    writes=["pg%d" % cb])
                            for kc in range(KC):
                                P.op("pe", lambda e, cb=cb, b=b, kc=kc, j=j: e.matmul(
                                    psu[cb][:], wus[b][:, kc, j * 128:(j + 1) * 128], hb[:, kc, :],
                                    start=(kc == 0), stop=(kc == KC - 1)), reads=["wu%d" % b, "hb"],
                                    writes=["pu%d" % cb])
                            P.op("act", lambda e, cb=cb: e.activation(out=sg[cb][:], in_=psg[cb][:], func=AF.Silu),
                                 reads=["pg%d" % cb], writes=["sg%d" % cb])
                            if gatesT is not None:
                                P.op("dve", lambda e, cb=cb, ex=ex: e.tensor_tensor(
                                    out=sg[cb][:], in0=sg[cb][:], in1=gbc[ex % 2][:], op=ALU.mult),
                                    reads=["sg%d" % cb, "gbc0"], writes=["sg%d" % cb])
                            P.op("dve", lambda e, cb=cb, b=b, j=j: e.tensor_tensor(
                                out=act_t[b][:, j, :], in0=psu[cb][:], in1=sg[cb][:], op=ALU.mult),
                                reads=["pu%d" % cb, "sg%d" % cb], writes=["act%d_%d" % (b, j)])
                        pend.append(b)
                        last_group = (ex == len(experts) - 1) and (fg == F // FG - 1)
                        if len(pend) < 2 and not last_group:
                            continue
                        items = [(pb, j) for pb in pend for j in range(FG // 128)]
                        pend = []
                        for nch in range(KC):
                            pt = psd[di % 3]
                            pk = "pd%d" % (di % 3)
                            di += 1
                            for ii, (pb, j) in enumerate(items):
                                P.op("pe", lambda e, pt=pt, pb=pb, j=j, nch=nch, ii=ii, n_=len(items): e.matmul(
                                    pt[:], wds[pb][:, j, nch * 128:(nch + 1) * 128], act_t[pb][:, j, :],
                                    start=(ii == 0), stop=(ii == n_ - 1)),
                                    reads=["wd%d" % pb, "act%d_%d" % (pb, j)], writes=[pk])
                            eng = "dve" if nch % 2 == 0 else "act"
                            if first:
                                if eng == "dve":
                                    P.op("dve", lambda e, pt=pt, nch=nch: e.tensor_copy(out=acc[:, nch, :], in_=pt[:]),
                                         reads=[pk], writes=["acc%d" % nch])
                                else:
                                    P.op("act", lambda e, pt=pt, nch=nch: e.copy(out=acc[:, nch, :], in_=pt[:]),
                                         reads=[pk], writes=["acc%d" % nch])
                            else:
                                P.op("dve", lambda e, pt=pt, nch=nch: e.tensor_tensor(
                                    out=acc[:, nch, :], in0=pt[:], in1=acc[:, nch, :], op=ALU.add),
                                    reads=[pk, "acc%d" % nch], writes=["acc%d" % nch])
                        first = False
                for nch in range(KC):
                    b = nch % 2
                    rows = slice(nch * 128, (nch + 1) * 128)
                    P.dma("sp", xo[b][:], xsrc[rows, ts], writes=["sg%d" % b])
                    P.op("dve", lambda e, b=b, nch=nch: e.scalar_tensor_tensor(
                        out=xo[b][:], in0=acc[:, nch, :], scalar=self.gate(m, nch), in1=xo[b][:],
                        op0=ALU.mult, op1=ALU.add), reads=["acc%d" % nch, "sg%d" % b], writes=["sg%d" % b])
                    P.dma("sp", xdst[rows, ts], xo[b][:], reads=["sg%d" % b])
            P.emit()

    def stage_attn(self, qT, kT, v, outT, n_heads=16):
        nc, P = self.nc, self.P
        S = self.S
        NB = S // 128
        sc = 1.0 / math.sqrt(128.0)
        CH = 1024
        with ExitStack() as st:
            T = self.mkT(st)
            qh = [T("a_q%d" % i, [128, S], BF16) for i in range(2)]
            kh = [T("a_k%d" % i, [128, S], BF16) for i in range(2)]
            vh = [T("a_v%d" % i, [128, NB, 128], BF16) for i in range(2)]
            oh = [T("a_o%d" % i, [128, S], BF16) for i in range(1)] * 2
            SP = [T("a_sp%d" % i, [128, S], F32) for i in range(2)]
            U = [T("a_u%d" % i, [128, S], F32) for i in range(2)]
            CS = [T("a_cs%d" % i, [128, S], F32) for i in range(2)]
            W = [T("a_w%d" % i, [128, S], BF16) for i in range(2)]
            WT = [T("a_wt%d" % i, [128, S], BF16) for i in range(2)]
            ones = T("a_ones", [128, S], F32)
            ntot = [T("a_nt%d" % i, [128, 1], F32) for i in range(2)]
            m01 = T("a_m01", [128, 128], F32)
            m01b = T("a_m01b", [128, 128], BF16)
            psS = [self.PS(st, "a_ps%d" % i, [128, CH], F32) for i in range(2)]
            psT = [self.PS(st, "a_pt%d" % i, [128, 1024], BF16) for i in range(2)]
            psO = [self.PS(st, "a_po%d" % i, [128, 512], F32) for i in range(2)]
            P.op("pool", lambda e: e.memset(ones[:], 1.0), writes=["ones"])
            P.op("pool", lambda e: e.affine_select(
                out=m01[:], in_=ones[:, 0:128], pattern=[[-1, 128]], compare_op=ALU.is_ge, fill=0.0,
                base=-1, channel_multiplier=1), reads=["ones"], writes=["m01"])
            P.op("pool", lambda e: e.tensor_copy(out=m01b[:], in_=m01[:]), reads=["m01"], writes=["m01b"])
            cnt = {"sci": 0, "tci": 0}

            def load_head(h):
                hb = h % 2
                rows = slice(h * 128, (h + 1) * 128)
                P.dma("sp", qh[hb][:], qT[rows, :], writes=["q%d" % hb])
                P.dma("sp", kh[hb][:], kT[rows, :], writes=["k%d" % hb])
                P.dma("sp", vh[hb][:], v[:, rows].rearrange("(kb p) d -> p kb d", p=128), writes=["v%d" % hb])

            def phase_a(h, qb, b):
                hb = h % 2
                nk = (qb + 1) * 128
                dg = slice(qb * 128, nk)
                for c0 in range(0, nk, CH):
                    w_ = min(CH, nk - c0)
                    sb_ = cnt["sci"] % 2
                    cnt["sci"] += 1
                    pS = psS[sb_]
                    sk = "pS%d" % sb_
                    for o_ in range(0, w_, 512):
                        ww = min(512, w_ - o_)
                        P.op("pe", lambda e, pS=pS, o_=o_, ww=ww, c0=c0, hb=hb, qb=qb: e.matmul(
                            pS[:, o_:o_ + ww], qh[hb][:, qb * 128:(qb + 1) * 128],
                            kh[hb][:, c0 + o_:c0 + o_ + ww], start=True, stop=True),
                            reads=["q%d" % hb, "k%d" % hb], writes=[sk])
                    cs_ = slice(c0, c0 + w_)
                    P.op("act", lambda e, pS=pS, w_=w_, cs_=cs_, b=b: e.activation(
                        out=SP[b][:, cs_], in_=pS[:, 0:w_], func=AF.Exp, scale=sc),
                        reads=[sk], writes=["SP%d" % b])
                    P.op("act", lambda e, cs_=cs_, b=b: e.activation(
                        out=SP[b][:, cs_], in_=SP[b][:, cs_], func=AF.Ln, bias=1.0),
                        reads=["SP%d" % b], writes=["SP%d" % b])
                    P.op("dve", lambda e, pS=pS, w_=w_, cs_=cs_, b=b: e.scalar_tensor_tensor(
                        out=U[b][:, cs_], in0=pS[:, 0:w_], scalar=sc, in1=SP[b][:, cs_],
                        op0=ALU.mult, op1=ALU.subtract), reads=[sk, "SP%d" % b], writes=["U%d" % b])
                P.op("dve", lambda e, b=b, dg=dg: e.tensor_tensor(
                    out=SP[b][:, dg], in0=SP[b][:, dg], in1=m01[:], op=ALU.mult),
                    reads=["SP%d" % b, "m01"], writes=["SP%d" % b])
                P.op("dve", lambda e, b=b, nk=nk: e.tensor_tensor_scan(
                    out=CS[b][:, 0:nk], data0=ones[:, 0:nk], data1=SP[b][:, 0:nk], initial=0.0,
                    op0=ALU.mult, op1=ALU.add), reads=["SP%d" % b, "ones"], writes=["CS%d" % b])
                P.op("dve", lambda e, b=b, nk=nk: e.tensor_tensor(
                    out=U[b][:, 0:nk], in0=U[b][:, 0:nk], in1=CS[b][:, 0:nk], op=ALU.add),
                    reads=["U%d" % b, "CS%d" % b], writes=["U%d" % b])
                P.op("dve", lambda e, b=b, nk=nk: e.tensor_scalar(
                    out=ntot[b][:], in0=CS[b][:, nk - 1:nk], scalar1=-1.0, scalar2=None, op0=ALU.mult),
                    reads=["CS%d" % b], writes=["nt%d" % b])

            def phase_b(h, qb, b):
                hb = h % 2
                nk = (qb + 1) * 128
                dg = slice(qb * 128, nk)
                P.op("act", lambda e, b=b, nk=nk: e.activation(
                    out=W[b][:, 0:nk], in_=U[b][:, 0:nk], func=AF.Exp, bias=ntot[b][:, 0:1]),
                    reads=["U%d" % b, "nt%d" % b], writes=["W%d" % b])
                P.op("dve", lambda e, b=b, dg=dg: e.tensor_tensor(
                    out=W[b][:, dg], in0=W[b][:, dg], in1=m01b[:], op=ALU.mult),
                    reads=["W%d" % b, "m01b"], writes=["W%d" % b])
                for k0 in range(0, qb + 1, 8):
                    k1 = min(qb + 1, k0 + 8)
                    tb_ = cnt["tci"] % 2
                    cnt["tci"] += 1
                    for kb in range(k0, k1):
                        P.op("pe", lambda e, tb_=tb_, kb=kb, k0=k0, b=b: e.transpose(
                            psT[tb_][:, (kb - k0) * 128:(kb - k0 + 1) * 128], W[b][:, kb * 128:(kb + 1) * 128],
                            self.ident_bf[:]), reads=["W%d" % b], writes=["pT%d" % tb_])
                    P.op("act", lambda e, tb_=tb_, k0=k0, k1=k1, b=b: e.copy(
                        out=WT[b][:, k0 * 128:k1 * 128], in_=psT[tb_][:, 0:(k1 - k0) * 128]),
                        reads=["pT%d" % tb_], writes=["WT%d_%d" % (b, k0)])
                for kb in range(qb + 1):
                    P.op("pe", lambda e, b=b, kb=kb, hb=hb, qb=qb: e.matmul(
                        psO[b][:, 0:128], vh[hb][:, kb, :], WT[b][:, kb * 128:(kb + 1) * 128],
                        start=(kb == 0), stop=(kb == qb)),
                        reads=["v%d" % hb, "WT%d_%d" % (b, (kb // 8) * 8)], writes=["pO%d" % b])
                P.op("act", lambda e, b=b, hb=hb, qb=qb: e.copy(
                    out=oh[hb][:, qb * 128:(qb + 1) * 128], in_=psO[b][:, 0:128]),
                    reads=["pO%d" % b], writes=["o0"])
                if qb == NB - 1:
                    rows = slice(h * 128, (h + 1) * 128)
                    P.dma("sp", outT[rows, :], oh[hb][:], reads=["o0"])

            sched = [(h, qb) for h in range(n_heads) for qb in range(NB)]
            for it, (h, qb) in enumerate(sched):
                if qb == 0:
                    load_head(h)
                phase_a(h, qb, it % 2)
                if it >= 1:
                    ph, pq = sched[it - 1]
                    phase_b(ph, pq, (it - 1) % 2)
            ph, pq = sched[-1]
            phase_b(ph, pq, (len(sched) - 1) % 2)
            P.emit()

    def stage_gmlp(self, uT, z2g, ln_g, w_s, b_s, outT, n_groups=16):
        nc, P = self.nc, self.P
        S = self.S
        G = n_groups
        with ExitStack() as st:
            T = self.mkT(st)
            lng = T("g_lng", [128, G * 128], F32)
            brow = T("g_brow", [1, G * 128], F32)
            wst = [T("g_ws%d" % i, [128, 128], F32) for i in range(2)]
            wmT = T("g_wmT", [128, G, 128], BF16)
            z2c = [T("g_z2%d" % i, [128, G, 128], F32) for i in range(2)]
            uc = [T("g_u%d" % i, [128, G, 128], BF16) for i in range(2)]
            stats = T("g_stats", [128, G, 6], F32)
            mv = T("g_mv", [128, G, 2], F32)
            rstd = T("g_rstd", [128, G], F32)
            vt = T("g_vt", [128, G, 128], F32)
            vn = [T("g_vn%d" % i, [128, G, 128], BF16) for i in range(2)]
            og = [T("g_og%d" % i, [128, G, 128], BF16) for i in range(2)]
            pw = self.PS(st, "g_pw", [128, 512], F32)
            pm = [self.PS(st, "g_pm%d" % i, [128, 512], F32) for i in range(4)]
            P.dma("sp", lng[:], ln_g.rearrange("g c -> (g c)").partition_broadcast(128), writes=["lng"])
            P.dma("sp", brow[:], b_s.rearrange("g t -> (g t)").partition_broadcast(1), writes=["brow"])
            for g in range(G):
                w = wst[g % 2]
                wk = "ws%d" % (g % 2)
                P.dma("sp", w[:], w_s[g, :, :], writes=[wk])
                P.op("pool", lambda e, w=w: e.affine_select(
                    out=w[:], in_=w[:], pattern=[[-1, 128]], compare_op=ALU.is_ge, fill=0.0, base=0,
                    channel_multiplier=1), reads=[wk], writes=[wk])
                P.op("pe", lambda e, w=w: e.transpose(pw[:, 0:128], w[:], self.ident_f[:]),
                     reads=[wk], writes=["pw"])
                P.op("act", lambda e, g=g: e.copy(out=wmT[:, g, :], in_=pw[:, 0:128]), reads=["pw"],
                     writes=["wmT"])
            pi = 0
            for n in range(S // 128):
                b = n % 2
                ts = slice(n * 128, (n + 1) * 128)
                P.dma("sp", z2c[b][:], z2g[ts, :].rearrange("t (g c) -> t g c", g=G), writes=["z2%d" % b])
                P.dma("sp", uc[b][:], uT[:, ts].rearrange("(g c) t -> c g t", g=G), writes=["u%d" % b])
                for g in range(G):
                    P.op("dve", lambda e, b=b, g=g: e.bn_stats(out=stats[:, g, :], in_=z2c[b][:, g, :]),
                         reads=["z2%d" % b], writes=["st%d" % g])
                    P.op("dve", lambda e, g=g: e.bn_aggr(out=mv[:, g, :], in_=stats[:, g, :]),
                         reads=["st%d" % g], writes=["mv%d" % g])
                mvk = ["mv%d" % g for g in range(G)]
                P.op("dve", lambda e: e.tensor_scalar(out=rstd[:], in0=mv[:, :, 1], scalar1=EPS, scalar2=None,
                                                      op0=ALU.add), reads=mvk, writes=["rstd"])
                P.op("act", lambda e: e.activation(out=rstd[:], in_=rstd[:], func=AF.Sqrt),
                     reads=["rstd"], writes=["rstd"])
                P.op("dve", lambda e: e.reciprocal(out=rstd[:], in_=rstd[:]), reads=["rstd"], writes=["rstd"])
                for g in range(G):
                    P.op("dve", lambda e, b=b, g=g: e.tensor_scalar(
                        out=vt[:, g, :], in0=z2c[b][:, g, :], scalar1=mv[:, g, 0:1], scalar2=rstd[:, g:g + 1],
                        op0=ALU.subtract, op1=ALU.mult), reads=["z2%d" % b, "mv%d" % g, "rstd"], writes=["vt"])
                P.op("dve", lambda e, b=b: e.tensor_tensor(
                    out=vn[b][:].rearrange("p g c -> p (g c)"), in0=vt[:].rearrange("p g c -> p (g c)"),
                    in1=lng[:], op=ALU.mult), reads=["vt", "lng"], writes=["vn%d" % b])
                for g4 in range(G // 4):
                    pb = pi % 4
                    pi += 1
                    for gg in range(4):
                        g = g4 * 4 + gg
                        P.op("pe", lambda e, pb=pb, gg=gg, g=g, b=b: e.matmul(
                            pm[pb][:, gg * 128:(gg + 1) * 128], vn[b][:, g, :], wmT[:, g, :],
                            start=True, stop=False), reads=["vn%d" % b, "wmT"], writes=["pm%d" % pb])
                        P.op("pe", lambda e, pb=pb, gg=gg, g=g: e.matmul(
                            pm[pb][:, gg * 128:(gg + 1) * 128], self.ones_f[0:1, :], brow[0:1, g * 128:(g + 1) * 128],
                            start=False, stop=True), reads=["brow"], writes=["pm%d" % pb])
                    P.op("dve", lambda e, pb=pb, g4=g4, b=b: e.tensor_tensor(
                        out=og[b][:, g4 * 4:(g4 + 1) * 4, :].rearrange("p g c -> p (g c)"), in0=pm[pb][:],
                        in1=uc[b][:, g4 * 4:(g4 + 1) * 4, :].rearrange("p g c -> p (g c)"), op=ALU.mult),
                        reads=["pm%d" % pb, "u%d" % b], writes=["og%d_%d" % (b, g4)])
                P.dma("sp", outT[:, ts].rearrange("(g c) t -> c g t", g=G), og[b][:],
                      reads=["og%d_%d" % (b, g4) for g4 in range(G // 4)])
            P.emit()

    def stage_ssm_prep(self, lam_re, lam_im, log_dt, b_re, b_im, c_re, c_im, MB, MP, NL, G=256):
        nc, P = self.nc, self.P
        GB = 32
        PI2 = math.pi / 2
        with ExitStack() as st:
            T = self.mkT(st)
            NS = 96
            PW = T("sp_pw", [128, NS, G], F32)
            cnt = [0]

            def new():
                i = cnt[0]
                cnt[0] += 1
                assert i < NS
                return (PW[:, i, :], "pw%d" % i)

            def TT(o, a, b, op, eng="dve"):
                P.op(eng, lambda e: e.tensor_tensor(out=o[0], in0=a[0], in1=b[0], op=op),
                     reads=[a[1], b[1]], writes=[o[1]])

            def TS(o, a, s1, op0, s2=None, op1=None):
                if op1 is None:
                    P.op("dve", lambda e: e.tensor_scalar(out=o[0], in0=a[0], scalar1=s1, scalar2=None, op0=op0),
                         reads=[a[1]], writes=[o[1]])
                else:
                    P.op("dve", lambda e: e.tensor_scalar(out=o[0], in0=a[0], scalar1=s1, scalar2=s2, op0=op0,
                                                          op1=op1), reads=[a[1]], writes=[o[1]])

            def STT(o, a, s, b, op0, op1):
                P.op("dve", lambda e: e.scalar_tensor_tensor(out=o[0], in0=a[0], scalar=s, in1=b[0], op0=op0,
                                                             op1=op1), reads=[a[1], b[1]], writes=[o[1]])

            def ACTF(o, a, func, scale=1.0, bias=0.0):
                P.op("act", lambda e: e.activation(out=o[0], in_=a[0], func=func, scale=scale, bias=bias),
                     reads=[a[1]], writes=[o[1]])

            t1, t2 = new(), new()

            def cmul(dr, di, ar, ai, br, bi):
                TT(t1, ar, br, ALU.mult)
                TT(t2, ai, bi, ALU.mult)
                TT(dr, t1, t2, ALU.subtract)
                TT(t1, ar, bi, ALU.mult)
                TT(t2, ai, br, ALU.mult)
                TT(di, t1, t2, ALU.add)

            def csq(dr, di, ar, ai):
                TT(t1, ar, ar, ALU.mult)
                TT(t2, ai, ai, ALU.mult)
                STT(di, ar, 2.0, ai, ALU.mult, ALU.mult)
                TT(dr, t1, t2, ALU.subtract)

            nat = T("sp_nat", [128, 128], F32)
            pst = self.PS(st, "sp_pst", [128, 512], F32)
            psA = [self.PS(st, "sp_psA%d" % i, [128, 512], F32) for i in range(2)]
            lrT, liT, ldt = new(), new(), new()
            for arr, dst in ((lam_re, lrT), (lam_im, liT)):
                for gt in range(G // 128):
                    P.dma("sp", nat[:, 0:64], arr[gt * 128:(gt + 1) * 128, :], writes=["nat"])
                    P.dma("sp", nat[:, 64:128], arr[gt * 128:(gt + 1) * 128, :], writes=["nat"])
                    P.op("pe", lambda e: e.transpose(pst[:, 0:128], nat[:], self.ident_f[:]), reads=["nat"],
                         writes=["pst"])
                    P.op("act", lambda e, dst=dst, gt=gt: e.copy(out=dst[0][:, gt * 128:(gt + 1) * 128],
                                                                 in_=pst[:, 0:128]), reads=["pst"], writes=[dst[1]])
            P.dma("sp", ldt[0], log_dt.rearrange("o g -> (o g)").partition_broadcast(128), writes=[ldt[1]])
            dt, lr, x1, th = new(), new(), new(), new()
            ACTF(dt, ldt, AF.Exp)
            TS(lr, lrT, -1e-4, ALU.min)
            TT(x1, lr, dt, ALU.mult)
            TT(th, liT, dt, ALU.mult)
            mag, sn, cs = new(), new(), new()
            ACTF(mag, x1, AF.Exp, scale=1.0 / 32)
            ACTF(sn, th, AF.Sin, scale=1.0 / 32)
            ACTF(cs, th, AF.Sin, scale=1.0 / 32, bias=PI2)
            cur = (new(), new())
            TT(cur[0], mag, cs, ALU.mult)
            TT(cur[1], mag, sn, ALU.mult)
            for _ in range(5):
                nx = (new(), new())
                csq(nx[0], nx[1], cur[0], cur[1])
                cur = nx
            pw = {1: cur}
            for i in range(2, 9):
                pw[i] = (new(), new())
            csq(*pw[2], *pw[1])
            cmul(*pw[3], *pw[2], *pw[1])
            csq(*pw[4], *pw[2])
            cmul(*pw[5], *pw[4], *pw[1])
            csq(*pw[6], *pw[3])
            cmul(*pw[7], *pw[6], *pw[1])
            csq(*pw[8], *pw[4])
            big = [pw[8]]
            for k in range(1, NL):
                nx = (new(), new())
                csq(nx[0], nx[1], big[-1][0], big[-1][1])
                big.append(nx)
            ipw = {}
            n2 = new()
            for s in range(1, 8):
                ipw[s] = (new(), new())
                TT(t1, pw[s][0], pw[s][0], ALU.mult)
                TT(t2, pw[s][1], pw[s][1], ALU.mult)
                TT(n2, t1, t2, ALU.add)
                P.op("dve", lambda e: e.reciprocal(out=n2[0], in_=n2[0]), reads=[n2[1]], writes=[n2[1]])
                TT(ipw[s][0], pw[s][0], n2, ALU.mult)
                STT(ipw[s][1], pw[s][1], -1.0, n2, ALU.mult, ALU.mult)
            nr, den, fre, fim = new(), new(), new(), new()
            are, aim = pw[1]
            TS(nr, are, -1.0, ALU.add)
            TT(t1, lr, lr, ALU.mult)
            TT(t2, liT, liT, ALU.mult)
            TT(den, t1, t2, ALU.add)
            P.op("dve", lambda e: e.reciprocal(out=den[0], in_=den[0]), reads=[den[1]], writes=[den[1]])
            TT(t1, nr, lr, ALU.mult)
            TT(t2, aim, liT, ALU.mult)
            TT(t1, t1, t2, ALU.add)
            TT(fre, t1, den, ALU.mult)
            TT(t1, aim, lr, ALU.mult)
            TT(t2, nr, liT, ALU.mult)
            TT(t1, t1, t2, ALU.subtract)
            TT(fim, t1, den, ALU.mult)
            zs = [pw[7]] + big
            sims = []
            for z in zs:
                sm = new()
                P.op("dve", lambda e, sm=sm, z=z: e.tensor_copy(out=sm[0][0:64, :], in_=z[1][0][0:64, :]),
                     reads=[z[1][1]], writes=[sm[1]])
                P.op("dve", lambda e, sm=sm, z=z: e.tensor_scalar(out=sm[0][64:128, :], in0=z[1][0][64:128, :],
                                                                  scalar1=-1.0, scalar2=None, op0=ALU.mult),
                     reads=[z[1][1]], writes=[sm[1]])
                sims.append(sm)
            jsw = T("sp_jsw", [128, 128], F32)
            P.op("pool", lambda e: e.memset(jsw[:], 0.0), writes=["jsw"])
            P.op("pool", lambda e: e.tensor_copy(out=jsw[0:64, 64:128], in_=self.ident_f[0:64, 0:64]),
                 reads=["jsw"], writes=["jsw"])
            P.op("pool", lambda e: e.tensor_copy(out=jsw[64:128, 0:64], in_=self.ident_f[64:128, 64:128]),
                 reads=["jsw"], writes=["jsw"])
            mT0 = T("sp_mT0", [128, 8, 16], F32)
            P.op("pool", lambda e: e.memset(mT0[:], 1.0), writes=["mT0"])
            P.op("pool", lambda e: e.affine_select(
                out=mT0[:], in_=mT0[:], pattern=[[16, 8], [0, 16]], compare_op=ALU.is_ge, fill=0.0, base=15,
                channel_multiplier=-1), reads=["mT0"], writes=["mT0"])

            Bre = T("sp_Bre", [128, GB, 16], F32)
            Bim = T("sp_Bim", [128, GB, 16], F32)
            Wre = T("sp_Wre", [128, GB, 16], F32)
            Wim = T("sp_Wim", [128, GB, 16], F32)
            CTre = T("sp_CTre", [128, GB, 16], F32)
            CTim = T("sp_CTim", [128, GB, 16], F32)
            u1 = T("sp_u1", [128, GB, 16], F32)
            u2 = T("sp_u2", [128, GB, 16], F32)
            Xs = T("sp_Xs", [128, GB, 8, 16], F32)
            Ys = T("sp_Ys", [128, GB, 9, 16], F32)
            natc = T("sp_natc", [128, 128], F32)
            Mt = [T("sp_Mt%d" % i, [128, 128], F32) for i in range(2)]
            stB = [T("sp_stB%d" % i, [128, 3, 128], BF16) for i in range(2)]
            stP = [T("sp_stP%d" % i, [128, NL, 128], F32) for i in range(2)]
            c_re2 = c_re.rearrange("g c p -> (g c) p")
            c_im2 = c_im.rearrange("g c p -> (g c) p")

            def bc(slot, g0):
                return slot[0][:, g0:g0 + GB].unsqueeze(2).broadcast_to([128, GB, 16])

            def hop(eng, fn, reads, writes):
                P.op(eng, fn, reads=reads, writes=writes)

            for gb in range(G // GB):
                g0 = gb * GB
                for half in range(2):
                    hs = slice(half * 64, (half + 1) * 64)
                    P.dma("sp", Bre[hs], b_re[g0:g0 + GB].rearrange("g p c -> p g c"), writes=["Bre"])
                    P.dma("sp", Bim[hs], b_im[g0:g0 + GB].rearrange("g p c -> p g c"), writes=["Bim"])
                fr, fi = bc(fre, g0), bc(fim, g0)
                hop("dve", lambda e, fr=fr: e.tensor_tensor(out=u1[:], in0=Bre[:], in1=fr, op=ALU.mult),
                    ["Bre", fre[1]], ["u1"])
                hop("dve", lambda e, fi=fi: e.tensor_tensor(out=u2[:], in0=Bim[:], in1=fi, op=ALU.mult),
                    ["Bim", fim[1]], ["u2"])
                hop("dve", lambda e: e.tensor_tensor(out=Wre[:], in0=u1[:], in1=u2[:], op=ALU.subtract),
                    ["u1", "u2"], ["Wre"])
                hop("dve", lambda e, fr=fr: e.tensor_tensor(out=u1[:], in0=Bim[:], in1=fr, op=ALU.mult),
                    ["Bim", fre[1]], ["u1"])
                hop("dve", lambda e, fi=fi: e.tensor_tensor(out=u2[:], in0=Bre[:], in1=fi, op=ALU.mult),
                    ["Bre", fim[1]], ["u2"])
                hop("dve", lambda e: e.tensor_tensor(out=Wim[:], in0=u1[:], in1=u2[:], op=ALU.add),
                    ["u1", "u2"], ["Wim"])
                top, bot = slice(0, 64), slice(64, 128)
                hop("dve", lambda e: e.tensor_copy(out=Xs[top, :, 0, :], in_=Wre[top]), ["Wre"], ["Xs"])
                hop("dve", lambda e: e.tensor_copy(out=Xs[bot, :, 0, :], in_=Wim[bot]), ["Wim"], ["Xs"])
                for s in range(1, 8):
                    ir, ii = bc(ipw[s][0], g0), bc(ipw[s][1], g0)
                    rk = [ipw[s][0][1], ipw[s][1][1]]
                    hop("dve", lambda e, ir=ir: e.tensor_tensor(out=u1[top], in0=Wre[top], in1=ir[top], op=ALU.mult),
                        ["Wre"] + rk, ["u1"])
                    hop("dve", lambda e, ii=ii: e.tensor_tensor(out=u2[top], in0=Wim[top], in1=ii[top], op=ALU.mult),
                        ["Wim"] + rk, ["u2"])
                    hop("dve", lambda e, s=s: e.tensor_tensor(out=Xs[top, :, s, :], in0=u1[top], in1=u2[top],
                                                              op=ALU.subtract), ["u1", "u2"], ["Xs"])
                    hop("dve", lambda e, ir=ir: e.tensor_tensor(out=u1[bot], in0=Wim[bot], in1=ir[bot], op=ALU.mult),
                        ["Wim"] + rk, ["u1"])
                    hop("dve", lambda e, ii=ii: e.tensor_tensor(out=u2[bot], in0=Wre[bot], in1=ii[bot], op=ALU.mult),
                        ["Wre"] + rk, ["u2"])
                    hop("dve", lambda e, s=s: e.tensor_tensor(out=Xs[bot, :, s, :], in0=u1[bot], in1=u2[bot],
                                                              op=ALU.add), ["u1", "u2"], ["Xs"])
                for arr2, dstT, dk in ((c_re2, CTre, "CTre"), (c_im2, CTim, "CTim")):
                    for i in range(GB * 16 // 128):
                        r0 = g0 * 16 + i * 128
                        P.dma("sp", natc[:, 0:64], arr2[r0:r0 + 128, :], writes=["natc"])
                        P.dma("sp", natc[:, 64:128], arr2[r0:r0 + 128, :], writes=["natc"])
                        P.op("pe", lambda e: e.transpose(pst[:, 128:256], natc[:], self.ident_f[:]),
                             reads=["natc"], writes=["pst2"])
                        P.op("act", lambda e, dstT=dstT, i=i: e.copy(
                            out=dstT[:, i * 8:(i + 1) * 8, :], in_=pst[:, 128:256].rearrange("p (g c) -> p g c", c=16)),
                            reads=["pst2"], writes=[dk])
                hop("dve", lambda e: e.tensor_copy(out=Ys[top, :, 0, :], in_=CTre[top]), ["CTre"], ["Ys"])
                hop("dve", lambda e: e.tensor_scalar(out=Ys[bot, :, 0, :], in0=CTim[bot], scalar1=-1.0, scalar2=None,
                                                     op0=ALU.mult), ["CTim"], ["Ys"])
                for t in range(1, 9):
                    pr, pi_ = bc(pw[t][0], g0), bc(pw[t][1], g0)
                    rk = [pw[t][0][1], pw[t][1][1]]
                    hop("dve", lambda e, pr=pr: e.tensor_tensor(out=u1[top], in0=CTre[top], in1=pr[top], op=ALU.mult),
                        ["CTre"] + rk, ["u1"])
                    hop("dve", lambda e, pi_=pi_: e.tensor_tensor(out=u2[top], in0=CTim[top], in1=pi_[top],
                                                                  op=ALU.mult), ["CTim"] + rk, ["u2"])
                    hop("dve", lambda e, t=t: e.tensor_tensor(out=Ys[top, :, t, :], in0=u1[top], in1=u2[top],
                                                              op=ALU.subtract), ["u1", "u2"], ["Ys"])
                    hop("dve", lambda e, pr=pr: e.tensor_tensor(out=u1[bot], in0=CTim[bot], in1=pr[bot], op=ALU.mult),
                        ["CTim"] + rk, ["u1"])
                    hop("dve", lambda e, pi_=pi_: e.tensor_tensor(out=u2[bot], in0=CTre[bot], in1=pi_[bot],
                                                                  op=ALU.mult), ["CTre"] + rk, ["u2"])
                    hop("dve", lambda e, t=t: e.scalar_tensor_tensor(
                        out=Ys[bot, :, t, :], in0=u1[bot], scalar=-1.0, in1=u2[bot], op0=ALU.mult, op1=ALU.subtract),
                        ["u1", "u2"], ["Ys"])
                for gl in range(GB):
                    g = g0 + gl
                    b = g % 2
                    Xg = Xs[:, gl, :, :].rearrange("p s c -> p (s c)")
                    Y0 = Ys[:, gl, 0:8, :].rearrange("p t c -> p (t c)")
                    Y1 = Ys[:, gl, 1:9, :].rearrange("p t c -> p (t c)")
                    pa = psA[b]
                    P.op("pe", lambda e, pa=pa, Xg=Xg, Y0=Y0: e.matmul(pa[:, 0:128], Xg, Y0, start=True, stop=True),
                         reads=["Xs", "Ys"], writes=["psA%d" % b])
                    P.op("dve", lambda e, pa=pa, b=b: e.tensor_tensor(
                        out=stB[b][:, 0, :], in0=pa[:, 0:128], in1=mT0[:].rearrange("p t c -> p (t c)"),
                        op=ALU.mult), reads=["psA%d" % b, "mT0"], writes=["stB%d" % b])
                    P.op("dve", lambda e, b=b, g=g: e.tensor_scalar(
                        out=Mt[b][:], in0=self.ident_f[:], scalar1=zs[0][0][0][:, g:g + 1], scalar2=None,
                        op0=ALU.mult), reads=[zs[0][0][1]], writes=["Mt%d" % b])
                    P.op("dve", lambda e, b=b, g=g: e.scalar_tensor_tensor(
                        out=Mt[b][:], in0=jsw[:], scalar=sims[0][0][:, g:g + 1], in1=Mt[b][:],
                        op0=ALU.mult, op1=ALU.add), reads=["jsw", sims[0][1], "Mt%d" % b], writes=["Mt%d" % b])
                    P.op("pe", lambda e, pa=pa, Xg=Xg, b=b: e.matmul(pa[:, 128:256], Xg, Mt[b][:], start=True,
                                                                     stop=True),
                         reads=["Xs", "Mt%d" % b], writes=["psA%d" % b])
                    P.op("act", lambda e, pa=pa, b=b: e.copy(out=stB[b][:, 1, :], in_=pa[:, 128:256]),
                         reads=["psA%d" % b], writes=["stB%d" % b])
                    P.op("act", lambda e, b=b, Y1=Y1: e.copy(out=stB[b][:, 2, :], in_=Y1),
                         reads=["Ys"], writes=["stB%d" % b])
                    P.dma("sp", MB[g], stB[b][:], reads=["stB%d" % b])
                    for k in range(NL):
                        z = zs[1 + k]
                        sm = sims[1 + k]
                        P.op("dve", lambda e, b=b, g=g, k=k, z=z: e.tensor_scalar(
                            out=stP[b][:, k, :], in0=self.ident_f[:], scalar1=z[0][0][:, g:g + 1], scalar2=None,
                            op0=ALU.mult), reads=[z[0][1]], writes=["stP%d" % b])
                        P.op("dve", lambda e, b=b, g=g, k=k, sm=sm: e.scalar_tensor_tensor(
                            out=stP[b][:, k, :], in0=jsw[:], scalar=sm[0][:, g:g + 1], in1=stP[b][:, k, :],
                            op0=ALU.mult, op1=ALU.add), reads=["jsw", sm[1], "stP%d" % b], writes=["stP%d" % b])
                    P.dma("sp", MP[g], stP[b][:], reads=["stP%d" % b])
            P.emit()

    def stage_ssm_run(self, utok, MB, MP, d_ap, yT, NL, G=256):
        nc, P = self.nc, self.P
        S = self.S
        NJ = S // 8
        JP = min(128, NJ)
        NJT = NJ // JP
        GBK = 32
        NW = 4
        NB2 = 2 * NW
        with ExitStack() as st:
            T = self.mkT(st)
            Ust = [T("r_Ust%d" % i, [128, 8, 512], BF16) for i in range(2)]
            U2 = T("r_U2", [128, NJT, GBK, 8, 16], BF16)
            Yblk = T("r_Y", [128, NJT, 8, 512], BF16)
            dbc = T("r_d", [128, 512], F32)
            mb = [T("r_mb%d" % i, [128, 3, 128], BF16) for i in range(NB2)]
            mp = [T("r_mp%d" % i, [128, NL, 128], F32) for i in range(NB2)]
            Ug = [T("r_Ug%d" % i, [128, NJ], BF16) for i in range(NB2)]
            Sg = [T("r_Sg%d" % i, [128, NJ], F32) for i in range(NB2)]
            Sp = [T("r_Sp%d" % i, [128, NJ], BF16) for i in range(NB2)]
            Yg = [T("r_Yg%d" % i, [128, NJ], BF16) for i in range(NB2)]
            tmp = T("r_tmp", [128, 4, 512], F32)
            g1 = T("r_g1", [128, 4, 512], F32)
            g2 = T("r_g2", [128, 4, 512], F32)
            yact = [T("r_ya%d" % i, [128, 4, 512], BF16) for i in range(2)]
            yTb = [T("r_yT%d" % i, [128, 4, JP * 8], BF16) for i in range(1)] * 2
            psU = self.PS(st, "r_psU", [128, 1024], BF16)
            ring = [self.PS(st, "r_ring%d" % i, [128, 512], F32) for i in range(NW)]
            psYT = self.PS(st, "r_psYT", [128, 1024], BF16)
            psT2 = self.PS(st, "r_psT2", [128, 1024], BF16)
            for i in range(NB2):
                P.op("pool", lambda e, i=i: e.memset(Sp[i][:], 0.0), writes=["Sp%d" % i])
            wv = 0
            ti = 0
            for blk in range(G // GBK):
                ch0 = blk * 512
                for jt in range(NJT):
                    ub = jt % 2
                    P.dma("pool", Ust[ub][0:JP], utok[jt * JP * 8:(jt + 1) * JP * 8, ch0:ch0 + 512].rearrange(
                        "(jp s) c -> jp s c", s=8), writes=["Ust%d" % ub])
                    P.op("dve", lambda e, ub=ub, jt=jt: e.tensor_copy(
                        out=U2[0:JP, jt], in_=Ust[ub][0:JP].rearrange("p s (g c) -> p g s c", c=16)),
                        reads=["Ust%d" % ub], writes=["U2"])
                P.dma("sp", dbc[0:JP], d_ap[0, ch0:ch0 + 512].partition_broadcast(JP), writes=["dbc"])
                cut = getattr(self, "cut", 9)
                for w0 in range(0, GBK, NW):
                    if cut < 2:
                        break
                    sl = [(wv % 2) * NW + i for i in range(NW)]
                    wv += 1
                    gls = [w0 + i for i in range(NW)]
                    for i in range(NW):
                        g = blk * GBK + gls[i]
                        P.dma("sp", mb[sl[i]][:], MB[g], writes=["mb%d" % sl[i]])
                        P.dma("sp", mp[sl[i]][:], MP[g], writes=["mp%d" % sl[i]])
                    if cut < 2.5:
                        continue
                    for i in range(NW):
                        s_, gl = sl[i], gls[i]
                        hf = 0
                        for jt in range(NJT):
                            P.op("pe", lambda e, jt=jt, gl=gl, hf=hf: e.transpose(
                                psU[:, hf * 512 + jt * JP: hf * 512 + (jt + 1) * JP],
                                U2[0:JP, jt, gl].rearrange("p s c -> p (s c)"), self.ident_bf[0:JP, 0:JP]),
                                reads=["U2"], writes=["psU%d" % hf])
                        if cut < 2.7:
                            continue
                        P.op("act", lambda e, s_=s_, hf=hf: e.copy(out=Ug[s_][:], in_=psU[:, hf * 512:hf * 512 + NJ]),
                             reads=["psU%d" % hf], writes=["Ug%d" % s_])
                    if cut < 3:
                        continue
                    for i in range(NW):
                        s_ = sl[i]
                        P.op("pe", lambda e, i=i, s_=s_: e.matmul(ring[i][:, 0:NJ], mb[s_][:, 1, :], Ug[s_][:],
                                                                  start=True, stop=True),
                             reads=["mb%d" % s_, "Ug%d" % s_], writes=["ring%d" % i])
                    for i in range(NW):
                        s_ = sl[i]
                        P.op("dve", lambda e, i=i, s_=s_: e.tensor_copy(out=Sg[s_][:], in_=ring[i][:, 0:NJ]),
                             reads=["ring%d" % i], writes=["Sg%d" % s_])
                    for k in range(NL):
                        sh = 1 << k
                        for i in range(NW):
                            s_ = sl[i]
                            P.op("pe", lambda e, i=i, s_=s_, k=k, sh=sh: e.matmul(
                                ring[i][:, 0:NJ - sh], mp[s_][:, k, :], Sg[s_][:, 0:NJ - sh], start=True, stop=True),
                                reads=["mp%d" % s_, "Sg%d" % s_], writes=["ring%d" % i])
                        for i in range(NW):
                            s_ = sl[i]
                            P.op("dve", lambda e, i=i, s_=s_, sh=sh: e.tensor_tensor(
                                out=Sg[s_][:, sh:NJ], in0=ring[i][:, 0:NJ - sh], in1=Sg[s_][:, sh:NJ], op=ALU.add),
                                reads=["ring%d" % i, "Sg%d" % s_], writes=["Sg%d" % s_])
                    if cut < 4:
                        continue
                    for i in range(NW):
                        s_ = sl[i]
                        P.op("act", lambda e, s_=s_: e.copy(out=Sp[s_][:, 1:NJ], in_=Sg[s_][:, 0:NJ - 1]),
                             reads=["Sg%d" % s_], writes=["Sp%d" % s_])
                    for i in range(NW):
                        s_ = sl[i]
                        P.op("pe", lambda e, i=i, s_=s_: e.matmul(ring[i][:, 0:NJ], mb[s_][:, 0, :], Ug[s_][:],
                                                                  start=True, stop=False),
                             reads=["mb%d" % s_, "Ug%d" % s_], writes=["ring%d" % i])
                        P.op("pe", lambda e, i=i, s_=s_: e.matmul(ring[i][:, 0:NJ], mb[s_][:, 2, :], Sp[s_][:],
                                                                  start=False, stop=True),
                             reads=["mb%d" % s_, "Sp%d" % s_], writes=["ring%d" % i])
                        P.op("act", lambda e, i=i, s_=s_: e.copy(out=Yg[s_][:], in_=ring[i][:, 0:NJ]),
                             reads=["ring%d" % i], writes=["Yg%d" % s_])
                    for i in range(NW):
                        s_, gl = sl[i], gls[i]
                        hf = 0
                        for jt in range(NJT):
                            P.op("pe", lambda e, jt=jt, s_=s_, hf=hf: e.transpose(
                                psYT[0:JP, hf * 512 + jt * 128: hf * 512 + (jt + 1) * 128],
                                Yg[s_][:, jt * JP:(jt + 1) * JP], self.ident_bf[:]),
                                reads=["Yg%d" % s_], writes=["psYT%d" % hf])
                        P.op("dve", lambda e, gl=gl, hf=hf: e.tensor_copy(
                            out=Yblk[0:JP, :, :, gl * 16:(gl + 1) * 16],
                            in_=psYT[0:JP, hf * 512:hf * 512 + NJT * 128].rearrange("p (j t c) -> p j t c", j=NJT, t=8)),
                            reads=["psYT%d" % hf], writes=["Yblk"])
                for jt in range(NJT):
                    if cut < 5:
                        break
                    for th in range(2):
                        tsl = slice(th * 4, (th + 1) * 4)
                        yb = ti % 2
                        ti += 1
                        P.op("dve", lambda e, jt=jt, tsl=tsl: e.tensor_tensor(
                            out=tmp[0:JP].rearrange("p t (g c) -> p g t c", c=16), in0=U2[0:JP, jt, :, tsl, :],
                            in1=dbc[0:JP].rearrange("p (g c) -> p g c", c=16).unsqueeze(2).broadcast_to(
                                [JP, GBK, 4, 16]), op=ALU.mult),
                            reads=["U2", "dbc"], writes=["tmp"])
                        P.op("dve", lambda e, jt=jt, tsl=tsl: e.tensor_tensor(
                            out=tmp[0:JP], in0=tmp[0:JP], in1=Yblk[0:JP, jt, tsl, :], op=ALU.add),
                            reads=["tmp", "Yblk"], writes=["tmp"])
                        self.gelu_ops("r", tmp[0:JP], ["tmp"], yact[yb][0:JP], "ya%d" % yb,
                                      g1[0:JP], "g1", g2[0:JP], "g2")
                        for cb in range(4):
                            hf = 0
                            for tt in range(4):
                                P.op("pe", lambda e, yb=yb, tt=tt, cb=cb, hf=hf: e.transpose(
                                    psT2[:, hf * 512 + tt * JP: hf * 512 + (tt + 1) * JP],
                                    yact[yb][0:JP, tt, cb * 128:(cb + 1) * 128], self.ident_bf[0:JP, 0:JP]),
                                    reads=["ya%d" % yb], writes=["psT2%d" % hf])
                            eng = "act" if cb % 2 == 0 else "dve"
                            src = psT2[:, hf * 512:hf * 512 + 4 * JP].rearrange("p (t j) -> p t j", t=4)
                            yb_ = jt % 2
                            dst = yTb[yb_][:, cb, :].rearrange("p (j t) -> p j t", t=8)[:, :, tsl].rearrange(
                                "p j t -> p t j")
                            if eng == "act":
                                P.op("act", lambda e, src=src, dst=dst: e.copy(out=dst, in_=src),
                                     reads=["psT2%d" % hf], writes=["yTb0"])
                            else:
                                P.op("dve", lambda e, src=src, dst=dst: e.tensor_copy(out=dst, in_=src),
                                     reads=["psT2%d" % hf], writes=["yTb0"])
                    P.dma("sp", yT[ch0:ch0 + 512, jt * JP * 8:(jt + 1) * JP * 8].rearrange("(cb p) t -> p cb t", p=128),
                          yTb[jt % 2][:], reads=["yTb0"])
            P.emit()


W_SPECS = [
    ("mix0_norm_g", [1, D]), ("mix0_ada_w", [D, 3 * D]), ("mix0_ada_b", [1, 3 * D]),
    ("mix0_w_in", [D, 10240]), ("gm_ln_g", [16, 128]), ("gm_w_s", [16, 128, 128]), ("gm_b_s", [16, 128]),
    ("mix0_w_out", [D, D]),
    ("ffn0_norm_g", [1, D]), ("ffn0_ada_w", [D, 3 * D]), ("ffn0_ada_b", [1, 3 * D]),
    ("ffn0_w_gate", [D, FFN_DENSE]), ("ffn0_w_up", [D, FFN_DENSE]), ("ffn0_w_down", [FFN_DENSE, D]),
    ("mix1_norm_g", [1, D]), ("mix1_ada_w", [D, 3 * D]), ("mix1_ada_b", [1, 3 * D]),
    ("ssm_w_in", [D, D]), ("ssm_lam_re", [256, 64]), ("ssm_lam_im", [256, 64]), ("ssm_log_dt", [1, 256]),
    ("ssm_b_re", [256, 64, 16]), ("ssm_b_im", [256, 64, 16]), ("ssm_c_re", [256, 16, 64]),
    ("ssm_c_im", [256, 16, 64]), ("ssm_d", [1, D]), ("glu_w_a", [D, D]), ("glu_w_b", [D, D]),
    ("moe_norm_g", [1, D]), ("moe_ada_w", [D, 3 * D]), ("moe_ada_b", [1, 3 * D]),
    ("moe_w_router", [D, N_EXP]), ("moe_w_gate", [N_EXP, D, FFN_EXP]), ("moe_w_up", [N_EXP, D, FFN_EXP]),
    ("moe_w_down", [N_EXP, FFN_EXP, D]), ("final_norm_g", [1, D]),
]


def build_model(S):
    B = Builder(S, None)
    NL = int(round(math.log2(S // 8)))
    xT = B.ext_in("xT", [D, S])
    c = B.ext_in("c", [1, D])
    w = {n: B.ext_in(n, shp) for n, shp in W_SPECS}
    outT = B.ext_out("outT", [D, S])
    hT = B.scratch("hT", [D, S], BF16)
    xres = B.scratch("xres", [D, S], F32)
    qT = B.scratch("qT", [2048, S], BF16)
    kT = B.scratch("kT", [2048, S], BF16)
    uT = B.scratch("uT", [2048, S], BF16)
    v = B.scratch("v", [S, 2048], BF16)
    z2g = B.scratch("z2g", [S, 2048], F32)
    mixT = B.scratch("mixT", [D, S], BF16)
    utok = B.scratch("utok", [S, D], BF16)
    yT = B.scratch("yT", [D, S], BF16)
    MB = B.scratch("MB", [256, 128, 3, 128], BF16)
    MP = B.scratch("MP", [256, 128, NL, 128], F32)
    gatesT = B.scratch("gatesT", [N_EXP, S], F32)
    B.setup_consts()
    B.stage_ada(c, [(w[p + "_ada_w"], w[p + "_ada_b"], w[p + "_norm_g"]) for p in ("mix0", "ffn0", "mix1", "moe")])
    B.stage_ssm_prep(w["ssm_lam_re"], w["ssm_lam_im"], w["ssm_log_dt"], w["ssm_b_re"], w["ssm_b_im"],
                     w["ssm_c_re"], w["ssm_c_im"], MB, MP, NL)
    win = w["mix0_w_in"]
    B.stage_norm(xT, hT, 0)
    B.stage_linear_fm(hT, D, [dict(w=[win[:, 0:2048]], N=2048, epi="bf16", out=qT),
                              dict(w=[win[:, 2048:4096]], N=2048, epi="bf16", out=kT),
                              dict(w=[win[:, 6144:8192]], N=2048, epi="gelu", out=uT)])
    B.stage_linear_tm(hT, win[:, 4096:6144], 2048, v, BF16)
    B.stage_linear_tm(hT, win[:, 8192:10240], 2048, z2g, F32, gelu=True)
    B.stage_attn(qT, kT, v, mixT[0:2048, :])
    B.stage_gmlp(uT, z2g, w["gm_ln_g"], w["gm_w_s"], w["gm_b_s"], mixT[2048:4096, :])
    B.stage_linear_fm(mixT, D, [dict(w=[w["mix0_w_out"]], N=D, epi="resid", m=0, xsrc=xT, xdst=xres)])
    B.stage_norm(xres, hT, 1)
    B.stage_ffn(hT, [(w["ffn0_w_gate"], w["ffn0_w_up"], w["ffn0_w_down"])], FFN_DENSE, 1, xres, xres)
    B.stage_norm(xres, hT, 2)
    B.stage_linear_tm(hT, w["ssm_w_in"], D, utok, BF16)
    B.stage_ssm_run(utok, MB, MP, w["ssm_d"], yT, NL)
    B.stage_linear_fm(yT, D, [dict(w=[w["glu_w_a"], w["glu_w_b"]], N=D, epi="glu", m=2, xsrc=xres, xdst=xres)])
    B.stage_norm(xres, hT, 3, router=(w["moe_w_router"], gatesT))
    B.stage_ffn(hT, [(w["moe_w_gate"][e], w["moe_w_up"][e], w["moe_w_down"][e]) for e in range(N_EXP)],
                FFN_EXP, 3, xres, xres, gatesT=gatesT)
    B.stage_norm(xres, outT, 0, out_dtype=F32, final_g=w["final_norm_g"])
    return B


def weight_map(inputs):
    m = {}
    for n, shp in W_SPECS:
        m[n] = np.ascontiguousarray(np.asarray(inputs[n], dtype=np.float32).reshape(shp))
    return m


SEQ = 4096
N_CORES = 2


def kernel(**inputs):
    x = np.asarray(inputs["x"], dtype=np.float32)
    c = np.asarray(inputs["c"], dtype=np.float32)
    bsz = x.shape[0]
    B = build_model(SEQ)
    wm = weight_map(inputs)
    in_maps = []
    for b in range(bsz):
        m = dict(wm)
        m["xT"] = np.ascontiguousarray(x[b].T)
        m["c"] = np.ascontiguousarray(c[b:b + 1])
        in_maps.append(m)
    res = run_bass_kernel_spmd(B.nc, in_maps, core_ids=list(range(bsz)))
    out = np.stack([np.ascontiguousarray(res.results[b]["outT"].T) for b in range(bsz)], axis=0)
    return out.astype(np.float32)
```

```python
import math
from contextlib import ExitStack

import numpy as np
import concourse.bass as bass
import concourse.mybir as mybir
from concourse.bass_utils import run_bass_kernel_spmd

F32 = mybir.dt.float32
BF16 = mybir.dt.bfloat16
ALU = mybir.AluOpType
AF = mybir.ActivationFunctionType
AX = mybir.AxisListType

D = 4096
KC = D // 128
TB = 512
EPS = 1e-6
FFN_DENSE = 11008
N_EXP = 8
FFN_EXP = 4096


class _Op:
    __slots__ = ("eng", "fn", "deps", "sig", "sigval", "is_dma", "dsem", "dval", "dprev")

    def __init__(self, eng, fn, deps, is_dma):
        self.eng = eng
        self.fn = fn
        self.deps = deps
        self.sig = False
        self.sigval = 0
        self.is_dma = is_dma
        self.dsem = -1
        self.dval = 0
        self.dprev = 0


class Prog:
    ENG = ("pe", "act", "dve", "pool", "sp")

    def __init__(self, nc, stack, n_dsem=32):
        self.nc = nc
        self.sems = {e: stack.enter_context(nc.semaphore("s_" + e)) for e in self.ENG}
        self.sigcount = {e: 0 for e in self.ENG}
        self.dsems = [stack.enter_context(nc.semaphore("dq%d" % i)) for i in range(n_dsem)]
        self.dtotal = [0] * n_dsem
        self.dnext = 0
        self.seen = {e: {} for e in self.ENG}
        self._reset()

    def _reset(self):
        self.ops = {e: [] for e in self.ENG}
        self.order = []
        self.last_w = {}
        self.readers = {}

    def _rec(self, eng, fn, reads, writes, is_dma):
        deps = []
        for k in reads:
            w = self.last_w.get(k)
            if w is not None:
                deps.append(w)
        for k in writes:
            w = self.last_w.get(k)
            if w is not None:
                deps.append(w)
            deps.extend(self.readers.get(k, ()))
        o = _Op(eng, fn, deps, is_dma)
        for k in reads:
            self.readers.setdefault(k, []).append(o)
        for k in writes:
            self.last_w[k] = o
            self.readers[k] = []
        self.ops[eng].append(o)
        self.order.append(o)
        return o

    def op(self, eng, fn, reads=(), writes=()):
        return self._rec(eng, fn, reads, writes, False)

    def dma(self, queue, out, in_, reads=(), writes=(), **kw):
        return self._rec(queue, lambda e: e.dma_start(out=out, in_=in_, **kw), reads, writes, True)

    def emit(self):
        nc = self.nc
        for o in self.order:
            for d in o.deps:
                if d.is_dma:
                    continue
                if d.eng == "pe" and o.eng == "pe" and not o.is_dma:
                    continue
                d.sig = True
        for e in self.ENG:
            for o in reversed(self.ops[e]):
                if not o.is_dma:
                    o.sig = True
                    break
        for e in self.ENG:
            for o in self.ops[e]:
                if not o.is_dma and o.sig:
                    self.sigcount[e] += 1
                    o.sigval = self.sigcount[e]
        n = len(self.dsems)
        for o in self.order:
            if o.is_dma:
                k = self.dnext
                self.dnext = (k + 1) % n
                o.dsem = k
                o.dprev = self.dtotal[k]
                self.dtotal[k] += 16
                o.dval = self.dtotal[k]

        def run(engobj, e):
            seen = self.seen[e]

            def wait(key, sem, val):
                if val <= 0 or seen.get(key, 0) >= val:
                    return
                seen[key] = val
                engobj.wait_ge(sem, val)

            for o in self.ops[e]:
                for d in o.deps:
                    if d.is_dma:
                        wait(("d", d.dsem), self.dsems[d.dsem], d.dval)
                    elif d.eng == "pe" and e == "pe" and not o.is_dma:
                        continue
                    else:
                        wait(("c", d.eng), self.sems[d.eng], d.sigval)
                if o.is_dma:
                    wait(("d", o.dsem), self.dsems[o.dsem], o.dprev)
                ins = o.fn(engobj)
                if o.is_dma:
                    ins.then_inc(self.dsems[o.dsem], 16)
                elif o.sig:
                    ins.then_inc(self.sems[e], 1)
            for e2 in self.ENG:
                wait(("c", e2), self.sems[e2], self.sigcount[e2])
            for k in range(n):
                wait(("d", k), self.dsems[k], self.dtotal[k])

        with nc.Block() as block:
            @block.tensor
            def _(t):
                run(t, "pe")

            @block.scalar
            def _(t):
                run(t, "act")

            @block.vector
            def _(t):
                run(t, "dve")

            @block.gpsimd
            def _(t):
                run(t, "pool")

            @block.sync
            def _(t):
                run(t, "sp")
        self._reset()


class Builder:
    def __init__(self, S, stages, dbg=()):
        self.S = S
        self.NTB = S // TB
        self.stages = stages
        self.dbg = set(dbg)
        self.nc = bass.Bass("TRN2", target_bir_lowering=False)
        self.gstack = ExitStack()
        self.P = Prog(self.nc, self.gstack)
        self.inputs = {}
        self.ucount = 0

    def ext_in(self, name, shape, dtype=F32):
        t = self.nc.dram_tensor(name, list(shape), dtype, kind="ExternalInput").ap()
        self.inputs[name] = t
        return t

    def ext_out(self, name, shape, dtype=F32):
        return self.nc.dram_tensor(name, list(shape), dtype, kind="ExternalOutput").ap()

    def scratch(self, name, shape, dtype):
        return self.nc.dram_tensor(name, list(shape), dtype, kind="Internal").ap()

    def gtile(self, name, shape, dtype):
        return self.gstack.enter_context(self.nc.sbuf_tensor(name, list(shape), dtype))

    def mkT(self, st):
        self.ucount += 1
        sid = self.ucount
        nc = self.nc
        return lambda n, s, d: st.enter_context(nc.sbuf_tensor("%s_s%d" % (n, sid), list(s), d))

    def PS(self, st, name, shape, dtype):
        self.ucount += 1
        return st.enter_context(self.nc.psum_tensor("%s_p%d" % (name, self.ucount), list(shape), dtype))

    def uid(self, p):
        self.ucount += 1
        return "%s%d" % (p, self.ucount)

    def setup_consts(self):
        P = self.P
        self.ones_bf = self.gtile("ones_bf", [128, 128], BF16)
        self.ones_f = self.gtile("ones_f", [128, 128], F32)
        self.ident_f = self.gtile("ident_f", [128, 128], F32)
        self.ident_bf = self.gtile("ident_bf", [128, 128], BF16)
        P.op("pool", lambda e: e.memset(self.ones_bf[:], 1.0), writes=["ones_bf"])
        P.op("pool", lambda e: e.memset(self.ones_f[:], 1.0), writes=["ones_f"])
        P.op("pool", lambda e: e.affine_select(
            out=self.ident_f[:], in_=self.ones_f[:], pattern=[[-1, 128]], compare_op=ALU.is_equal,
            fill=0.0, base=0, channel_multiplier=1), reads=["ones_f"], writes=["ident_f"])
        P.op("dve", lambda e: e.tensor_copy(out=self.ident_bf[:], in_=self.ident_f[:]),
             reads=["ident_f"], writes=["ident_bf"])

    def stage_ada(self, c_ap, mats):
        nc, P = self.nc, self.P
        M = len(mats)
        self.ada = self.gtile("ada", [128, M, 96], F32)
        self.gsc = self.gtile("gsc", [128, M, KC], F32)
        with ExitStack() as st:
            T = self.mkT(st)
            cT = T("cT", [128, KC], F32)
            scT = T("scT", [128, KC], BF16)
            row = T("row", [1, 3 * D], F32)
            bias = T("bias", [128, 96], F32)
            gT = T("gT", [128, KC], F32)
            one11 = T("one11", [1, 1], F32)
            slab = [T("aslab%d" % i, [128, KC, 512], BF16) for i in range(2)]
            ps = [self.PS(st, "aps%d" % i, [128, 512], F32) for i in range(2)]
            psT = self.PS(st, "apsT", [128, 512], F32)
            P.dma("sp", cT[:], c_ap.rearrange("o (c p) -> p (o c)", p=128), writes=["cT"],
                  allow_slow_non_contiguous=True)
            P.op("pool", lambda e: e.memset(one11[:], 1.0), writes=["one11"])
            P.op("act", lambda e: e.activation(out=scT[:], in_=cT[:], func=AF.Silu),
                 reads=["cT"], writes=["scT"])
            it = 0
            for m, (w, b, g) in enumerate(mats):
                for q in range(3):
                    P.dma("sp", bias[:, q * 32:(q + 1) * 32],
                          b[:, q * D:(q + 1) * D].rearrange("o (c p) -> p (o c)", p=128),
                          writes=["bias%d" % q], allow_slow_non_contiguous=True)
                P.dma("sp", gT[:], g.rearrange("o (c p) -> p (o c)", p=128), writes=["gT"],
                      allow_slow_non_contiguous=True)
                for j in range(3 * D // 512):
                    sl = slab[it % 2]
                    pj = ps[it % 2]
                    sk = "aslab%d" % (it % 2)
                    pk = "aps%d" % (it % 2)
                    it += 1
                    P.dma("pool", sl[:], w[:, j * 512:(j + 1) * 512].rearrange("(c p) n -> p c n", p=128),
                          writes=[sk])
                    for kc in range(KC):
                        P.op("pe", lambda e, sl=sl, pj=pj, kc=kc: e.matmul(
                            pj[0:1, :], scT[:, kc:kc + 1], sl[:, kc, :], start=(kc == 0), stop=(kc == KC - 1)),
                            reads=[sk, "scT"], writes=[pk])
                    P.op("act", lambda e, pj=pj, j=j: e.copy(out=row[0:1, j * 512:(j + 1) * 512], in_=pj[0:1, :]),
                         reads=[pk], writes=["row"])
                for i in range(96):
                    P.op("pe", lambda e, i=i: e.matmul(psT[:, i:i + 1], row[0:1, i * 128:(i + 1) * 128],
                                                       one11[0:1, 0:1], start=True, stop=True),
                         reads=["row", "one11"], writes=["psT"])
                P.op("dve", lambda e, m=m: e.tensor_tensor(out=self.ada[:, m, :], in0=psT[:, 0:96], in1=bias[:],
                                                           op=ALU.add),
                     reads=["psT", "bias0", "bias1", "bias2"], writes=["ada"])
                P.op("dve", lambda e, m=m: e.scalar_tensor_tensor(
                    out=self.gsc[:, m, :], in0=self.ada[:, m, 32:64], scalar=1.0, in1=gT[:],
                    op0=ALU.add, op1=ALU.mult), reads=["ada", "gT"], writes=["gsc"])
            P.emit()

    def shift(self, m, c):
        return self.ada[:, m, c:c + 1]

    def gate(self, m, c):
        return self.ada[:, m, 64 + c:65 + c]

    def stage_norm(self, src, dst, m, out_dtype=BF16, final_g=None, router=None):
        nc, P = self.nc, self.P
        with ExitStack() as st:
            T = self.mkT(st)
            xt = T("n_xt", [128, KC, TB], F32)
            sq = T("n_sq", [128, KC, TB], BF16)
            hsb = T("n_h", [128, KC, TB], out_dtype)
            rstd = T("n_rstd", [128, TB], F32)
            tmp = [T("n_tmp%d" % i, [128, TB], F32) for i in range(2)]
            ps = self.PS(st, "n_ps", [128, TB], F32)
            if final_g is not None:
                gfin = T("n_gfin", [128, KC], F32)
                P.dma("sp", gfin[:], final_g.rearrange("o (c p) -> p (o c)", p=128), writes=["gfin"],
                      allow_slow_non_contiguous=True)
            if router is not None:
                wr, gdst = router
                wr_sb = T("n_wr", [128, KC, N_EXP], F32)
                h32 = [T("n_h32%d" % i, [128, TB], F32) for i in range(2)]
                lg = T("n_lg", [N_EXP, TB], F32)
                lgt = T("n_lgt", [128, 4, N_EXP], F32)
                top8 = T("n_top8", [128, 4, 8], F32)
                gm = T("n_gm", [128, 4, N_EXP], F32)
                ge = T("n_ge", [128, 4, N_EXP], F32)
                den = T("n_den", [128, 4], F32)
                nm1 = T("n_nm1", [128, 4], F32)
                gts = T("n_gts", [N_EXP, TB], F32)
                psl = self.PS(st, "n_psl", [128, TB], F32)
                pst = self.PS(st, "n_pst", [128, TB], F32)
                P.dma("sp", wr_sb[:], wr.rearrange("(c p) e -> p c e", p=128), writes=["wr"],
                      allow_slow_non_contiguous=True)
            for tb in range(self.NTB):
                ts = slice(tb * TB, (tb + 1) * TB)
                P.dma("sp", xt[:], src[:, ts].rearrange("(c p) t -> p c t", p=128), writes=["xt"])
                for q in range(4):
                    P.op("act", lambda e, q=q: e.activation(out=sq[:, q * 8:(q + 1) * 8, :],
                                                            in_=xt[:, q * 8:(q + 1) * 8, :], func=AF.Square),
                         reads=["xt"], writes=["sq%d" % q])
                for c in range(KC):
                    P.op("pe", lambda e, c=c: e.matmul(ps[:], self.ones_bf[:], sq[:, c, :],
                                                       start=(c == 0), stop=(c == KC - 1)),
                         reads=["sq%d" % (c // 8)], writes=["ps"])
                P.op("dve", lambda e: e.tensor_scalar(out=rstd[:], in0=ps[:], scalar1=1.0 / D, scalar2=EPS,
                                                      op0=ALU.mult, op1=ALU.add), reads=["ps"], writes=["rstd"])
                P.op("act", lambda e: e.activation(out=rstd[:], in_=rstd[:], func=AF.Sqrt),
                     reads=["rstd"], writes=["rstd"])
                P.op("dve", lambda e: e.reciprocal(out=rstd[:], in_=rstd[:]), reads=["rstd"], writes=["rstd"])
                for c in range(KC):
                    tk = "tmp%d" % (c % 2)
                    tt = tmp[c % 2]
                    P.op("dve", lambda e, c=c, tt=tt: e.tensor_tensor(out=tt[:], in0=xt[:, c, :], in1=rstd[:],
                                                                      op=ALU.mult),
                         reads=["xt", "rstd"], writes=[tk])
                    if final_g is not None:
                        P.op("act", lambda e, c=c, tt=tt: e.activation(out=hsb[:, c, :], in_=tt[:], func=AF.Copy,
                                                                       scale=gfin[:, c:c + 1]),
                             reads=[tk, "gfin"], writes=["hsb"])
                    elif router is None:
                        P.op("act", lambda e, c=c, tt=tt: e.activation(
                            out=hsb[:, c, :], in_=tt[:], func=AF.Identity, scale=self.gsc[:, m, c:c + 1],
                            bias=self.shift(m, c)), reads=[tk], writes=["hsb"])
                    else:
                        hk = "h32%d" % (c % 2)
                        hh = h32[c % 2]
                        P.op("act", lambda e, c=c, tt=tt, hh=hh: e.activation(
                            out=hh[:], in_=tt[:], func=AF.Identity, scale=self.gsc[:, m, c:c + 1],
                            bias=self.shift(m, c)), reads=[tk], writes=[hk])
                        P.op("dve", lambda e, c=c, hh=hh: e.tensor_copy(out=hsb[:, c, :], in_=hh[:]),
                             reads=[hk], writes=["hsb"])
                        P.op("pe", lambda e, c=c, hh=hh: e.matmul(psl[0:N_EXP, :], wr_sb[:, c, :], hh[:],
                                                                  start=(c == 0), stop=(c == KC - 1)),
                             reads=[hk, "wr"], writes=["psl"])
                P.dma("sp", dst[:, ts].rearrange("(c p) t -> p c t", p=128), hsb[:], reads=["hsb"])
                if router is not None:
                    P.op("act", lambda e: e.copy(out=lg[:], in_=psl[0:N_EXP, :]), reads=["psl"], writes=["lg"])
                    for q in range(4):
                        P.op("pe", lambda e, q=q: e.transpose(pst[:, q * N_EXP:(q + 1) * N_EXP],
                                                              lg[:, q * 128:(q + 1) * 128],
                                                              self.ident_f[0:N_EXP, 0:N_EXP]),
                             reads=["lg"], writes=["pst"])
                    P.op("dve", lambda e: e.tensor_copy(out=lgt[:], in_=pst[:, 0:4 * N_EXP].rearrange(
                        "p (q e) -> p q e", q=4)), reads=["pst"], writes=["lgt"])
                    for q in range(4):
                        P.op("dve", lambda e, q=q: e.max(out=top8[:, q, :], in_=lgt[:, q, :]),
                             reads=["lgt"], writes=["top8_%d" % q])
                    for q in range(4):
                        P.op("dve", lambda e, q=q: e.tensor_scalar(
                            out=gm[:, q, :], in0=lgt[:, q, :], scalar1=top8[:, q, 1:2], scalar2=None,
                            op0=ALU.is_ge), reads=["lgt", "top8_%d" % q], writes=["gm%d" % q])
                        P.op("dve", lambda e, q=q: e.tensor_scalar(
                            out=nm1[:, q:q + 1], in0=top8[:, q, 0:1], scalar1=-1.0, scalar2=None,
                            op0=ALU.mult), reads=["top8_%d" % q], writes=["nm1_%d" % q])
                        P.op("act", lambda e, q=q: e.activation(out=ge[:, q, :], in_=lgt[:, q, :], func=AF.Exp,
                                                                bias=nm1[:, q:q + 1]),
                             reads=["lgt", "nm1_%d" % q], writes=["ge%d" % q])
                        P.op("dve", lambda e, q=q: e.tensor_tensor(out=ge[:, q, :], in0=ge[:, q, :],
                                                                   in1=gm[:, q, :], op=ALU.mult),
                             reads=["ge%d" % q, "gm%d" % q], writes=["ge%d" % q])
                        P.op("dve", lambda e, q=q: e.reduce_sum(out=den[:, q:q + 1], in_=ge[:, q, :], axis=AX.X),
                             reads=["ge%d" % q], writes=["den%d" % q])
                        P.op("dve", lambda e, q=q: e.reciprocal(out=den[:, q:q + 1], in_=den[:, q:q + 1]),
                             reads=["den%d" % q], writes=["den%d" % q])
                        P.op("dve", lambda e, q=q: e.tensor_scalar(
                            out=ge[:, q, :], in0=ge[:, q, :], scalar1=den[:, q:q + 1], scalar2=None,
                            op0=ALU.mult), reads=["ge%d" % q, "den%d" % q], writes=["ge%d" % q])
                        P.op("pe", lambda e, q=q: e.transpose(psl[0:N_EXP, q * 128:(q + 1) * 128],
                                                              ge[:, q, :], self.ident_f[:]),
                             reads=["ge%d" % q], writes=["psl"])
                    P.op("act", lambda e: e.copy(out=gts[:], in_=psl[0:N_EXP, :]), reads=["psl"], writes=["gts"])
                    P.dma("sp", gdst[:, ts], gts[:], reads=["gts"])
            P.emit()

    def gelu_ops(self, pfx, src_ap, src_keys, out_ap, out_key, t1, t1k, t2, t2k):
        P = self.P
        P.op("act", lambda e: e.activation(out=t1, in_=src_ap, func=AF.Square), reads=src_keys, writes=[t1k])
        P.op("dve", lambda e: e.tensor_scalar(out=t1, in0=t1, scalar1=0.044715, scalar2=1.0,
                                              op0=ALU.mult, op1=ALU.add), reads=[t1k], writes=[t1k])
        P.op("dve", lambda e: e.tensor_tensor(out=t1, in0=t1, in1=src_ap, op=ALU.mult),
             reads=[t1k] + list(src_keys), writes=[t1k])
        P.op("act", lambda e: e.activation(out=t2, in_=t1, func=AF.Sigmoid, scale=1.5957691216057308),
             reads=[t1k], writes=[t2k])
        P.op("dve", lambda e: e.tensor_tensor(out=out_ap, in0=t2, in1=src_ap, op=ALU.mult),
             reads=[t2k] + list(src_keys), writes=[out_key])

    def stage_linear_fm(self, inT, K, jobs):
        nc, P = self.nc, self.P
        kcn = K // 128
        with ExitStack() as st:
            T = self.mkT(st)
            inb = T("l_in", [128, kcn, TB], BF16)
            nslab = 4
            slab = [T("l_slab%d" % i, [128, kcn, 256], BF16) for i in range(nslab)]
            ost = [T("l_ost%d" % i, [128, 2, TB], BF16) for i in range(2)]
            xo = [T("l_xo%d" % i, [128, TB], F32) for i in range(2)]
            xn = [T("l_xn%d" % i, [128, TB], F32) for i in range(2)]
            t1 = [T("l_t1%d" % i, [128, TB], F32) for i in range(2)]
            t2 = [T("l_t2%d" % i, [128, TB], F32) for i in range(2)]
            ps = [self.PS(st, "l_ps%d" % i, [128, TB], F32) for i in range(4)]
            si = 0
            ci = 0
            use_cache = self.NTB > 1
            if use_cache:
                sid = self.uid("lc")
                for ji, job in enumerate(jobs):
                    job["cache"] = [self.scratch("%s_%d_%d" % (sid, ji, wi), [job["N"] // 256, 128, kcn * 256], BF16)
                                    for wi in range(len(job["w"]))]
            for tb in range(self.NTB):
                ts = slice(tb * TB, (tb + 1) * TB)
                P.dma("sp", inb[:], inT[:, ts].rearrange("(c p) t -> p c t", p=128), writes=["inb"])
                for ji, job in enumerate(jobs):
                    N = job["N"]
                    epi = job["epi"]
                    for ng in range(N // 256):
                        sls = []
                        for wi, w in enumerate(job["w"]):
                            sl = slab[si % nslab]
                            sk = "slab%d" % (si % nslab)
                            si += 1
                            ck = "c%d_%d_%d" % (ji, wi, ng)
                            if tb == 0 or not use_cache:
                                P.dma("pool", sl[:], w[:, ng * 256:(ng + 1) * 256].rearrange("(c p) n -> p c n", p=128),
                                      writes=[sk])
                                if use_cache:
                                    P.dma("sp", job["cache"][wi][ng], sl[:].rearrange("p c n -> p (c n)"),
                                          reads=[sk], writes=[ck])
                            else:
                                P.dma("pool", sl[:].rearrange("p c n -> p (c n)"), job["cache"][wi][ng],
                                      reads=[ck], writes=[sk])
                            sls.append((sl, sk))
                        for j in range(2):
                            nch = ng * 2 + j
                            pss = []
                            for (sl, sk) in sls:
                                pt = ps[ci % 4]
                                pk = "ps%d" % (ci % 4)
                                ci += 1
                                for kc in range(kcn):
                                    P.op("pe", lambda e, pt=pt, sl=sl, kc=kc, j=j: e.matmul(
                                        pt[:], sl[:, kc, j * 128:(j + 1) * 128], inb[:, kc, :],
                                        start=(kc == 0), stop=(kc == kcn - 1)), reads=[sk, "inb"], writes=[pk])
                                pss.append((pt, pk))
                            b = ci % 2
                            if epi == "bf16":
                                o = ost[ng % 2]
                                ok = "ost%d_%d" % (ng % 2, j)
                                P.op("act", lambda e, o=o, j=j, pt=pss[0][0]: e.copy(out=o[:, j, :], in_=pt[:]),
                                     reads=[pss[0][1]], writes=[ok])
                            elif epi == "gelu":
                                o = ost[ng % 2]
                                ok = "ost%d_%d" % (ng % 2, j)
                                self.gelu_ops("l", pss[0][0][:], [pss[0][1]], o[:, j, :], ok,
                                              t1[b][:], "t1%d" % b, t2[b][:], "t2%d" % b)
                            else:
                                m = job["m"]
                                rows = slice(nch * 128, (nch + 1) * 128)
                                P.dma("sp", xo[b][:], job["xsrc"][rows, ts], writes=["xo%d" % b])
                                if epi == "resid":
                                    val, vk = pss[0][0][:], pss[0][1]
                                else:
                                    P.op("act", lambda e, b=b, pt=pss[1][0]: e.activation(
                                        out=t1[b][:], in_=pt[:], func=AF.Sigmoid), reads=[pss[1][1]],
                                        writes=["t1%d" % b])
                                    P.op("dve", lambda e, b=b, pt=pss[0][0]: e.tensor_tensor(
                                        out=t2[b][:], in0=pt[:], in1=t1[b][:], op=ALU.mult),
                                        reads=[pss[0][1], "t1%d" % b], writes=["t2%d" % b])
                                    val, vk = t2[b][:], "t2%d" % b
                                P.op("dve", lambda e, b=b, val=val, m=m, nch=nch: e.scalar_tensor_tensor(
                                    out=xn[b][:], in0=val, scalar=self.gate(m, nch), in1=xo[b][:],
                                    op0=ALU.mult, op1=ALU.add), reads=[vk, "xo%d" % b], writes=["xn%d" % b])
                                P.dma("sp", job["xdst"][rows, ts], xn[b][:], reads=["xn%d" % b])
                        if epi in ("bf16", "gelu"):
                            o = ost[ng % 2]
                            P.dma("sp", job["out"][ng * 256:(ng + 1) * 256, ts].rearrange("(j p) t -> p j t", p=128),
                                  o[:], reads=["ost%d_0" % (ng % 2), "ost%d_1" % (ng % 2)])
            P.emit()

    def stage_linear_tm(self, inT, w, N, out, out_dtype, gelu=False):
        nc, P = self.nc, self.P
        with ExitStack() as st:
            T = self.mkT(st)
            inb = T("m_in", [128, KC, TB], BF16)
            slab = [T("m_slab%d" % i, [128, KC, 512], BF16) for i in range(2)]
            ost = [T("m_ost%d" % i, [128, 512], out_dtype) for i in range(2)]
            t1 = [T("m_t1%d" % i, [128, 512], F32) for i in range(2)]
            t2 = [T("m_t2%d" % i, [128, 512], F32) for i in range(2)]
            ps = [self.PS(st, "m_ps%d" % i, [128, 512], F32) for i in range(2)]
            si = 0
            ci = 0
            use_cache = self.NTB > 1
            if use_cache:
                cache = self.scratch(self.uid("mc"), [N // 512, 128, KC * 512], BF16)
            for tb in range(self.NTB):
                ts = slice(tb * TB, (tb + 1) * TB)
                P.dma("sp", inb[:], inT[:, ts].rearrange("(c p) t -> p c t", p=128), writes=["inb"])
                for ns in range(N // 512):
                    sl = slab[si % 2]
                    sk = "slab%d" % (si % 2)
                    si += 1
                    if tb == 0 or not use_cache:
                        P.dma("pool", sl[:], w[:, ns * 512:(ns + 1) * 512].rearrange("(c p) n -> p c n", p=128),
                              writes=[sk])
                        if use_cache:
                            P.dma("sp", cache[ns], sl[:].rearrange("p c n -> p (c n)"), reads=[sk],
                                  writes=["c%d" % ns])
                    else:
                        P.dma("pool", sl[:].rearrange("p c n -> p (c n)"), cache[ns], reads=["c%d" % ns],
                              writes=[sk])
                    for tq in range(TB // 128):
                        b = ci % 2
                        ci += 1
                        pt = ps[b]
                        pk = "ps%d" % b
                        for kc in range(KC):
                            P.op("pe", lambda e, pt=pt, sl=sl, kc=kc, tq=tq: e.matmul(
                                pt[:], inb[:, kc, tq * 128:(tq + 1) * 128], sl[:, kc, :],
                                start=(kc == 0), stop=(kc == KC - 1)), reads=[sk, "inb"], writes=[pk])
                        if gelu:
                            self.gelu_ops("m", pt[:], [pk], ost[b][:], "ost%d" % b,
                                          t1[b][:], "t1%d" % b, t2[b][:], "t2%d" % b)
                        else:
                            P.op("act", lambda e, b=b, pt=pt: e.copy(out=ost[b][:], in_=pt[:]),
                                 reads=[pk], writes=["ost%d" % b])
                        r0 = tb * TB + tq * 128
                        P.dma("sp", out[r0:r0 + 128, ns * 512:(ns + 1) * 512], ost[b][:], reads=["ost%d" % b])
            P.emit()

    def stage_ffn(self, hT, experts, F, m, xsrc, xdst, gatesT=None):
        nc, P = self.nc, self.P
        FG = 256
        with ExitStack() as st:
            T = self.mkT(st)
            hb = T("f_h", [128, KC, TB], BF16)
            acc = T("f_acc", [128, KC, TB], F32)
            wgs = [T("f_wg%d" % i, [128, KC, FG], BF16) for i in range(2)]
            wus = [T("f_wu%d" % i, [128, KC, FG], BF16) for i in range(2)]
            wds = [T("f_wd%d" % i, [128, FG // 128, D], BF16) for i in range(2)]
            act_t = [T("f_act%d" % i, [128, FG // 128, TB], BF16) for i in range(2)]
            sg = [T("f_sg%d" % i, [128, TB], F32) for i in range(2)]
            xo = sg
            if gatesT is not None:
                grow = T("f_grow", [1, TB], F32)
                gbc = [T("f_gbc%d" % i, [128, TB], F32) for i in range(1)] * 2
            psg = [self.PS(st, "f_pg%d" % i, [128, TB], F32) for i in range(2)]
            psu = [self.PS(st, "f_pu%d" % i, [128, TB], F32) for i in range(2)]
            psd = [self.PS(st, "f_pd%d" % i, [128, TB], F32) for i in range(3)]
            gi = 0
            ci = 0
            di = 0
            use_cache = self.NTB > 1
            if use_cache:
                sid = self.uid("fc")
                NG = F // FG
                cgs = [self.scratch("%s_g%d" % (sid, i), [NG, 128, KC * FG], BF16) for i in range(len(experts))]
                cus = [self.scratch("%s_u%d" % (sid, i), [NG, 128, KC * FG], BF16) for i in range(len(experts))]
                cds = [self.scratch("%s_d%d" % (sid, i), [NG, 128, (FG // 128) * D], BF16)
                       for i in range(len(experts))]
            for tb in range(self.NTB):
                ts = slice(tb * TB, (tb + 1) * TB)
                P.dma("sp", hb[:], hT[:, ts].rearrange("(c p) t -> p c t", p=128), writes=["hb"])
                first = True
                pend = []
                for ex, (wg, wu, wd) in enumerate(experts):
                    if gatesT is not None:
                        pt = psd[di % 3]
                        pk = "pd%d" % (di % 3)
                        di += 1
                        P.dma("sp", grow[:], gatesT[ex:ex + 1, ts], writes=["grow"])
                        P.op("pe", lambda e, pt=pt: e.matmul(pt[:], self.ones_f[0:1, :], grow[0:1, :],
                                                             start=True, stop=True),
                             reads=["grow"], writes=[pk])
                        P.op("act", lambda e, pt=pt, ex=ex: e.copy(out=gbc[0][:], in_=pt[:]),
                             reads=[pk], writes=["gbc0"])
                    for fg in range(F // FG):
                        b = gi % 2
                        gi += 1
                        fs = slice(fg * FG, (fg + 1) * FG)
                        if tb == 0 or not use_cache:
                            P.dma("pool", wgs[b][:], wg[:, fs].rearrange("(c p) n -> p c n", p=128),
                                  writes=["wg%d" % b])
                            P.dma("pool", wus[b][:], wu[:, fs].rearrange("(c p) n -> p c n", p=128),
                                  writes=["wu%d" % b])
                            P.dma("pool", wds[b][:], wd[fs, :].rearrange("(j p) n -> p j n", p=128), writes=["wd%d" % b])
                            if use_cache:
                                P.dma("sp", cgs[ex][fg], wgs[b][:].rearrange("p c n -> p (c n)"),
                                      reads=["wg%d" % b], writes=["cg%d_%d" % (ex, fg)])
                                P.dma("sp", cus[ex][fg], wus[b][:].rearrange("p c n -> p (c n)"),
                                      reads=["wu%d" % b], writes=["cu%d_%d" % (ex, fg)])
                                P.dma("sp", cds[ex][fg], wds[b][:].rearrange("p j n -> p (j n)"),
                                      reads=["wd%d" % b], writes=["cd%d_%d" % (ex, fg)])
                        else:
                            P.dma("pool", wgs[b][:].rearrange("p c n -> p (c n)"), cgs[ex][fg],
                                  reads=["cg%d_%d" % (ex, fg)], writes=["wg%d" % b])
                            P.dma("pool", wus[b][:].rearrange("p c n -> p (c n)"), cus[ex][fg],
                                  reads=["cu%d_%d" % (ex, fg)], writes=["wu%d" % b])
                            P.dma("pool", wds[b][:].rearrange("p j n -> p (j n)"), cds[ex][fg],
                                  reads=["cd%d_%d" % (ex, fg)], writes=["wd%d" % b])
                        for j in range(FG // 128):
                            cb = ci % 2
                            ci += 1
                            for kc in range(KC):
                                P.op("pe", lambda e, cb=cb, b=b, kc=kc, j=j: e.matmul(
                                    psg[cb][:], wgs[b][:, kc, j * 128:(j + 1) * 128], hb[:, kc, :],
                                    start=(kc == 0), stop=(kc == KC - 1)), reads=["wg%d" % b, "hb"],
                                    writes=["pg%d" % cb])
                            for kc in range(KC):
                                P.op("pe", lambda e, cb=cb, b=b, kc=kc, j=j: e.matmul(
                                    psu[cb][:], wus[b][:, kc, j * 128:(j + 1) * 128], hb[:, kc, :],
                                    start=(kc == 0), stop=(kc == KC - 1)), reads=["wu%d" % b, "hb"],
                                    writes=["pu%d" % cb])
                            P.op("act", lambda e, cb=cb: e.activation(out=sg[cb][:], in_=psg[cb][:], func=AF.Silu),
                                 reads=["pg%d" % cb], writes=["sg%d" % cb])
                            if gatesT is not None:
                                P.op("dve", lambda e, cb=cb, ex=ex: e.tensor_tensor(
                                    out=sg[cb][:], in0=sg[cb][:], in1=gbc[ex % 2][:], op=ALU.mult),
                                    reads=["sg%d" % cb, "gbc0"], writes=["sg%d" % cb])
                            P.op("dve", lambda e, cb=cb, b=b, j=j: e.tensor_tensor(
                                out=act_t[b][:, j, :], in0=psu[cb][:], in1=sg[cb][:], op=ALU.mult),
                                reads=["pu%d" % cb, "sg%d" % cb], writes=["act%d_%d" % (b, j)])
                        pend.append(b)
                        last_group = (ex == len(experts) - 1) and (fg == F // FG - 1)
                        if len(pend) < 2 and not last_group:
                            continue
                        items = [(pb, j) for pb in pend for j in range(FG // 128)]
                        pend = []
                        for nch in range(KC):
                            pt = psd[di % 3]
                            pk = "pd%d" % (di % 3)
                            di += 1
                            for ii, (pb, j) in enumerate(items):
                                P.op("pe", lambda e, pt=pt, pb=pb, j=j, nch=nch, ii=ii, n_=len(items): e.matmul(
                                    pt[:], wds[pb][:, j, nch * 128:(nch + 1) * 128], act_t[pb][:, j, :],
                                    start=(ii == 0), stop=(ii == n_ - 1)),
                                    reads=["wd%d" % pb, "act%d_%d" % (pb, j)], writes=[pk])
                            eng = "dve" if nch % 2 == 0 else "act"
                            if first:
                                if eng == "dve":
                                    P.op("dve", lambda e, pt=pt, nch=nch: e.tensor_copy(out=acc[:, nch, :], in_=pt[:]),
                                         reads=[pk], writes=["acc%d" % nch])
                                else:
                                    P.op("act", lambda e, pt=pt, nch=nch: e.copy(out=acc[:, nch, :], in_=pt[:]),
                                         reads=[pk], writes=["acc%d" % nch])
                            else:
                                P.op("dve", lambda e, pt=pt, nch=nch: e.tensor_tensor(
                                    out=acc[:, nch, :], in0=pt[:], in1=acc[:, nch, :], op=ALU.add),
                                    reads=[pk, "acc%d" % nch], writes=["acc%d" % nch])
                        first = False
                for nch in range(KC):
                    b = nch % 2
                    rows = slice(nch * 128, (nch + 1) * 128)
                    P.dma("sp", xo[b][:], xsrc[rows, ts], writes=["sg%d" % b])
                    P.op("dve", lambda e, b=b, nch=nch: e.scalar_tensor_tensor(
                        out=xo[b][:], in0=acc[:, nch, :], scalar=self.gate(m, nch), in1=xo[b][:],
                        op0=ALU.mult, op1=ALU.add), reads=["acc%d" % nch, "sg%d" % b], writes=["sg%d" % b])
                    P.dma("sp", xdst[rows, ts], xo[b][:], reads=["sg%d" % b])
            P.emit()

    def stage_attn(self, qT, kT, v, outT, n_heads=16):
        nc, P = self.nc, self.P
        S = self.S
        NB = S // 128
        sc = 1.0 / math.sqrt(128.0)
        CH = 1024
        with ExitStack() as st:
            T = self.mkT(st)
            qh = [T("a_q%d" % i, [128, S], BF16) for i in range(2)]
            kh = [T("a_k%d" % i, [128, S], BF16) for i in range(2)]
            vh = [T("a_v%d" % i, [128, NB, 128], BF16) for i in range(2)]
            oh = [T("a_o%d" % i, [128, S], BF16) for i in range(1)] * 2
            SP = [T("a_sp%d" % i, [128, S], F32) for i in range(2)]
            U = [T("a_u%d" % i, [128, S], F32) for i in range(3)]
            CS = [T("a_cs%d" % i, [128, S], F32) for i in range(2)]
            W = [T("a_w%d" % i, [128, S], BF16) for i in range(1)] * 2
            WT = [T("a_wt%d" % i, [128, S], BF16) for i in range(1)] * 2
            ones = T("a_ones", [128, S], F32)
            ntot = [T("a_nt%d" % i, [128, 1], F32) for i in range(2)]
            m01 = T("a_m01", [128, 128], F32)
            m01b = T("a_m01b", [128, 128], BF16)
            psS = [self.PS(st, "a_ps%d" % i, [128, CH], F32) for i in range(2)]
            psT = [self.PS(st, "a_pt%d" % i, [128, 1024], BF16) for i in range(2)]
            psO = [self.PS(st, "a_po%d" % i, [128, 512], F32) for i in range(2)]
            P.op("pool", lambda e: e.memset(ones[:], 1.0), writes=["ones"])
            P.op("pool", lambda e: e.affine_select(
                out=m01[:], in_=ones[:, 0:128], pattern=[[-1, 128]], compare_op=ALU.is_ge, fill=0.0,
                base=-1, channel_multiplier=1), reads=["ones"], writes=["m01"])
            P.op("pool", lambda e: e.tensor_copy(out=m01b[:], in_=m01[:]), reads=["m01"], writes=["m01b"])
            mneg = T("a_mneg", [128, 128], F32)
            P.op("pool", lambda e: e.memset(mneg[:], 0.0), writes=["mneg"])
            P.op("pool", lambda e: e.affine_select(
                out=mneg[:], in_=mneg[:], pattern=[[-1, 128]], compare_op=ALU.is_ge, fill=-30000.0,
                base=-1, channel_multiplier=1), reads=["mneg"], writes=["mneg"])
            cnt = {"sci": 0, "tci": 0}

            def load_head(h):
                hb = h % 2
                rows = slice(h * 128, (h + 1) * 128)
                P.dma("sp", qh[hb][:], qT[rows, :], writes=["q%d" % hb])
                P.dma("sp", kh[hb][:], kT[rows, :], writes=["k%d" % hb])
                P.dma("sp", vh[hb][:], v[:, rows].rearrange("(kb p) d -> p kb d", p=128), writes=["v%d" % hb])

            def phase_a(h, qb, b, ub):
                hb = h % 2
                nk = (qb + 1) * 128
                dg = slice(qb * 128, nk)
                for c0 in range(0, nk, CH):
                    w_ = min(CH, nk - c0)
                    sb_ = cnt["sci"] % 2
                    cnt["sci"] += 1
                    pS = psS[sb_]
                    sk = "pS%d" % sb_
                    for o_ in range(0, w_, 512):
                        ww = min(512, w_ - o_)
                        P.op("pe", lambda e, pS=pS, o_=o_, ww=ww, c0=c0, hb=hb, qb=qb: e.matmul(
                            pS[:, o_:o_ + ww], qh[hb][:, qb * 128:(qb + 1) * 128],
                            kh[hb][:, c0 + o_:c0 + o_ + ww], start=True, stop=True),
                            reads=["q%d" % hb, "k%d" % hb], writes=[sk])
                    cs_ = slice(c0, c0 + w_)
                    P.op("act", lambda e, pS=pS, w_=w_, cs_=cs_, b=b, ub=ub: e.activation(
                        out=SP[b][:, cs_], in_=pS[:, 0:w_], func=AF.Exp, scale=sc),
                        reads=[sk], writes=["SP%d" % b])
                    P.op("act", lambda e, cs_=cs_, b=b, ub=ub: e.activation(
                        out=SP[b][:, cs_], in_=SP[b][:, cs_], func=AF.Ln, bias=1.0),
                        reads=["SP%d" % b], writes=["SP%d" % b])
                    P.op("dve", lambda e, pS=pS, w_=w_, cs_=cs_, b=b, ub=ub: e.scalar_tensor_tensor(
                        out=U[ub][:, cs_], in0=pS[:, 0:w_], scalar=sc, in1=SP[b][:, cs_],
                        op0=ALU.mult, op1=ALU.subtract), reads=[sk, "SP%d" % b], writes=["U%d" % ub])
                P.op("dve", lambda e, b=b, ub=ub, dg=dg: e.tensor_tensor(
                    out=SP[b][:, dg], in0=SP[b][:, dg], in1=m01[:], op=ALU.mult),
                    reads=["SP%d" % b, "m01"], writes=["SP%d" % b])
                P.op("dve", lambda e, b=b, ub=ub, nk=nk: e.tensor_tensor_scan(
                    out=CS[b][:, 0:nk], data0=ones[:, 0:nk], data1=SP[b][:, 0:nk], initial=0.0,
                    op0=ALU.mult, op1=ALU.add), reads=["SP%d" % b, "ones"], writes=["CS%d" % b])
                P.op("dve", lambda e, b=b, ub=ub, nk=nk: e.tensor_tensor(
                    out=U[ub][:, 0:nk], in0=U[ub][:, 0:nk], in1=CS[b][:, 0:nk], op=ALU.add),
                    reads=["U%d" % ub, "CS%d" % b], writes=["U%d" % ub])
                P.op("dve", lambda e, b=b, ub=ub, dg=dg: e.tensor_tensor(
                    out=U[ub][:, dg], in0=U[ub][:, dg], in1=mneg[:], op=ALU.add),
                    reads=["U%d" % ub, "mneg"], writes=["U%d" % ub])
                P.op("dve", lambda e, b=b, ub=ub, nk=nk: e.tensor_scalar(
                    out=ntot[b][:], in0=CS[b][:, nk - 1:nk], scalar1=-1.0, scalar2=None, op0=ALU.mult),
                    reads=["CS%d" % b], writes=["nt%d" % b])

            def phase_b(h, qb, b, ub):
                hb = h % 2
                nk = (qb + 1) * 128
                dg = slice(qb * 128, nk)
                P.op("act", lambda e, b=b, ub=ub, nk=nk: e.activation(
                    out=W[b][:, 0:nk], in_=U[ub][:, 0:nk], func=AF.Exp, bias=ntot[b][:, 0:1]),
                    reads=["U%d" % ub, "nt%d" % b], writes=["W0"])
                for k0 in range(0, qb + 1, 8):
                    k1 = min(qb + 1, k0 + 8)
                    tb_ = cnt["tci"] % 2
                    cnt["tci"] += 1
                    for kb in range(k0, k1):
                        P.op("pe", lambda e, tb_=tb_, kb=kb, k0=k0, b=b: e.transpose(
                            psT[tb_][:, (kb - k0) * 128:(kb - k0 + 1) * 128], W[b][:, kb * 128:(kb + 1) * 128],
                            self.ident_bf[:]), reads=["W0"], writes=["pT%d" % tb_])
                    P.op("act", lambda e, tb_=tb_, k0=k0, k1=k1, b=b: e.copy(
                        out=WT[b][:, k0 * 128:k1 * 128], in_=psT[tb_][:, 0:(k1 - k0) * 128]),
                        reads=["pT%d" % tb_], writes=["WT_%d" % k0])
                for kb in range(qb + 1):
                    P.op("pe", lambda e, b=b, kb=kb, hb=hb, qb=qb: e.matmul(
                        psO[b][:, 0:128], vh[hb][:, kb, :], WT[b][:, kb * 128:(kb + 1) * 128],
                        start=(kb == 0), stop=(kb == qb)),
                        reads=["v%d" % hb, "WT_%d" % ((kb // 8) * 8)], writes=["pO%d" % b])
                P.op("act", lambda e, b=b, hb=hb, qb=qb: e.copy(
                    out=oh[hb][:, qb * 128:(qb + 1) * 128], in_=psO[b][:, 0:128]),
                    reads=["pO%d" % b], writes=["o0"])
                if qb == NB - 1:
                    rows = slice(h * 128, (h + 1) * 128)
                    P.dma("sp", outT[rows, :], oh[hb][:], reads=["o0"])

            sched = [(h, qb) for h in range(n_heads) for qb in range(NB)]
            for it, (h, qb) in enumerate(sched):
                if qb == 0:
                    load_head(h)
                phase_a(h, qb, it % 2, it % 3)
                if it >= 1:
                    ph, pq = sched[it - 1]
                    phase_b(ph, pq, (it - 1) % 2, (it - 1) % 3)
            ph, pq = sched[-1]
            phase_b(ph, pq, (len(sched) - 1) % 2, (len(sched) - 1) % 3)
            P.emit()

    def stage_gmlp(self, uT, z2g, ln_g, w_s, b_s, outT, n_groups=16):
        nc, P = self.nc, self.P
        S = self.S
        G = n_groups
        with ExitStack() as st:
            T = self.mkT(st)
            lng = T("g_lng", [128, G * 128], F32)
            brow = T("g_brow", [1, G * 128], F32)
            wst = [T("g_ws%d" % i, [128, 128], F32) for i in range(2)]
            wmT = T("g_wmT", [128, G, 128], BF16)
            z2c = [T("g_z2%d" % i, [128, G, 128], F32) for i in range(2)]
            uc = [T("g_u%d" % i, [128, G, 128], BF16) for i in range(2)]
            stats = T("g_stats", [128, G, 6], F32)
            mv = T("g_mv", [128, G, 2], F32)
            rstd = T("g_rstd", [128, G], F32)
            vt = T("g_vt", [128, G, 128], F32)
            vn = [T("g_vn%d" % i, [128, G, 128], BF16) for i in range(2)]
            og = [T("g_og%d" % i, [128, G, 128], BF16) for i in range(2)]
            pw = self.PS(st, "g_pw", [128, 512], F32)
            pm = [self.PS(st, "g_pm%d" % i, [128, 512], F32) for i in range(4)]
            P.dma("sp", lng[:], ln_g.rearrange("g c -> (g c)").partition_broadcast(128), writes=["lng"])
            P.dma("sp", brow[:], b_s.rearrange("g t -> (g t)").partition_broadcast(1), writes=["brow"])
            for g in range(G):
                w = wst[g % 2]
                wk = "ws%d" % (g % 2)
                P.dma("sp", w[:], w_s[g, :, :], writes=[wk])
                P.op("pool", lambda e, w=w: e.affine_select(
                    out=w[:], in_=w[:], pattern=[[-1, 128]], compare_op=ALU.is_ge, fill=0.0, base=0,
                    channel_multiplier=1), reads=[wk], writes=[wk])
                P.op("pe", lambda e, w=w: e.transpose(pw[:, 0:128], w[:], self.ident_f[:]),
                     reads=[wk], writes=["pw"])
                P.op("act", lambda e, g=g: e.copy(out=wmT[:, g, :], in_=pw[:, 0:128]), reads=["pw"],
                     writes=["wmT"])
            pi = 0
            for n in range(S // 128):
                b = n % 2
                ts = slice(n * 128, (n + 1) * 128)
                P.dma("sp", z2c[b][:], z2g[ts, :].rearrange("t (g c) -> t g c", g=G), writes=["z2%d" % b])
                P.dma("sp", uc[b][:], uT[:, ts].rearrange("(g c) t -> c g t", g=G), writes=["u%d" % b])
                for g in range(G):
                    P.op("dve", lambda e, b=b, g=g: e.bn_stats(out=stats[:, g, :], in_=z2c[b][:, g, :]),
                         reads=["z2%d" % b], writes=["st%d" % g])
                    P.op("dve", lambda e, g=g: e.bn_aggr(out=mv[:, g, :], in_=stats[:, g, :]),
                         reads=["st%d" % g], writes=["mv%d" % g])
                mvk = ["mv%d" % g for g in range(G)]
                P.op("dve", lambda e: e.tensor_scalar(out=rstd[:], in0=mv[:, :, 1], scalar1=EPS, scalar2=None,
                                                      op0=ALU.add), reads=mvk, writes=["rstd"])
                P.op("act", lambda e: e.activation(out=rstd[:], in_=rstd[:], func=AF.Sqrt),
                     reads=["rstd"], writes=["rstd"])
                P.op("dve", lambda e: e.reciprocal(out=rstd[:], in_=rstd[:]), reads=["rstd"], writes=["rstd"])
                for g in range(G):
                    P.op("dve", lambda e, b=b, g=g: e.tensor_scalar(
                        out=vt[:, g, :], in0=z2c[b][:, g, :], scalar1=mv[:, g, 0:1], scalar2=rstd[:, g:g + 1],
                        op0=ALU.subtract, op1=ALU.mult), reads=["z2%d" % b, "mv%d" % g, "rstd"], writes=["vt"])
                P.op("dve", lambda e, b=b: e.tensor_tensor(
                    out=vn[b][:].rearrange("p g c -> p (g c)"), in0=vt[:].rearrange("p g c -> p (g c)"),
                    in1=lng[:], op=ALU.mult), reads=["vt", "lng"], writes=["vn%d" % b])
                for g4 in range(G // 4):
                    pb = pi % 4
                    pi += 1
                    for gg in range(4):
                        g = g4 * 4 + gg
                        P.op("pe", lambda e, pb=pb, gg=gg, g=g, b=b: e.matmul(
                            pm[pb][:, gg * 128:(gg + 1) * 128], vn[b][:, g, :], wmT[:, g, :],
                            start=True, stop=False), reads=["vn%d" % b, "wmT"], writes=["pm%d" % pb])
                        P.op("pe", lambda e, pb=pb, gg=gg, g=g: e.matmul(
                            pm[pb][:, gg * 128:(gg + 1) * 128], self.ones_f[0:1, :], brow[0:1, g * 128:(g + 1) * 128],
                            start=False, stop=True), reads=["brow"], writes=["pm%d" % pb])
                    P.op("dve", lambda e, pb=pb, g4=g4, b=b: e.tensor_tensor(
                        out=og[b][:, g4 * 4:(g4 + 1) * 4, :].rearrange("p g c -> p (g c)"), in0=pm[pb][:],
                        in1=uc[b][:, g4 * 4:(g4 + 1) * 4, :].rearrange("p g c -> p (g c)"), op=ALU.mult),
                        reads=["pm%d" % pb, "u%d" % b], writes=["og%d_%d" % (b, g4)])
                P.dma("sp", outT[:, ts].rearrange("(g c) t -> c g t", g=G), og[b][:],
                      reads=["og%d_%d" % (b, g4) for g4 in range(G // 4)])
            P.emit()

    def stage_ssm_prep(self, lam_re, lam_im, log_dt, b_re, b_im, c_re, c_im, MB, MP, NL, G=256):
        nc, P = self.nc, self.P
        GB = 32
        PI2 = math.pi / 2
        with ExitStack() as st:
            T = self.mkT(st)
            NS = 96
            PW = T("sp_pw", [128, NS, G], F32)
            cnt = [0]

            def new():
                i = cnt[0]
                cnt[0] += 1
                assert i < NS
                return (PW[:, i, :], "pw%d" % i)

            def TT(o, a, b, op, eng="dve"):
                P.op(eng, lambda e: e.tensor_tensor(out=o[0], in0=a[0], in1=b[0], op=op),
                     reads=[a[1], b[1]], writes=[o[1]])

            def TS(o, a, s1, op0, s2=None, op1=None):
                if op1 is None:
                    P.op("dve", lambda e: e.tensor_scalar(out=o[0], in0=a[0], scalar1=s1, scalar2=None, op0=op0),
                         reads=[a[1]], writes=[o[1]])
                else:
                    P.op("dve", lambda e: e.tensor_scalar(out=o[0], in0=a[0], scalar1=s1, scalar2=s2, op0=op0,
                                                          op1=op1), reads=[a[1]], writes=[o[1]])

            def STT(o, a, s, b, op0, op1):
                P.op("dve", lambda e: e.scalar_tensor_tensor(out=o[0], in0=a[0], scalar=s, in1=b[0], op0=op0,
                                                             op1=op1), reads=[a[1], b[1]], writes=[o[1]])

            def ACTF(o, a, func, scale=1.0, bias=0.0):
                P.op("act", lambda e: e.activation(out=o[0], in_=a[0], func=func, scale=scale, bias=bias),
                     reads=[a[1]], writes=[o[1]])

            t1, t2 = new(), new()

            def cmul(dr, di, ar, ai, br, bi):
                TT(t1, ar, br, ALU.mult)
                TT(t2, ai, bi, ALU.mult)
                TT(dr, t1, t2, ALU.subtract)
                TT(t1, ar, bi, ALU.mult)
                TT(t2, ai, br, ALU.mult)
                TT(di, t1, t2, ALU.add)

            def csq(dr, di, ar, ai):
                TT(t1, ar, ar, ALU.mult)
                TT(t2, ai, ai, ALU.mult)
                STT(di, ar, 2.0, ai, ALU.mult, ALU.mult)
                TT(dr, t1, t2, ALU.subtract)

            nat = T("sp_nat", [128, 128], F32)
            pst = self.PS(st, "sp_pst", [128, 512], F32)
            psA = [self.PS(st, "sp_psA%d" % i, [128, 512], F32) for i in range(2)]
            lrT, liT, ldt = new(), new(), new()
            for arr, dst in ((lam_re, lrT), (lam_im, liT)):
                for gt in range(G // 128):
                    P.dma("sp", nat[:, 0:64], arr[gt * 128:(gt + 1) * 128, :], writes=["nat"])
                    P.dma("sp", nat[:, 64:128], arr[gt * 128:(gt + 1) * 128, :], writes=["nat"])
                    P.op("pe", lambda e: e.transpose(pst[:, 0:128], nat[:], self.ident_f[:]), reads=["nat"],
                         writes=["pst"])
                    P.op("act", lambda e, dst=dst, gt=gt: e.copy(out=dst[0][:, gt * 128:(gt + 1) * 128],
                                                                 in_=pst[:, 0:128]), reads=["pst"], writes=[dst[1]])
            P.dma("sp", ldt[0], log_dt.rearrange("o g -> (o g)").partition_broadcast(128), writes=[ldt[1]])
            dt, lr, x1, th = new(), new(), new(), new()
            ACTF(dt, ldt, AF.Exp)
            TS(lr, lrT, -1e-4, ALU.min)
            TT(x1, lr, dt, ALU.mult)
            TT(th, liT, dt, ALU.mult)
            mag, sn, cs = new(), new(), new()
            ACTF(mag, x1, AF.Exp, scale=1.0 / 32)
            ACTF(sn, th, AF.Sin, scale=1.0 / 32)
            ACTF(cs, th, AF.Sin, scale=1.0 / 32, bias=PI2)
            cur = (new(), new())
            TT(cur[0], mag, cs, ALU.mult)
            TT(cur[1], mag, sn, ALU.mult)
            for _ in range(5):
                nx = (new(), new())
                csq(nx[0], nx[1], cur[0], cur[1])
                cur = nx
            pw = {1: cur}
            for i in range(2, 9):
                pw[i] = (new(), new())
            csq(*pw[2], *pw[1])
            cmul(*pw[3], *pw[2], *pw[1])
            csq(*pw[4], *pw[2])
            cmul(*pw[5], *pw[4], *pw[1])
            csq(*pw[6], *pw[3])
            cmul(*pw[7], *pw[6], *pw[1])
            csq(*pw[8], *pw[4])
            big = [pw[8]]
            for k in range(1, NL):
                nx = (new(), new())
                csq(nx[0], nx[1], big[-1][0], big[-1][1])
                big.append(nx)
            ipw = {}
            n2 = new()
            for s in range(1, 8):
                ipw[s] = (new(), new())
                TT(t1, pw[s][0], pw[s][0], ALU.mult)
                TT(t2, pw[s][1], pw[s][1], ALU.mult)
                TT(n2, t1, t2, ALU.add)
                P.op("dve", lambda e: e.reciprocal(out=n2[0], in_=n2[0]), reads=[n2[1]], writes=[n2[1]])
                TT(ipw[s][0], pw[s][0], n2, ALU.mult)
                STT(ipw[s][1], pw[s][1], -1.0, n2, ALU.mult, ALU.mult)
            nr, den, fre, fim = new(), new(), new(), new()
            are, aim = pw[1]
            TS(nr, are, -1.0, ALU.add)
            TT(t1, lr, lr, ALU.mult)
            TT(t2, liT, liT, ALU.mult)
            TT(den, t1, t2, ALU.add)
            P.op("dve", lambda e: e.reciprocal(out=den[0], in_=den[0]), reads=[den[1]], writes=[den[1]])
            TT(t1, nr, lr, ALU.mult)
            TT(t2, aim, liT, ALU.mult)
            TT(t1, t1, t2, ALU.add)
            TT(fre, t1, den, ALU.mult)
            TT(t1, aim, lr, ALU.mult)
            TT(t2, nr, liT, ALU.mult)
            TT(t1, t1, t2, ALU.subtract)
            TT(fim, t1, den, ALU.mult)
            zs = [pw[7]] + big
            sims = []
            for z in zs:
                sm = new()
                P.op("dve", lambda e, sm=sm, z=z: e.tensor_copy(out=sm[0][0:64, :], in_=z[1][0][0:64, :]),
                     reads=[z[1][1]], writes=[sm[1]])
                P.op("dve", lambda e, sm=sm, z=z: e.tensor_scalar(out=sm[0][64:128, :], in0=z[1][0][64:128, :],
                                                                  scalar1=-1.0, scalar2=None, op0=ALU.mult),
                     reads=[z[1][1]], writes=[sm[1]])
                sims.append(sm)
            jsw = T("sp_jsw", [128, 128], F32)
            P.op("pool", lambda e: e.memset(jsw[:], 0.0), writes=["jsw"])
            P.op("pool", lambda e: e.tensor_copy(out=jsw[0:64, 64:128], in_=self.ident_f[0:64, 0:64]),
                 reads=["jsw"], writes=["jsw"])
            P.op("pool", lambda e: e.tensor_copy(out=jsw[64:128, 0:64], in_=self.ident_f[64:128, 64:128]),
                 reads=["jsw"], writes=["jsw"])
            mT0 = T("sp_mT0", [128, 8, 16], F32)
            P.op("pool", lambda e: e.memset(mT0[:], 1.0), writes=["mT0"])
            P.op("pool", lambda e: e.affine_select(
                out=mT0[:], in_=mT0[:], pattern=[[16, 8], [0, 16]], compare_op=ALU.is_ge, fill=0.0, base=15,
                channel_multiplier=-1), reads=["mT0"], writes=["mT0"])

            Bre = T("sp_Bre", [128, GB, 16], F32)
            Bim = T("sp_Bim", [128, GB, 16], F32)
            Wre = T("sp_Wre", [128, GB, 16], F32)
            Wim = T("sp_Wim", [128, GB, 16], F32)
            CTre = T("sp_CTre", [128, GB, 16], F32)
            CTim = T("sp_CTim", [128, GB, 16], F32)
            u1 = T("sp_u1", [128, GB, 16], F32)
            u2 = T("sp_u2", [128, GB, 16], F32)
            Xs = T("sp_Xs", [128, GB, 8, 16], F32)
            Ys = T("sp_Ys", [128, GB, 9, 16], F32)
            natc = T("sp_natc", [128, 128], F32)
            Mt = [T("sp_Mt%d" % i, [128, 128], F32) for i in range(2)]
            stB = [T("sp_stB%d" % i, [128, 3, 128], BF16) for i in range(2)]
            stP = [T("sp_stP%d" % i, [128, NL, 128], F32) for i in range(2)]
            c_re2 = c_re.rearrange("g c p -> (g c) p")
            c_im2 = c_im.rearrange("g c p -> (g c) p")

            def bc(slot, g0):
                return slot[0][:, g0:g0 + GB].unsqueeze(2).broadcast_to([128, GB, 16])

            def hop(eng, fn, reads, writes):
                P.op(eng, fn, reads=reads, writes=writes)

            for gb in range(G // GB):
                g0 = gb * GB
                for half in range(2):
                    hs = slice(half * 64, (half + 1) * 64)
                    P.dma("sp", Bre[hs], b_re[g0:g0 + GB].rearrange("g p c -> p g c"), writes=["Bre"])
                    P.dma("sp", Bim[hs], b_im[g0:g0 + GB].rearrange("g p c -> p g c"), writes=["Bim"])
                fr, fi = bc(fre, g0), bc(fim, g0)
                hop("dve", lambda e, fr=fr: e.tensor_tensor(out=u1[:], in0=Bre[:], in1=fr, op=ALU.mult),
                    ["Bre", fre[1]], ["u1"])
                hop("dve", lambda e, fi=fi: e.tensor_tensor(out=u2[:], in0=Bim[:], in1=fi, op=ALU.mult),
                    ["Bim", fim[1]], ["u2"])
                hop("dve", lambda e: e.tensor_tensor(out=Wre[:], in0=u1[:], in1=u2[:], op=ALU.subtract),
                    ["u1", "u2"], ["Wre"])
                hop("dve", lambda e, fr=fr: e.tensor_tensor(out=u1[:], in0=Bim[:], in1=fr, op=ALU.mult),
                    ["Bim", fre[1]], ["u1"])
                hop("dve", lambda e, fi=fi: e.tensor_tensor(out=u2[:], in0=Bre[:], in1=fi, op=ALU.mult),
                    ["Bre", fim[1]], ["u2"])
                hop("dve", lambda e: e.tensor_tensor(out=Wim[:], in0=u1[:], in1=u2[:], op=ALU.add),
                    ["u1", "u2"], ["Wim"])
                top, bot = slice(0, 64), slice(64, 128)
                hop("dve", lambda e: e.tensor_copy(out=Xs[top, :, 0, :], in_=Wre[top]), ["Wre"], ["Xs"])
                hop("dve", lambda e: e.tensor_copy(out=Xs[bot, :, 0, :], in_=Wim[bot]), ["Wim"], ["Xs"])
                for s in range(1, 8):
                    ir, ii = bc(ipw[s][0], g0), bc(ipw[s][1], g0)
                    rk = [ipw[s][0][1], ipw[s][1][1]]
                    hop("dve", lambda e, ir=ir: e.tensor_tensor(out=u1[top], in0=Wre[top], in1=ir[top], op=ALU.mult),
                        ["Wre"] + rk, ["u1"])
                    hop("dve", lambda e, ii=ii: e.tensor_tensor(out=u2[top], in0=Wim[top], in1=ii[top], op=ALU.mult),
                        ["Wim"] + rk, ["u2"])
                    hop("dve", lambda e, s=s: e.tensor_tensor(out=Xs[top, :, s, :], in0=u1[top], in1=u2[top],
                                                              op=ALU.subtract), ["u1", "u2"], ["Xs"])
                    hop("dve", lambda e, ir=ir: e.tensor_tensor(out=u1[bot], in0=Wim[bot], in1=ir[bot], op=ALU.mult),
                        ["Wim"] + rk, ["u1"])
                    hop("dve", lambda e, ii=ii: e.tensor_tensor(out=u2[bot], in0=Wre[bot], in1=ii[bot], op=ALU.mult),
                        ["Wre"] + rk, ["u2"])
                    hop("dve", lambda e, s=s: e.tensor_tensor(out=Xs[bot, :, s, :], in0=u1[bot], in1=u2[bot],
                                                              op=ALU.add), ["u1", "u2"], ["Xs"])
                for arr2, dstT, dk in ((c_re2, CTre, "CTre"), (c_im2, CTim, "CTim")):
                    for i in range(GB * 16 // 128):
                        r0 = g0 * 16 + i * 128
                        P.dma("sp", natc[:, 0:64], arr2[r0:r0 + 128, :], writes=["natc"])
                        P.dma("sp", natc[:, 64:128], arr2[r0:r0 + 128, :], writes=["natc"])
                        P.op("pe", lambda e: e.transpose(pst[:, 128:256], natc[:], self.ident_f[:]),
                             reads=["natc"], writes=["pst2"])
                        P.op("act", lambda e, dstT=dstT, i=i: e.copy(
                            out=dstT[:, i * 8:(i + 1) * 8, :], in_=pst[:, 128:256].rearrange("p (g c) -> p g c", c=16)),
                            reads=["pst2"], writes=[dk])
                hop("dve", lambda e: e.tensor_copy(out=Ys[top, :, 0, :], in_=CTre[top]), ["CTre"], ["Ys"])
                hop("dve", lambda e: e.tensor_scalar(out=Ys[bot, :, 0, :], in0=CTim[bot], scalar1=-1.0, scalar2=None,
                                                     op0=ALU.mult), ["CTim"], ["Ys"])
                for t in range(1, 9):
                    pr, pi_ = bc(pw[t][0], g0), bc(pw[t][1], g0)
                    rk = [pw[t][0][1], pw[t][1][1]]
                    hop("dve", lambda e, pr=pr: e.tensor_tensor(out=u1[top], in0=CTre[top], in1=pr[top], op=ALU.mult),
                        ["CTre"] + rk, ["u1"])
                    hop("dve", lambda e, pi_=pi_: e.tensor_tensor(out=u2[top], in0=CTim[top], in1=pi_[top],
                                                                  op=ALU.mult), ["CTim"] + rk, ["u2"])
                    hop("dve", lambda e, t=t: e.tensor_tensor(out=Ys[top, :, t, :], in0=u1[top], in1=u2[top],
                                                              op=ALU.subtract), ["u1", "u2"], ["Ys"])
                    hop("dve", lambda e, pr=pr: e.tensor_tensor(out=u1[bot], in0=CTim[bot], in1=pr[bot], op=ALU.mult),
                        ["CTim"] + rk, ["u1"])
                    hop("dve", lambda e, pi_=pi_: e.tensor_tensor(out=u2[bot], in0=CTre[bot], in1=pi_[bot],
                                                                  op=ALU.mult), ["CTre"] + rk, ["u2"])
                    hop("dve", lambda e, t=t: e.scalar_tensor_tensor(
                        out=Ys[bot, :, t, :], in0=u1[bot], scalar=-1.0, in1=u2[bot], op0=ALU.mult, op1=ALU.subtract),
                        ["u1", "u2"], ["Ys"])
                for gl in range(GB):
                    g = g0 + gl
                    b = g % 2
                    Xg = Xs[:, gl, :, :].rearrange("p s c -> p (s c)")
                    Y0 = Ys[:, gl, 0:8, :].rearrange("p t c -> p (t c)")
                    Y1 = Ys[:, gl, 1:9, :].rearrange("p t c -> p (t c)")
                    pa = psA[b]
                    P.op("pe", lambda e, pa=pa, Xg=Xg, Y0=Y0: e.matmul(pa[:, 0:128], Xg, Y0, start=True, stop=True),
                         reads=["Xs", "Ys"], writes=["psA%d" % b])
                    P.op("dve", lambda e, pa=pa, b=b: e.tensor_tensor(
                        out=stB[b][:, 0, :], in0=pa[:, 0:128], in1=mT0[:].rearrange("p t c -> p (t c)"),
                        op=ALU.mult), reads=["psA%d" % b, "mT0"], writes=["stB%d" % b])
                    P.op("dve", lambda e, b=b, g=g: e.tensor_scalar(
                        out=Mt[b][:], in0=self.ident_f[:], scalar1=zs[0][0][0][:, g:g + 1], scalar2=None,
                        op0=ALU.mult), reads=[zs[0][0][1]], writes=["Mt%d" % b])
                    P.op("dve", lambda e, b=b, g=g: e.scalar_tensor_tensor(
                        out=Mt[b][:], in0=jsw[:], scalar=sims[0][0][:, g:g + 1], in1=Mt[b][:],
                        op0=ALU.mult, op1=ALU.add), reads=["jsw", sims[0][1], "Mt%d" % b], writes=["Mt%d" % b])
                    P.op("pe", lambda e, pa=pa, Xg=Xg, b=b: e.matmul(pa[:, 128:256], Xg, Mt[b][:], start=True,
                                                                     stop=True),
                         reads=["Xs", "Mt%d" % b], writes=["psA%d" % b])
                    P.op("act", lambda e, pa=pa, b=b: e.copy(out=stB[b][:, 1, :], in_=pa[:, 128:256]),
                         reads=["psA%d" % b], writes=["stB%d" % b])
                    P.op("act", lambda e, b=b, Y1=Y1: e.copy(out=stB[b][:, 2, :], in_=Y1),
                         reads=["Ys"], writes=["stB%d" % b])
                    P.dma("sp", MB[g], stB[b][:], reads=["stB%d" % b])
                    for k in range(NL):
                        z = zs[1 + k]
                        sm = sims[1 + k]
                        P.op("dve", lambda e, b=b, g=g, k=k, z=z: e.tensor_scalar(
                            out=stP[b][:, k, :], in0=self.ident_f[:], scalar1=z[0][0][:, g:g + 1], scalar2=None,
                            op0=ALU.mult), reads=[z[0][1]], writes=["stP%d" % b])
                        P.op("dve", lambda e, b=b, g=g, k=k, sm=sm: e.scalar_tensor_tensor(
                            out=stP[b][:, k, :], in0=jsw[:], scalar=sm[0][:, g:g + 1], in1=stP[b][:, k, :],
                            op0=ALU.mult, op1=ALU.add), reads=["jsw", sm[1], "stP%d" % b], writes=["stP%d" % b])
                    P.dma("sp", MP[g], stP[b][:], reads=["stP%d" % b])
            P.emit()

    def stage_ssm_run(self, utok, MB, MP, d_ap, yT, NL, G=256):
        nc, P = self.nc, self.P
        S = self.S
        NJ = S // 8
        JP = min(128, NJ)
        NJT = NJ // JP
        GBK = 32
        NW = 4
        NB2 = 2 * NW
        with ExitStack() as st:
            T = self.mkT(st)
            Ust = [T("r_Ust%d" % i, [128, 8, 512], BF16) for i in range(2)]
            U2 = T("r_U2", [128, NJT, GBK, 8, 16], BF16)
            Yblk = T("r_Y", [128, NJT, 8, 512], BF16)
            dbc = T("r_d", [128, 512], F32)
            mb = [T("r_mb%d" % i, [128, 3, 128], BF16) for i in range(NB2)]
            mp = [T("r_mp%d" % i, [128, NL, 128], F32) for i in range(NB2)]
            Ug = [T("r_Ug%d" % i, [128, NJ], BF16) for i in range(NB2)]
            Sg = [T("r_Sg%d" % i, [128, NJ], F32) for i in range(NB2)]
            Sp = [T("r_Sp%d" % i, [128, NJ], BF16) for i in range(NB2)]
            Yg = [T("r_Yg%d" % i, [128, NJ], BF16) for i in range(NB2)]
            tmp = T("r_tmp", [128, 4, 512], F32)
            g1 = T("r_g1", [128, 4, 512], F32)
            g2 = T("r_g2", [128, 4, 512], F32)
            yact = [T("r_ya%d" % i, [128, 4, 512], BF16) for i in range(2)]
            yTb = [T("r_yT%d" % i, [128, 4, JP * 8], BF16) for i in range(1)] * 2
            psU = self.PS(st, "r_psU", [128, 1024], BF16)
            ring = [self.PS(st, "r_ring%d" % i, [128, 512], F32) for i in range(NW)]
            psYT = self.PS(st, "r_psYT", [128, 1024], BF16)
            psT2 = self.PS(st, "r_psT2", [128, 1024], BF16)
            for i in range(NB2):
                P.op("pool", lambda e, i=i: e.memset(Sp[i][:], 0.0), writes=["Sp%d" % i])
            wv = 0
            ti = 0
            for blk in range(G // GBK):
                ch0 = blk * 512
                for jt in range(NJT):
                    ub = jt % 2
                    P.dma("pool", Ust[ub][0:JP], utok[jt * JP * 8:(jt + 1) * JP * 8, ch0:ch0 + 512].rearrange(
                        "(jp s) c -> jp s c", s=8), writes=["Ust%d" % ub])
                    P.op("dve", lambda e, ub=ub, jt=jt: e.tensor_copy(
                        out=U2[0:JP, jt], in_=Ust[ub][0:JP].rearrange("p s (g c) -> p g s c", c=16)),
                        reads=["Ust%d" % ub], writes=["U2"])
                P.dma("sp", dbc[0:JP], d_ap[0, ch0:ch0 + 512].partition_broadcast(JP), writes=["dbc"])
                cut = getattr(self, "cut", 9)
                for w0 in range(0, GBK, NW):
                    if cut < 2:
                        break
                    sl = [(wv % 2) * NW + i for i in range(NW)]
                    wv += 1
                    gls = [w0 + i for i in range(NW)]
                    for i in range(NW):
                        g = blk * GBK + gls[i]
                        P.dma("sp", mb[sl[i]][:], MB[g], writes=["mb%d" % sl[i]])
                        P.dma("sp", mp[sl[i]][:], MP[g], writes=["mp%d" % sl[i]])
                    if cut < 2.5:
                        continue
                    for i in range(NW):
                        s_, gl = sl[i], gls[i]
                        hf = 0
                        for jt in range(NJT):
                            P.op("pe", lambda e, jt=jt, gl=gl, hf=hf: e.transpose(
                                psU[:, hf * 512 + jt * JP: hf * 512 + (jt + 1) * JP],
                                U2[0:JP, jt, gl].rearrange("p s c -> p (s c)"), self.ident_bf[0:JP, 0:JP]),
                                reads=["U2"], writes=["psU%d" % hf])
                        if cut < 2.7:
                            continue
                        P.op("act", lambda e, s_=s_, hf=hf: e.copy(out=Ug[s_][:], in_=psU[:, hf * 512:hf * 512 + NJ]),
                             reads=["psU%d" % hf], writes=["Ug%d" % s_])
                    if cut < 3:
                        continue
                    for i in range(NW):
                        s_ = sl[i]
                        P.op("pe", lambda e, i=i, s_=s_: e.matmul(ring[i][:, 0:NJ], mb[s_][:, 1, :], Ug[s_][:],
                                                                  start=True, stop=True),
                             reads=["mb%d" % s_, "Ug%d" % s_], writes=["ring%d" % i])
                    for i in range(NW):
                        s_ = sl[i]
                        P.op("dve", lambda e, i=i, s_=s_: e.tensor_copy(out=Sg[s_][:], in_=ring[i][:, 0:NJ]),
                             reads=["ring%d" % i], writes=["Sg%d" % s_])
                    for k in range(NL):
                        sh = 1 << k
                        for i in range(NW):
                            s_ = sl[i]
                            P.op("pe", lambda e, i=i, s_=s_, k=k, sh=sh: e.matmul(
                                ring[i][:, 0:NJ - sh], mp[s_][:, k, :], Sg[s_][:, 0:NJ - sh], start=True, stop=True),
                                reads=["mp%d" % s_, "Sg%d" % s_], writes=["ring%d" % i])
                        for i in range(NW):
                            s_ = sl[i]
                            P.op("dve", lambda e, i=i, s_=s_, sh=sh: e.tensor_tensor(
                                out=Sg[s_][:, sh:NJ], in0=ring[i][:, 0:NJ - sh], in1=Sg[s_][:, sh:NJ], op=ALU.add),
                                reads=["ring%d" % i, "Sg%d" % s_], writes=["Sg%d" % s_])
                    if cut < 4:
                        continue
                    for i in range(NW):
                        s_ = sl[i]
                        P.op("act", lambda e, s_=s_: e.copy(out=Sp[s_][:, 1:NJ], in_=Sg[s_][:, 0:NJ - 1]),
                             reads=["Sg%d" % s_], writes=["Sp%d" % s_])
                    for i in range(NW):
                        s_ = sl[i]
                        P.op("pe", lambda e, i=i, s_=s_: e.matmul(ring[i][:, 0:NJ], mb[s_][:, 0, :], Ug[s_][:],
                                                                  start=True, stop=False),
                             reads=["mb%d" % s_, "Ug%d" % s_], writes=["ring%d" % i])
                        P.op("pe", lambda e, i=i, s_=s_: e.matmul(ring[i][:, 0:NJ], mb[s_][:, 2, :], Sp[s_][:],
                                                                  start=False, stop=True),
                             reads=["mb%d" % s_, "Sp%d" % s_], writes=["ring%d" % i])
                        P.op("act", lambda e, i=i, s_=s_: e.copy(out=Yg[s_][:], in_=ring[i][:, 0:NJ]),
                             reads=["ring%d" % i], writes=["Yg%d" % s_])
                    for i in range(NW):
                        s_, gl = sl[i], gls[i]
                        hf = 0
                        for jt in range(NJT):
                            P.op("pe", lambda e, jt=jt, s_=s_, hf=hf: e.transpose(
                                psYT[0:JP, hf * 512 + jt * 128: hf * 512 + (jt + 1) * 128],
                                Yg[s_][:, jt * JP:(jt + 1) * JP], self.ident_bf[:]),
                                reads=["Yg%d" % s_], writes=["psYT%d" % hf])
                        P.op("dve", lambda e, gl=gl, hf=hf: e.tensor_copy(
                            out=Yblk[0:JP, :, :, gl * 16:(gl + 1) * 16],
                            in_=psYT[0:JP, hf * 512:hf * 512 + NJT * 128].rearrange("p (j t c) -> p j t c", j=NJT, t=8)),
                            reads=["psYT%d" % hf], writes=["Yblk"])
                for jt in range(NJT):
                    if cut < 5:
                        break
                    for th in range(2):
                        tsl = slice(th * 4, (th + 1) * 4)
                        yb = ti % 2
                        ti += 1
                        P.op("dve", lambda e, jt=jt, tsl=tsl: e.tensor_tensor(
                            out=tmp[0:JP].rearrange("p t (g c) -> p g t c", c=16), in0=U2[0:JP, jt, :, tsl, :],
                            in1=dbc[0:JP].rearrange("p (g c) -> p g c", c=16).unsqueeze(2).broadcast_to(
                                [JP, GBK, 4, 16]), op=ALU.mult),
                            reads=["U2", "dbc"], writes=["tmp"])
                        P.op("dve", lambda e, jt=jt, tsl=tsl: e.tensor_tensor(
                            out=tmp[0:JP], in0=tmp[0:JP], in1=Yblk[0:JP, jt, tsl, :], op=ALU.add),
                            reads=["tmp", "Yblk"], writes=["tmp"])
                        self.gelu_ops("r", tmp[0:JP], ["tmp"], yact[yb][0:JP], "ya%d" % yb,
                                      g1[0:JP], "g1", g2[0:JP], "g2")
                        for cb in range(4):
                            hf = 0
                            for tt in range(4):
                                P.op("pe", lambda e, yb=yb, tt=tt, cb=cb, hf=hf: e.transpose(
                                    psT2[:, hf * 512 + tt * JP: hf * 512 + (tt + 1) * JP],
                                    yact[yb][0:JP, tt, cb * 128:(cb + 1) * 128], self.ident_bf[0:JP, 0:JP]),
                                    reads=["ya%d" % yb], writes=["psT2%d" % hf])
                            eng = "act" if cb % 2 == 0 else "dve"
                            src = psT2[:, hf * 512:hf * 512 + 4 * JP].rearrange("p (t j) -> p t j", t=4)
                            yb_ = jt % 2
                            dst = yTb[yb_][:, cb, :].rearrange("p (j t) -> p j t", t=8)[:, :, tsl].rearrange(
                                "p j t -> p t j")
                            if eng == "act":
                                P.op("act", lambda e, src=src, dst=dst: e.copy(out=dst, in_=src),
                                     reads=["psT2%d" % hf], writes=["yTb0"])
                            else:
                                P.op("dve", lambda e, src=src, dst=dst: e.tensor_copy(out=dst, in_=src),
                                     reads=["psT2%d" % hf], writes=["yTb0"])
                    P.dma("sp", yT[ch0:ch0 + 512, jt * JP * 8:(jt + 1) * JP * 8].rearrange("(cb p) t -> p cb t", p=128),
                          yTb[jt % 2][:], reads=["yTb0"])
            P.emit()


W_SPECS = [
    ("mix0_norm_g", [1, D]), ("mix0_ada_w", [D, 3 * D]), ("mix0_ada_b", [1, 3 * D]),
    ("mix0_w_in", [D, 10240]), ("gm_ln_g", [16, 128]), ("gm_w_s", [16, 128, 128]), ("gm_b_s", [16, 128]),
    ("mix0_w_out", [D, D]),
    ("ffn0_norm_g", [1, D]), ("ffn0_ada_w", [D, 3 * D]), ("ffn0_ada_b", [1, 3 * D]),
    ("ffn0_w_gate", [D, FFN_DENSE]), ("ffn0_w_up", [D, FFN_DENSE]), ("ffn0_w_down", [FFN_DENSE, D]),
    ("mix1_norm_g", [1, D]), ("mix1_ada_w", [D, 3 * D]), ("mix1_ada_b", [1, 3 * D]),
    ("ssm_w_in", [D, D]), ("ssm_lam_re", [256, 64]), ("ssm_lam_im", [256, 64]), ("ssm_log_dt", [1, 256]),
    ("ssm_b_re", [256, 64, 16]), ("ssm_b_im", [256, 64, 16]), ("ssm_c_re", [256, 16, 64]),
    ("ssm_c_im", [256, 16, 64]), ("ssm_d", [1, D]), ("glu_w_a", [D, D]), ("glu_w_b", [D, D]),
    ("moe_norm_g", [1, D]), ("moe_ada_w", [D, 3 * D]), ("moe_ada_b", [1, 3 * D]),
    ("moe_w_router", [D, N_EXP]), ("moe_w_gate", [N_EXP, D, FFN_EXP]), ("moe_w_up", [N_EXP, D, FFN_EXP]),
    ("moe_w_down", [N_EXP, FFN_EXP, D]), ("final_norm_g", [1, D]),
]


def build_model(S):
    B = Builder(S, None)
    NL = int(round(math.log2(S // 8)))
    xT = B.ext_in("xT", [D, S])
    c = B.ext_in("c", [1, D])
    w = {n: B.ext_in(n, shp) for n, shp in W_SPECS}
    outT = B.ext_out("outT", [D, S])
    hT = B.scratch("hT", [D, S], BF16)
    xres = B.scratch("xres", [D, S], F32)
    qT = B.scratch("qT", [2048, S], BF16)
    kT = B.scratch("kT", [2048, S], BF16)
    uT = B.scratch("uT", [2048, S], BF16)
    v = B.scratch("v", [S, 2048], BF16)
    z2g = B.scratch("z2g", [S, 2048], F32)
    mixT = B.scratch("mixT", [D, S], BF16)
    utok = B.scratch("utok", [S, D], BF16)
    yT = B.scratch("yT", [D, S], BF16)
    MB = B.scratch("MB", [256, 128, 3, 128], BF16)
    MP = B.scratch("MP", [256, 128, NL, 128], F32)
    gatesT = B.scratch("gatesT", [N_EXP, S], F32)
    B.setup_consts()
    B.stage_ada(c, [(w[p + "_ada_w"], w[p + "_ada_b"], w[p + "_norm_g"]) for p in ("mix0", "ffn0", "mix1", "moe")])
    B.stage_ssm_prep(w["ssm_lam_re"], w["ssm_lam_im"], w["ssm_log_dt"], w["ssm_b_re"], w["ssm_b_im"],
                     w["ssm_c_re"], w["ssm_c_im"], MB, MP, NL)
    win = w["mix0_w_in"]
    B.stage_norm(xT, hT, 0)
    B.stage_linear_fm(hT, D, [dict(w=[win[:, 0:2048]], N=2048, epi="bf16", out=qT),
                              dict(w=[win[:, 2048:4096]], N=2048, epi="bf16", out=kT),
                              dict(w=[win[:, 6144:8192]], N=2048, epi="gelu", out=uT)])
    B.stage_linear_tm(hT, win[:, 4096:6144], 2048, v, BF16)
    B.stage_linear_tm(hT, win[:, 8192:10240], 2048, z2g, F32, gelu=True)
    B.stage_attn(qT, kT, v, mixT[0:2048, :])
    B.stage_gmlp(uT, z2g, w["gm_ln_g"], w["gm_w_s"], w["gm_b_s"], mixT[2048:4096, :])
    B.stage_linear_fm(mixT, D, [dict(w=[w["mix0_w_out"]], N=D, epi="resid", m=0, xsrc=xT, xdst=xres)])
    B.stage_norm(xres, hT, 1)
    B.stage_ffn(hT, [(w["ffn0_w_gate"], w["ffn0_w_up"], w["ffn0_w_down"])], FFN_DENSE, 1, xres, xres)
    B.stage_norm(xres, hT, 2)
    B.stage_linear_tm(hT, w["ssm_w_in"], D, utok, BF16)
    B.stage_ssm_run(utok, MB, MP, w["ssm_d"], yT, NL)
    B.stage_linear_fm(yT, D, [dict(w=[w["glu_w_a"], w["glu_w_b"]], N=D, epi="glu", m=2, xsrc=xres, xdst=xres)])
    B.stage_norm(xres, hT, 3, router=(w["moe_w_router"], gatesT))
    B.stage_ffn(hT, [(w["moe_w_gate"][e], w["moe_w_up"][e], w["moe_w_down"][e]) for e in range(N_EXP)],
                FFN_EXP, 3, xres, xres, gatesT=gatesT)
    B.stage_norm(xres, outT, 0, out_dtype=F32, final_g=w["final_norm_g"])
    return B


def weight_map(inputs):
    m = {}
    for n, shp in W_SPECS:
        m[n] = np.ascontiguousarray(np.asarray(inputs[n], dtype=np.float32).reshape(shp))
    return m


SEQ = 4096
N_CORES = 2


def kernel(**inputs):
    x = np.asarray(inputs["x"], dtype=np.float32)
    c = np.asarray(inputs["c"], dtype=np.float32)
    bsz = x.shape[0]
    B = build_model(SEQ)
    wm = weight_map(inputs)
    in_maps = []
    for b in range(bsz):
        m = dict(wm)
        m["xT"] = np.ascontiguousarray(x[b].T)
        m["c"] = np.ascontiguousarray(c[b:b + 1])
        in_maps.append(m)
    res = run_bass_kernel_spmd(B.nc, in_maps, core_ids=list(range(bsz)))
    out = np.stack([np.ascontiguousarray(res.results[b]["outT"].T) for b in range(bsz)], axis=0)
    return out.astype(np.float32)
```
